# Optimizing a Trainium2 kernel written in Bass

```python
import math
import jax, jax.numpy as jnp
from jax import lax
import numpy as np

D_MODEL = 1024
BATCH = 16
SEQ = 4096
DEPTH = 4

GRID_W = 64
CTX_LEN = 256
HEAD_DIM = 64
ATTN_WIDTH = D_MODEL // 2
ATTN_HEADS = ATTN_WIDTH // HEAD_DIM
KV_HEADS = ATTN_HEADS // 4
GQA_GROUP = ATTN_HEADS // KV_HEADS
Q_BLOCK = 128
ATTN_SCALE = HEAD_DIM ** -0.5
ROPE_THETA = 10000.0
ROPE_NF = HEAD_DIM // 4
DN_WIDTH = D_MODEL - ATTN_WIDTH
DN_DIM = 64
DN_HEADS = DN_WIDTH // DN_DIM
DN_SCALE = DN_DIM ** -0.5
CONV_K = 5
CHUNK = 64
N_GROUPS = 4
EXPERTS_PER_GROUP = 8
N_EXPERTS = N_GROUPS * EXPERTS_PER_GROUP
TOP_K = 2
EXPERT_FF = D_MODEL // 4
MOE_BLOCK = 128
EPS = 1e-6
PROJ_SIZES = (ATTN_WIDTH, KV_HEADS * HEAD_DIM, KV_HEADS * HEAD_DIM, 3 * DN_WIDTH, DN_WIDTH, 2 * DN_HEADS, 2 * DN_HEADS)
PROJ_DIM = sum(PROJ_SIZES)
SPLIT_IDX = tuple(int(v) for v in np.cumsum(PROJ_SIZES)[:-1])

kernel_name = "hybrid_gqa_gdn_hmoe_diffusion_trunk"


def rms_norm(x, w):
    xf = x.astype(jnp.float32)
    y = xf * lax.rsqrt(jnp.mean(xf * xf, axis=-1, keepdims=True) + EPS)
    return (y * w.astype(jnp.float32)).astype(x.dtype)


def l2_normalize(x):
    xf = x.astype(jnp.float32)
    return (xf * lax.rsqrt(jnp.sum(xf * xf, axis=-1, keepdims=True) + EPS)).astype(x.dtype)


def modulate(h, shift, scale):
    return h * (1.0 + scale.astype(h.dtype)) + shift.astype(h.dtype)


def axial_rope_tables(n):
    rows = n // GRID_W
    pos_row = jnp.repeat(jnp.arange(rows, dtype=jnp.float32), GRID_W)
    pos_col = jnp.tile(jnp.arange(GRID_W, dtype=jnp.float32), rows)
    inv = ROPE_THETA ** (-jnp.arange(ROPE_NF, dtype=jnp.float32) / ROPE_NF)
    ang = jnp.stack([pos_row, pos_col], axis=-1)[..., None] * inv
    return jnp.cos(ang), jnp.sin(ang)


def apply_rope(x, rope):
    cos, sin = rope
    B, S, H, _ = x.shape
    xf = x.astype(jnp.float32).reshape(B, S, H, 2, 2, ROPE_NF)
    c, s = cos[None, :, None], sin[None, :, None]
    x1, x2 = xf[..., 0, :], xf[..., 1, :]
    out = jnp.stack([x1 * c - x2 * s, x1 * s + x2 * c], axis=-2)
    return out.reshape(B, S, H, HEAD_DIM).astype(x.dtype)


def short_conv(x, w):
    return lax.conv_general_dilated(
        x, w[:, None, :].astype(x.dtype), window_strides=(1,),
        padding=((CONV_K // 2, CONV_K // 2),),
        dimension_numbers=("NWC", "WIO", "NWC"), feature_group_count=x.shape[-1])


def gqa_attend(q, k, v):
    s = jnp.einsum("bqkgd,bskd->bkgqs", q, k).astype(jnp.float32) * ATTN_SCALE
    p = jax.nn.softmax(s, axis=-1).astype(v.dtype)
    return jnp.einsum("bkgqs,bskd->bqkgd", p, v)


def gated_delta_chunked(q, k, v, g, beta, s0):
    out_dtype = v.dtype
    B, T, H, Dk = q.shape
    n = T // CHUNK
    f32 = jnp.float32
    to_chunks = lambda a: jnp.moveaxis(a.astype(f32).reshape(B, n, CHUNK, H, -1), 3, 1)
    qc, kc, vc = to_chunks(q), to_chunks(k), to_chunks(v)
    gcum = jnp.cumsum(to_chunks(g[..., None])[..., 0], axis=-1)
    bc = to_chunks(beta[..., None])
    incl = jnp.tril(jnp.ones((CHUNK, CHUNK), dtype=bool))
    strict = jnp.tril(jnp.ones((CHUNK, CHUNK), dtype=bool), -1)
    diff = gcum[..., :, None] - gcum[..., None, :]
    decay = jnp.where(incl, jnp.exp(jnp.where(incl, diff, 0.0)), 0.0)
    kb = kc * bc
    lower = jnp.where(strict, jnp.einsum("bhnid,bhnjd->bhnij", kb, kc) * decay, 0.0)
    eye = jnp.eye(CHUNK, dtype=f32)
    tmat = lax.linalg.triangular_solve(lower + eye, jnp.broadcast_to(eye, lower.shape),
                                       left_side=True, lower=True, unit_diagonal=True)
    u = jnp.einsum("bhnij,bhnje->bhnie", tmat, vc * bc)
    w = jnp.einsum("bhnij,bhnjd->bhnid", tmat, kb * jnp.exp(gcum)[..., None])
    intra = jnp.where(incl, jnp.einsum("bhnid,bhnjd->bhnij", qc, kc) * decay, 0.0)
    q_dec = qc * jnp.exp(gcum)[..., None]
    g_last = gcum[..., -1]
    k_dec = kc * jnp.exp(g_last[..., None] - gcum)[..., None]

    def step(state, xs):
        u_i, w_i, qd_i, kd_i, a_i, gl_i = xs
        v_new = u_i - jnp.einsum("bhid,bhde->bhie", w_i, state)
        o_i = jnp.einsum("bhid,bhde->bhie", qd_i, state) + jnp.einsum("bhij,bhje->bhie", a_i, v_new)
        state = state * jnp.exp(gl_i)[..., None, None] + jnp.einsum("bhid,bhie->bhde", kd_i, v_new)
        return state, o_i

    xs = tuple(jnp.moveaxis(t, 2, 0) for t in (u, w, q_dec, k_dec, intra, g_last))
    s_final, o = lax.scan(step, s0, xs)
    o = jnp.transpose(o, (1, 0, 3, 2, 4)).reshape(B, T, H, v.shape[-1])
    return o.astype(out_dtype), s_final


def mixer_inputs(h, lp, rope):
    B, S, _ = h.shape
    aq, ak, av, dqkv, dgate, dbeta, dalpha = jnp.split(h @ lp["w_in"], SPLIT_IDX, axis=-1)
    q = rms_norm(aq.reshape(B, S, ATTN_HEADS, HEAD_DIM), lp["q_norm_w"])
    k = rms_norm(ak.reshape(B, S, KV_HEADS, HEAD_DIM), lp["k_norm_w"])
    if rope is not None:
        q, k = apply_rope(q, rope), apply_rope(k, rope)
    dq, dk, dv = jnp.split(jax.nn.silu(short_conv(dqkv, lp["conv_w"])), 3, axis=-1)
    shp = (B, S, DN_HEADS, DN_DIM)
    beta = jax.nn.sigmoid(dbeta.astype(jnp.float32)).reshape(B, S, 2, DN_HEADS)
    g = -jnp.exp(lp["dn_A_log"].astype(jnp.float32)) * jax.nn.softplus(
        dalpha.astype(jnp.float32).reshape(B, S, 2, DN_HEADS) + lp["dn_dt_bias"].astype(jnp.float32))
    return dict(q=q, k=k, v=av.reshape(B, S, KV_HEADS, HEAD_DIM),
                dq=l2_normalize(dq.reshape(shp)) * DN_SCALE, dk=l2_normalize(dk.reshape(shp)),
                dv=dv.reshape(shp), gate=dgate.reshape(shp), beta=beta, g=g)


def merge_groups(a, o_delta, gate, lp):
    B, S = a.shape[:2]
    dn = rms_norm(o_delta, lp["dn_norm_w"]) * jax.nn.silu(gate)
    mixed = jnp.concatenate([a, dn.reshape(B, S, DN_WIDTH).astype(a.dtype)], axis=-1)
    return mixed @ lp["w_out"]


def hybrid_mixer(h_lat, h_ctx, lp, rope, ctx_out):
    B, S, _ = h_lat.shape
    L = h_ctx.shape[1]
    pl = mixer_inputs(h_lat, lp, rope)
    pc = mixer_inputs(h_ctx, lp, None)
    k_all = jnp.concatenate([pc["k"], pl["k"]], axis=1)
    v_all = jnp.concatenate([pc["v"], pl["v"]], axis=1)
    nb = S // Q_BLOCK
    qb = jnp.moveaxis(pl["q"].reshape(B, nb, Q_BLOCK, KV_HEADS, GQA_GROUP, HEAD_DIM), 1, 0)
    a_lat = lax.map(lambda qi: gqa_attend(qi, k_all, v_all), qb)
    a_lat = jnp.moveaxis(a_lat, 0, 1).reshape(B, S, ATTN_WIDTH)
    zero = jnp.zeros((B, DN_HEADS, DN_DIM, DN_DIM), jnp.float32)
    d_lat, d_ctx = 0.0, 0.0
    for d in range(2):
        rev = (lambda a: jnp.flip(a, axis=1)) if d == 1 else (lambda a: a)
        o_c, s_c = gated_delta_chunked(*(rev(t) for t in (pc["dq"], pc["dk"], pc["dv"], pc["g"][:, :, d], pc["beta"][:, :, d])), zero)
        o_l, _ = gated_delta_chunked(*(rev(t) for t in (pl["dq"], pl["dk"], pl["dv"], pl["g"][:, :, d], pl["beta"][:, :, d])), s_c)
        d_lat = d_lat + rev(o_l)
        d_ctx = d_ctx + rev(o_c)
    y_lat = merge_groups(a_lat, d_lat, pl["gate"], lp)
    if not ctx_out:
        return y_lat, None
    a_ctx = gqa_attend(pc["q"].reshape(B, L, KV_HEADS, GQA_GROUP, HEAD_DIM), pc["k"], pc["v"]).reshape(B, L, ATTN_WIDTH)
    y_ctx = merge_groups(a_ctx, d_ctx, pc["gate"], lp)
    return y_lat, y_ctx


def hier_moe(h, lp):
    N, D = h.shape
    g_logit = (h @ lp["rg_w"]).astype(jnp.float32) + lp["rg_b"].astype(jnp.float32)
    g_prob = jax.nn.softmax(g_logit, axis=-1)
    g_sel = jnp.argmax(g_logit, axis=-1)
    p_g = jnp.take_along_axis(g_prob, g_sel[:, None], axis=-1)
    e_logit = ((h @ lp["re_w"]).astype(jnp.float32) + lp["re_b"].astype(jnp.float32)).reshape(N, N_GROUPS, EXPERTS_PER_GROUP)
    e_prob = jax.nn.softmax(e_logit[jnp.arange(N), g_sel], axis=-1)
    top_p, top_i = lax.top_k(e_prob, TOP_K)
    wts = top_p / jnp.sum(top_p, axis=-1, keepdims=True) * p_g
    eid = (g_sel[:, None] * EXPERTS_PER_GROUP + top_i).reshape(-1).astype(jnp.int32)
    A = N * TOP_K
    tok = jnp.repeat(jnp.arange(N, dtype=jnp.int32), TOP_K)
    order = jnp.argsort(eid)
    e_s, tok_s, w_s = eid[order], tok[order], wts.reshape(-1)[order]
    counts = jnp.zeros((N_EXPERTS,), jnp.int32).at[eid].add(1)
    padded = (counts + MOE_BLOCK - 1) // MOE_BLOCK * MOE_BLOCK
    pad_end = jnp.cumsum(padded)
    pad_start = pad_end - padded
    seg_start = jnp.cumsum(counts) - counts
    dest = pad_start[e_s] + jnp.arange(A, dtype=jnp.int32) - seg_start[e_s]
    n_blocks = -(-A // MOE_BLOCK) + N_EXPERTS
    P = n_blocks * MOE_BLOCK
    slot_tok = jnp.zeros((P,), jnp.int32).at[dest].set(tok_s)
    xb = h[slot_tok].reshape(n_blocks, MOE_BLOCK, D)
    blk_e = jnp.minimum(jnp.searchsorted(pad_end, jnp.arange(n_blocks, dtype=jnp.int32) * MOE_BLOCK, side="right"), N_EXPERTS - 1)
    w1, w3, w2 = lp["w1"], lp["w3"], lp["w2"]

    def expert_block(args):
        xi, e = args
        return (jax.nn.silu(xi @ w1[e]) * (xi @ w3[e])) @ w2[e]

    y = lax.map(expert_block, (xb, blk_e)).reshape(P, D)
    return jax.ops.segment_sum(y[dest] * w_s[:, None].astype(y.dtype), tok_s, num_segments=N)


def setup_inputs(seed: int = 0) -> dict:
    key = jax.random.key(seed)
    ks = jax.random.split(key, 24)
    f32 = jnp.float32
    nrm = lambda k, shape, s: jax.random.normal(k, shape, f32) * s
    gain = lambda k, shape: 1.0 + 0.05 * jax.random.normal(k, shape, f32)
    dt = jnp.exp(jax.random.uniform(ks[10], (DEPTH, 2, DN_HEADS), f32, math.log(1e-3), math.log(1e-1)))
    return {
        "x": nrm(ks[0], (BATCH, SEQ, D_MODEL), 1.0),
        "c": nrm(ks[1], (BATCH, D_MODEL), 1.0),
        "ctx": nrm(ks[2], (BATCH, CTX_LEN, D_MODEL), 1.0),
        "c_ctx": nrm(ks[3], (D_MODEL,), 1.0),
        "ada_w": nrm(ks[4], (DEPTH, D_MODEL, 6 * D_MODEL), 0.5 * D_MODEL ** -0.5),
        "ada_b": nrm(ks[5], (DEPTH, 6 * D_MODEL), 0.01),
        "norm_mix_w": gain(ks[6], (DEPTH, D_MODEL)),
        "norm_ffn_w": gain(ks[7], (DEPTH, D_MODEL)),
        "w_in": nrm(ks[8], (DEPTH, D_MODEL, PROJ_DIM), D_MODEL ** -0.5),
        "q_norm_w": gain(ks[9], (DEPTH, HEAD_DIM)),
        "k_norm_w": gain(ks[11], (DEPTH, HEAD_DIM)),
        "conv_w": nrm(ks[12], (DEPTH, CONV_K, 3 * DN_WIDTH), CONV_K ** -0.5),
        "dn_A_log": jnp.log(jax.random.uniform(ks[13], (DEPTH, 2, DN_HEADS), f32, 1.0, 16.0)),
        "dn_dt_bias": dt + jnp.log(-jnp.expm1(-dt)),
        "dn_norm_w": gain(ks[14], (DEPTH, DN_DIM)),
        "w_out": nrm(ks[15], (DEPTH, D_MODEL, D_MODEL), D_MODEL ** -0.5),
        "rg_w": nrm(ks[16], (DEPTH, D_MODEL, N_GROUPS), D_MODEL ** -0.5),
        "rg_b": nrm(ks[17], (DEPTH, N_GROUPS), 0.01),
        "re_w": nrm(ks[18], (DEPTH, D_MODEL, N_EXPERTS), D_MODEL ** -0.5),
        "re_b": nrm(ks[19], (DEPTH, N_EXPERTS), 0.01),
        "w1": nrm(ks[20], (DEPTH, N_EXPERTS, D_MODEL, EXPERT_FF), D_MODEL ** -0.5),
        "w3": nrm(ks[21], (DEPTH, N_EXPERTS, D_MODEL, EXPERT_FF), D_MODEL ** -0.5),
        "w2": nrm(ks[22], (DEPTH, N_EXPERTS, EXPERT_FF, D_MODEL), EXPERT_FF ** -0.5),
        "final_norm_w": gain(ks[23], (D_MODEL,)),
    }


def reference(x, c, ctx, c_ctx, ada_w, ada_b, norm_mix_w, norm_ffn_w, w_in, q_norm_w, k_norm_w,
              conv_w, dn_A_log, dn_dt_bias, dn_norm_w, w_out, rg_w, rg_b, re_w, re_b, w1, w3, w2,
              final_norm_w):
    B, S, D = x.shape
    L = ctx.shape[1]
    rope = axial_rope_tables(S)
    sc, scc = jax.nn.silu(c), jax.nn.silu(c_ctx)
    for i in range(DEPTH):
        lp = dict(w_in=w_in[i], q_norm_w=q_norm_w[i], k_norm_w=k_norm_w[i], conv_w=conv_w[i],
                  dn_A_log=dn_A_log[i], dn_dt_bias=dn_dt_bias[i], dn_norm_w=dn_norm_w[i], w_out=w_out[i],
                  rg_w=rg_w[i], rg_b=rg_b[i], re_w=re_w[i], re_b=re_b[i], w1=w1[i], w3=w3[i], w2=w2[i])
        last = i == DEPTH - 1
        m_lat = jnp.split((sc @ ada_w[i] + ada_b[i])[:, None, :], 6, axis=-1)
        m_ctx = jnp.split(scc @ ada_w[i] + ada_b[i], 6, axis=-1)
        h_lat = modulate(rms_norm(x, norm_mix_w[i]), m_lat[0], m_lat[1])
        h_ctx = modulate(rms_norm(ctx, norm_mix_w[i]), m_ctx[0], m_ctx[1])
        y_lat, y_ctx = hybrid_mixer(h_lat, h_ctx, lp, rope, not last)
        x = x + m_lat[2].astype(x.dtype) * y_lat
        h_lat = modulate(rms_norm(x, norm_ffn_w[i]), m_lat[3], m_lat[4])
        if last:
            x = x + m_lat[5].astype(x.dtype) * hier_moe(h_lat.reshape(-1, D), lp).reshape(B, S, D)
        else:
            ctx = ctx + m_ctx[2].astype(ctx.dtype) * y_ctx
            h_ctx = modulate(rms_norm(ctx, norm_ffn_w[i]), m_ctx[3], m_ctx[4])
            f = hier_moe(jnp.concatenate([h_ctx.reshape(-1, D), h_lat.reshape(-1, D)], axis=0), lp)
            ctx = ctx + m_ctx[5].astype(ctx.dtype) * f[:B * L].reshape(B, L, D)
            x = x + m_lat[5].astype(x.dtype) * f[B * L:].reshape(B, S, D)
    return rms_norm(x, final_norm_w)
```

```python
import contextlib
import math
import numpy as np
import concourse.bass as bass
import concourse.mybir as mybir
from concourse.bass_utils import run_bass_kernel_spmd

F32 = mybir.dt.float32
BF16 = mybir.dt.bfloat16
AF = mybir.ActivationFunctionType
ALU = mybir.AluOpType
AX = mybir.AxisListType

D = 1024
NH = 8
HD = 64
EPS = 1e-6
CQ, CK, CV, CDQKV, CGATE, CBA, CRQ, CRK, WEXT = 0, 512, 640, 768, 2304, 2816, 2848, 3360, 3488
NEXP = 32
FF = 256
BIG = 1.0e5
CH = 128


class Buf:
    def __init__(self, t, key):
        self.t = t
        self.key = key

    def __getitem__(self, idx):
        return self.t[idx]


class Sched:
    NSLOT = 16

    def __init__(self, nc):
        self.nc = nc
        self.es = contextlib.ExitStack()
        self.engs = {"pe": nc.tensor, "act": nc.scalar, "dve": nc.vector, "pool": nc.gpsimd, "sp": nc.sync}
        self.sem = {}
        self.cnt = {}
        for n in self.engs:
            self.sem[n] = self.es.enter_context(nc.semaphore("s_" + n))
            self.cnt[n] = 0
        for i in range(self.NSLOT):
            k = ("d", i)
            self.sem[k] = self.es.enter_context(nc.semaphore("s_d%d" % i))
            self.cnt[k] = 0
        self.waited = {n: {} for n in self.engs}
        self.res = {}
        self.slot = 0
        self.ninst = 0
        self.dead = False
        self.excl = set()

    def sbuf(self, name, shape, dtype=F32, stack=None):
        self.nbuf = getattr(self, "nbuf", 0) + 1
        name = "%s_u%d" % (name, self.nbuf)
        t = (stack or self.es).enter_context(self.nc.sbuf_tensor(name, list(shape), dtype))
        return Buf(t, name)

    def psum(self, name, shape, dtype=F32):
        t = self.es.enter_context(self.nc.psum_tensor(name, list(shape), dtype))
        self.excl.add(name)
        return Buf(t, name)

    def _val(self, k, c):
        return c * 16 if isinstance(k, tuple) else c

    def _keys(self, xs):
        return [x.key if isinstance(x, Buf) else x for x in xs]

    def _deps(self, me, reads, writes):
        deps = {}
        for r in reads:
            st = self.res.get(r)
            if st is None:
                continue
            for k, c in st[0].items():
                if k == me and me == "pe":
                    continue
                if c > deps.get(k, 0):
                    deps[k] = c
            if r in self.excl:
                for k, c in st[1].items():
                    if k != me and c > deps.get(k, 0):
                        deps[k] = c
        for w in writes:
            st = self.res.get(w)
            if st is None:
                continue
            for dd in st:
                for k, c in dd.items():
                    if (k != me or me == "pool") and c > deps.get(k, 0):
                        deps[k] = c
        return deps

    def _wait(self, eng, deps):
        wd = self.waited[eng]
        e = self.engs[eng]
        for k, c in deps.items():
            v = self._val(k, c)
            if wd.get(k, 0) >= v:
                continue
            e.wait_ge(self.sem[k], v)
            wd[k] = v

    def _commit(self, me, reads, writes):
        c = self.cnt[me]
        for r in reads:
            st = self.res.setdefault(r, [{}, {}])
            st[1][me] = c
        for w in writes:
            st = self.res.setdefault(w, [{}, {}])
            st[0] = {me: c}
            st[1] = {}

    def op(self, eng, fn, reads=(), writes=()):
        if self.dead:
            return None
        reads = self._keys(reads)
        writes = self._keys(writes)
        self._wait(eng, self._deps(eng, reads, writes))
        ins = fn(self.engs[eng])
        ins.then_inc(self.sem[eng], 1)
        self.cnt[eng] += 1
        self.ninst += 1
        self._commit(eng, reads, writes)
        return ins

    def dma(self, out, in_, reads=(), writes=(), eng="sp", **kw):
        if self.dead:
            return None
        reads = self._keys(reads)
        writes = self._keys(writes)
        k = ("d", self.slot)
        self.slot = (self.slot + 1) % self.NSLOT
        deps = self._deps(k, reads, writes)
        if self.cnt[k] > 0:
            deps[k] = max(deps.get(k, 0), self.cnt[k])
        self._wait(eng, deps)
        ins = self.engs[eng].dma_start(out=out, in_=in_, **kw)
        ins.then_inc(self.sem[k], 16)
        self.cnt[k] += 1
        self.ninst += 1
        self._commit(k, reads, writes)
        return ins

    def barrier(self):
        if self.dead:
            return
        deps = {}
        for k, c in self.cnt.items():
            if c > 0 and k != "sp":
                deps[k] = c
        for n in self.engs:
            d = {k: c for k, c in deps.items() if k != n}
            self._wait(n, d)
        self.res = {}

    def finish(self):
        if self.dead:
            return
        deps = {k: c for k, c in self.cnt.items() if c > 0 and k != "sp"}
        self._wait("sp", deps)

    def close(self):
        self.es.close()


def _tiles(L, S):
    ts = [(0, L)]
    for i in range(S // 512):
        ts.append((L + i * 512, 512))
    return ts


def build(cfg):
    NB, S, L, DEPTH = cfg["NB"], cfg["S"], cfg["L"], cfg["DEPTH"]
    taps = cfg.get("taps", ())
    stop = cfg.get("stop", "end")
    T = S + L
    TP = T
    NT128 = T // 128
    NCH = T // CH
    tiles = _tiles(L, S)
    PL = cfg.get('pooleng', 'dve')
    nc = bass.Bass("TRN2", target_bir_lowering=False)

    def din(name, shape, dt=F32):
        return nc.dram_tensor(name, list(shape), dt, kind="ExternalInput").ap()

    dbg_kind = "ExternalOutput" if taps else "Internal"

    def dscr(name, shape, dt=F32):
        kind = "ExternalOutput" if name in taps else "Internal"
        return nc.dram_tensor(name, list(shape), dt, kind=kind).ap()

    x_in = din("x", [NB, S, D])
    ctx_in = din("ctx", [NB, L, D])
    cT_in = din("cT", [128, 8, NB + 1])
    ada_w = din("ada_w", [DEPTH, D, 6 * D])
    ada_bT = din("ada_bT", [DEPTH, 128, 48])
    nmixT = din("nmixT", [DEPTH, 128, 8])
    nffnT = din("nffnT", [DEPTH, 128, 8])
    w_in_ext = din("w_in_ext", [DEPTH, D, WEXT])
    qkw = din("qkw", [DEPTH, 128, 4])
    qkw_row = din("qkw_row", [DEPTH, 2, 64])
    conv_wT = din("conv_wT", [DEPTH, 128, 12, 5])
    dn_A_log = din("dn_A_log", [DEPTH, 16])
    dn_dt_bias = din("dn_dt_bias", [DEPTH, 16])
    dn_norm_w = din("dn_norm_w", [DEPTH, 64])
    w_out = din("w_out", [DEPTH, D, D])
    wr_in = din("wr", [DEPTH, D, 36])
    rb_in = din("rb", [DEPTH, 36])
    w1_in = din("w1", [DEPTH, NEXP, D, FF])
    w3_in = din("w3", [DEPTH, NEXP, D, FF])
    w2_in = din("w2", [DEPTH, NEXP, FF, D])
    fnwT = din("fnwT", [128, 8])
    c_ident = din("c_ident", [128, 128])
    c_bd = din("c_bd", [128, 128])
    c_cos = din("c_cos", [128, T])
    c_sin = din("c_sin", [128, T])
    c_masks = din("c_masks", [6, 128, 128])
    out_hbm = nc.dram_tensor("out", [NB, S, D], F32, kind="ExternalOutput").ap()

    xT = dscr("xT", [NB, D, T])
    dq_raw = dscr("dq_raw", [NB, 1536, TP])
    gate_s = dscr("gate_s", [NB, T, 512])
    gb = dscr("gb", [NB, T, 32])
    attnT = dscr("attnT", [NB, NH, HD, T], BF16)
    qnT = dscr("qnT", [NB, 512, T], BF16)
    knT = dscr("knT", [NB, 512, T], BF16)
    k_tok = dscr("k_tok", [NB, T, 512])
    v_tok = dscr("v_tok", [NB, T, 512])
    o_dir = [dscr("o_f", [NB, T, 512]), dscr("o_b", [NB, T, 512])]
    h2T = dscr("h2T", [NB, D, T], BF16)
    WtT = dscr("WtT", [NB, NEXP, T])
    tapd = {}
    for nm, shp, dt in [("t_qT", [NB, 128, 4, T], BF16), ("t_kT", [NB, 128, T], BF16), ("t_V", [NB, 128, NT128, 200], BF16),
                        ("t_mod", [DEPTH, 128, 48, NB + 1], F32)]:
        if nm in taps:
            tapd[nm] = nc.dram_tensor(nm, shp, dt, kind="ExternalOutput").ap()

    s = Sched(nc)
    _tapped = set()

    def tap(name, buf, ap, shape, dt=F32):
        if ("dbg_" + name) not in taps or name in _tapped:
            return
        _tapped.add(name)
        dtens = nc.dram_tensor("dbg_" + name, list(shape), dt, kind="ExternalOutput").ap()
        s.dma(dtens, ap, reads=[buf])

    ps = [s.psum("ps%d" % i, [128, 512]) for i in range(4)]
    psS = [s.psum("psS%d" % i, [128, 1024]) for i in range(2)]
    for i in range(2):
        for hf in range(2):
            kname = "psv%d" % (4 + i * 2 + hf)
            s.excl.add(kname)
            ps.append(Buf(psS[i].t[:, hf * 512:(hf + 1) * 512], kname))

    ident = s.sbuf("ident", [128, 128])
    identb = s.sbuf("identb", [128, 128], BF16)
    ones = s.sbuf("ones", [128, 128])
    onesm = s.sbuf("onesm", [128, 128])
    bd64 = s.sbuf("bd64", [128, 128])
    bd1 = s.sbuf("bd1", [128, 128])
    epsc = s.sbuf("epsc", [128, 1])
    mod = [s.sbuf("mod%d" % l, [128, 48, NB + 1]) for l in range(DEPTH)]
    amix = [s.sbuf("amix%d" % l, [128, 8, NB + 1]) for l in range(DEPTH)]
    affn = [s.sbuf("affn%d" % l, [128, 8, NB + 1]) for l in range(DEPTH)]
    s.dma(ident[:], c_ident[:, :], writes=[ident])
    s.dma(bd1[:], c_bd[:, :], writes=[bd1])
    s.op("dve", lambda e: e.tensor_copy(out=identb[:], in_=ident[:]), reads=[ident], writes=[identb])
    s.op("dve", lambda e: e.memset(ones[:], 1.0), writes=[ones])
    s.op("dve", lambda e: e.memset(onesm[:], 1.0 / D), writes=[onesm])
    s.op("dve", lambda e: e.memset(epsc[:], EPS), writes=[epsc])
    s.op("dve", lambda e: e.tensor_scalar(out=bd64[:], in0=bd1[:], scalar1=1.0 / 64, scalar2=None, op0=ALU.mult), reads=[bd1], writes=[bd64])

    def rsqrt_(eng_ln, out_ap, in_ap, reads, writes, tmp, scale=1.0):
        s.op("act", lambda e: e.activation(out=tmp, in_=in_ap, func=AF.Ln, bias=epsc[:, 0:1], scale=scale), reads=list(reads) + [epsc], writes=writes)
        s.op("act", lambda e: e.activation(out=out_ap, in_=tmp, func=AF.Exp, scale=-0.5), reads=writes, writes=writes)

    with contextlib.ExitStack() as ph:
        scT = s.sbuf("scT", [128, 8, NB + 1], stack=ph)
        sct = s.sbuf("sct", [128, 8, NB + 1], stack=ph)
        adab = s.sbuf("adab", [128, 48], stack=ph)
        nw = s.sbuf("nw", [128, 8], stack=ph)
        nw2 = s.sbuf("nw2", [128, 8], stack=ph)
        awp = [s.sbuf("awp%d" % i, [128, 8, 1024], stack=ph) for i in range(2)]
        s.dma(scT[:], cT_in[:, :, :], writes=[scT])
        s.op("act", lambda e: e.activation(out=sct[:], in_=scT[:], func=AF.Exp, scale=-1.0), reads=[scT], writes=[sct])
        s.op("dve", lambda e: e.tensor_scalar(out=sct[:], in0=sct[:], scalar1=1.0, scalar2=None, op0=ALU.add), reads=[sct], writes=[sct])
        s.op("dve", lambda e: e.reciprocal(out=sct[:], in_=sct[:]), reads=[sct], writes=[sct])
        s.op("dve", lambda e: e.tensor_tensor(out=scT[:], in0=scT[:], in1=sct[:], op=ALU.mult), reads=[scT, sct], writes=[scT])
        NJ = NB + 1
        for l in range(DEPTH):
            s.dma(adab[:], ada_bT[l, :, :], writes=[adab])
            s.dma(nw[:], nmixT[l, :, :], writes=[nw])
            s.dma(nw2[:], nffnT[l, :, :], writes=[nw2])
            for piece in range(6):
                aw = awp[piece % 2]
                s.dma(aw[:], ada_w[l, :, piece * 1024:(piece + 1) * 1024].rearrange("(kc p) n -> p kc n", p=128), writes=[aw])
                pb = ps[piece % 2]
                for oc in range(8):
                    for kc in range(8):
                        s.op("pe", lambda e: e.matmul(pb[:, oc * NJ:(oc + 1) * NJ], lhsT=aw[:, kc, oc * 128:(oc + 1) * 128], rhs=scT[:, kc, :],
                                                     start=(kc == 0), stop=(kc == 7)), reads=[aw, scT], writes=[pb])
                s.op("dve", lambda e: e.tensor_tensor(out=mod[l][:, piece * 8:(piece + 1) * 8, :],
                                                      in0=pb[:, 0:8 * NJ].rearrange("p (a b) -> p a b", b=NJ),
                                                      in1=adab[:, piece * 8:(piece + 1) * 8].unsqueeze(2).broadcast_to([128, 8, NJ]), op=ALU.add),
                     reads=[pb, adab], writes=[mod[l]])
            for (dst, wv, off) in ((amix[l], nw, 8), (affn[l], nw2, 32)):
                s.op("dve", lambda e: e.tensor_scalar(out=dst[:], in0=mod[l][:, off:off + 8, :], scalar1=1.0, scalar2=None, op0=ALU.add), reads=[mod[l]], writes=[dst])
                s.op("dve", lambda e: e.tensor_tensor(out=dst[:], in0=dst[:], in1=wv[:].unsqueeze(2).broadcast_to([128, 8, NJ]), op=ALU.mult), reads=[dst, wv], writes=[dst])
            if "t_mod" in tapd:
                s.dma(tapd["t_mod"][l], mod[l][:], reads=[mod[l]])
        s.barrier()
    if stop == "prep":
        s.finish(); s.close(); return nc

    with contextlib.ExitStack() as ph:
        xin = [s.sbuf("xin%d" % i, [128, 4, D], stack=ph) for i in range(2)]
        xo = [s.sbuf("xo%d" % i, [128, 8, 512], stack=ph) for i in range(2)]
        zt = s.sbuf("zt", [128, 2], stack=ph)
        s.op("dve", lambda e: e.memset(zt[:], 0.0), writes=[zt])
        it = 0
        for b in range(NB):
            for (t0, n) in tiles:
                xi, xb_ = xin[it % 2], xo[it % 2]
                nsub = n // 128
                src = ctx_in[b, t0:t0 + n, :] if t0 < L else x_in[b, t0 - L:t0 - L + n, :]
                s.dma(xi[:, 0:nsub, :], src.rearrange("(s p) f -> p s f", p=128), writes=[xi])
                for fc in range(8):
                    pb = ps[fc % 4]
                    for sub in range(nsub):
                        s.op("pe", lambda e: e.transpose(pb[:, sub * 128:(sub + 1) * 128], xi[:, sub, fc * 128:(fc + 1) * 128], ident[:]), reads=[xi, ident], writes=[pb])
                    if fc % 2 == 0:
                        s.op("act", lambda e: e.copy(out=xb_[:, fc, 0:n], in_=pb[:, 0:n]), reads=[pb], writes=[xb_])
                    else:
                        s.op("dve", lambda e: e.tensor_copy(out=xb_[:, fc, 0:n], in_=pb[:, 0:n]), reads=[pb], writes=[xb_])
                s.dma(xT[b, :, t0:t0 + n].rearrange("(c p) t -> p c t", p=128), xb_[:, :, 0:n], reads=[xb_], writes=[("xT", b, t0)])
                it += 1
        s.barrier()

    if stop == "s0":
        s.finish(); s.close(); return nc

    for l in range(DEPTH):
        last = (l == DEPTH - 1)
        with contextlib.ExitStack() as ph:
            winb = s.sbuf("winb", [128, 8, WEXT], BF16, stack=ph)
            qkwt = s.sbuf("qkwt", [128, 4], stack=ph)
            qkrow = s.sbuf("qkrow", [128, 128], stack=ph)
            nshift = s.sbuf("nshift", [128, 1], stack=ph)
            mx = s.sbuf("mx", [128, 2], stack=ph)
            alog = s.sbuf("alog", [128, 16], stack=ph)
            dtb = s.sbuf("dtb", [128, 16], stack=ph)
            qTb = s.sbuf("qTb", [128, 4, T], BF16, stack=ph)
            kTb = s.sbuf("kTb", [128, T], BF16, stack=ph)
            Vb = s.sbuf("Vb", [128, NT128, 200], BF16, stack=ph)
            phw = contextlib.ExitStack()
            wst = [s.sbuf("wst%d" % i, [128, 872], stack=phw) for i in range(2)]
            ci = 0
            for kc in range(8):
                for q4 in range(4):
                    st = wst[ci % 2]
                    s.dma(st[:], w_in_ext[l, kc * 128:(kc + 1) * 128, q4 * 872:(q4 + 1) * 872], writes=[st])
                    eng = "act" if ci % 2 == 0 else "dve"
                    if eng == "act":
                        s.op("act", lambda e: e.copy(out=winb[:, kc, q4 * 872:(q4 + 1) * 872], in_=st[:]), reads=[st], writes=[winb])
                    else:
                        s.op("dve", lambda e: e.tensor_copy(out=winb[:, kc, q4 * 872:(q4 + 1) * 872], in_=st[:]), reads=[st], writes=[winb])
                    ci += 1
            s.barrier()
            phw.close()
            s.dma(qkwt[:], qkw[l, :, :], writes=[qkwt])
            s.dma(qkrow[:], qkw_row[l:l + 1, :, :].rearrange("o a b -> o (a b)").partition_broadcast(128), writes=[qkrow])
            s.dma(alog[:], dn_A_log[l:l + 1, :].partition_broadcast(128), writes=[alog])
            s.dma(dtb[:], dn_dt_bias[l:l + 1, :].partition_broadcast(128), writes=[dtb])
            s.op("act", lambda e: e.activation(out=alog[:], in_=alog[:], func=AF.Exp), reads=[alog], writes=[alog])
            s.op("dve", lambda e: e.tensor_scalar(out=alog[:], in0=alog[:], scalar1=-1.0, scalar2=None, op0=ALU.mult), reads=[alog], writes=[alog])
            s.op("dve", lambda e: e.tensor_reduce(out=mx[:, 0:1], in_=qkrow[:, 0:64], axis=AX.X, op=ALU.max, apply_absolute_value=True), reads=[qkrow], writes=[mx])
            s.op("dve", lambda e: e.tensor_reduce(out=mx[:, 1:2], in_=qkrow[:, 64:128], axis=AX.X, op=ALU.max, apply_absolute_value=True), reads=[qkrow, mx], writes=[mx])
            s.op("dve", lambda e: e.tensor_tensor(out=nshift[:], in0=mx[:, 0:1], in1=mx[:, 1:2], op=ALU.mult), reads=[mx], writes=[nshift])
            s.op("dve", lambda e: e.tensor_scalar(out=nshift[:], in0=nshift[:], scalar1=-8.0, scalar2=None, op0=ALU.mult), reads=[nshift], writes=[nshift])

            if cfg.get('cut') == 1:
                s.finish(); s.dead = True

            s.op("dve", lambda e: e.memset(Vb[:], 1.0), writes=[Vb])

            if cfg.get('cut') == 2:
                s.finish(); s.dead = True
            xt2 = [s.sbuf("xt%d" % i, [128, 8, 512], stack=ph) for i in range(1)]
            sqb = s.sbuf("sqb", [128, 8, 512], stack=ph)
            hTb = s.sbuf("hTb", [128, 8, 512], BF16, stack=ph)
            rstd = s.sbuf("rstd", [128, 512], stack=ph)
            cst = [s.sbuf("cst%d" % i, [128, 2, 512], stack=ph) for i in range(1)]
            rq = s.sbuf("rq", [128, 512], stack=ph)
            t1 = s.sbuf("t1", [128, 512], stack=ph)
            t2 = s.sbuf("t2", [128, 512], stack=ph)
            sqq = t2
            dstl = [s.sbuf("dqst%d" % i, [128, 4, 512], stack=ph) for i in range(1)]
            qzl = [[s.sbuf("qz%d_%d" % (k_, r_), [128, 1024], BF16, stack=ph) for r_ in range(2)] for k_ in range(2)]
            for k_ in range(2):
                for r_ in range(2):
                    s.op("dve", lambda e: e.memset(qzl[k_][r_][:], 0.0), writes=[qzl[k_][r_]])
            gstl = [s.sbuf("gst%d" % i, [128, 512], stack=ph) for i in range(1)]
            ge = s.sbuf("ge", [128, 512], stack=ph)
            gbt = s.sbuf("gbt", [128, 4, 32], stack=ph)
            gb1 = s.sbuf("gb1", [128, 4, 16], stack=ph)
            gb2 = s.sbuf("gb2", [128, 4, 16], stack=ph)
            ptb = [s.sbuf("ptb%d" % i, [128, 1024], BF16, stack=ph) for i in range(3)]
            rrowl = [s.sbuf("rrow%d" % i, [128, 512], stack=ph) for i in range(2)]
            aul = [s.sbuf("au%d" % i, [64, 512], stack=ph) for i in range(2)]
            ao = [s.sbuf("ao%d" % i, [64, 512], BF16, stack=ph) for i in range(2)]

            def proj(pb, col0, ncol, n):
                for kc in range(8):
                    s.op("pe", lambda e: e.matmul(pb[0:ncol, 0:n], lhsT=winb[:, kc, col0:col0 + ncol], rhs=hTb[:, kc, 0:n], start=(kc == 0), stop=(kc == 7)),
                         reads=[winb, hTb], writes=[pb])

            it = 0
            for b in range(NB):
                for (t0, n) in tiles:
                    j = NB if t0 < L else b
                    nsub = n // 128
                    xt = xt2[0]
                    cs = cst[0]
                    it += 1
                    s.dma(xt[:, :, 0:n], xT[b, :, t0:t0 + n].rearrange("(c p) t -> p c t", p=128), reads=[("xT", b, t0)], writes=[xt])
                    s.dma(cs[:, 0, 0:n], c_cos[:, t0:t0 + n], writes=[cs])
                    s.dma(cs[:, 1, 0:n], c_sin[:, t0:t0 + n], writes=[cs])
                    s.op("act", lambda e: e.activation(out=sqb[:, :, 0:n], in_=xt[:, :, 0:n], func=AF.Square), reads=[xt], writes=[sqb])
                    for c in range(8):
                        s.op("pe", lambda e: e.matmul(ps[0][:, 0:n], lhsT=onesm[:], rhs=sqb[:, c, 0:n], start=(c == 0), stop=(c == 7)), reads=[onesm, sqb], writes=[ps[0]])
                    rsqrt_("act", rstd[:, 0:n], ps[0][:, 0:n], [ps[0]], [rstd], rstd[:, 0:n])
                    s.op("dve", lambda e: e.tensor_tensor(out=sqb[:, :, 0:n], in0=xt[:, :, 0:n], in1=rstd[:, 0:n].unsqueeze(1).broadcast_to([128, 8, n]), op=ALU.mult),
                         reads=[xt, rstd], writes=[sqb])
                    for c in range(8):
                        s.op("dve", lambda e: e.tensor_scalar(out=hTb[:, c, 0:n], in0=sqb[:, c, 0:n], scalar1=amix[l][:, c, j:j + 1], scalar2=mod[l][:, c, j:j + 1],
                                                              op0=ALU.mult, op1=ALU.add), reads=[sqb, amix[l], mod[l]], writes=[hTb])

                    if cfg.get('cut') == 3 and it == cfg.get('cutit', 1):
                        s.finish(); s.dead = True
                    for c in range(5):
                        isq = c < 4
                        col = CQ + c * 128 if isq else CK
                        rcol = CRQ + c * 128 if isq else CRK
                        wi = 0 if isq else 2
                        pq, prq, pm = ps[1 + (c % 2) * 3], ps[2 + (c % 2) * 3], ps[3 + (c % 2) * 3]
                        proj(pq, col, 128, n)
                        if cfg.get('cut') == 11 and it == cfg.get('cutit', 1):
                            s.finish(); s.dead = True
                        proj(prq, rcol, 128, n)
                        if cfg.get('cut') == 10 and it == cfg.get('cutit', 1):
                            s.finish(); s.dead = True
                        s.op("act", lambda e: e.activation(out=sqq[:, 0:n], in_=pq[:, 0:n], func=AF.Square), reads=[pq], writes=[sqq])
                        if cfg.get('cut') == 12 and it == cfg.get('cutit', 1):
                            s.finish(); s.dead = True
                        if cfg.get('exp') == 1:
                            s.op("dve", lambda e: e.memset(rq[:, 0:n], 1.0), writes=[rq])
                        else:
                            s.op("pe", lambda e: e.matmul(pm[:, 0:n], lhsT=bd64[:], rhs=sqq[:, 0:n], start=True, stop=True), reads=[bd64, sqq], writes=[pm])
                            rsqrt_("act", rq[:, 0:n], pm[:, 0:n], [pm], [rq], rq[:, 0:n])
                        s.op("dve", lambda e: e.scalar_tensor_tensor(out=t1[:, 0:n], in0=pq[:, 0:n], scalar=qkwt[:, wi:wi + 1], in1=cs[:, 0, 0:n], op0=ALU.mult, op1=ALU.mult),
                             reads=[pq, qkwt, cs], writes=[t1])
                        if cfg.get('cut') == 13 and it == cfg.get('cutit', 1):
                            s.finish(); s.dead = True
                        s.op("dve", lambda e: e.scalar_tensor_tensor(out=t2[:, 0:n], in0=prq[:, 0:n], scalar=qkwt[:, wi + 1:wi + 2], in1=cs[:, 1, 0:n], op0=ALU.mult, op1=ALU.mult),
                             reads=[prq, qkwt, cs], writes=[t2])
                        if cfg.get('cut') == 14 and it == cfg.get('cutit', 1):
                            s.finish(); s.dead = True
                        s.op(PL, lambda e: e.tensor_tensor(out=t1[:, 0:n], in0=t1[:, 0:n], in1=t2[:, 0:n], op=ALU.add), reads=[t1, t2], writes=[t1])
                        dstb = qTb[:, c, t0:t0 + n] if isq else kTb[:, t0:t0 + n]
                        s.op(PL, lambda e: e.tensor_tensor(out=dstb, in0=t1[:, 0:n], in1=rq[:, 0:n], op=ALU.mult), reads=[t1, rq], writes=[qTb if isq else kTb])

                    if cfg.get('cut') == 4 and it == cfg.get('cutit', 1):
                        s.finish(); s.dead = True
                    for cc in range(12):
                        pb = ps[1 + cc % 6]
                        dst_ = dstl[0]
                        proj(pb, CDQKV + cc * 128, 128, n)
                        if cc % 2 == 0:
                            s.op("act", lambda e: e.copy(out=dst_[:, cc % 4, 0:n], in_=pb[:, 0:n]), reads=[pb], writes=[dst_])
                        else:
                            s.op("dve", lambda e: e.tensor_copy(out=dst_[:, cc % 4, 0:n], in_=pb[:, 0:n]), reads=[pb], writes=[dst_])
                        if cc % 4 == 3:
                            c4 = cc // 4
                            s.dma(dq_raw[b, c4 * 512:(c4 + 1) * 512, t0:t0 + n].rearrange("(c p) t -> p c t", p=128), dst_[:, :, 0:n], reads=[dst_], writes=[("dq_raw", b)])

                    if cfg.get('cut') == 5 and it == cfg.get('cutit', 1):
                        s.finish(); s.dead = True
                    pv = ps[7]
                    for sub in range(nsub):
                        for kc in range(8):
                            s.op("pe", lambda e: e.matmul(pv[:, sub * 128:(sub + 1) * 128], lhsT=hTb[:, kc, sub * 128:(sub + 1) * 128], rhs=winb[:, kc, CV:CV + 128],
                                                         start=(kc == 0), stop=(kc == 7)), reads=[hTb, winb], writes=[pv])
                    s.op("act", lambda e: e.copy(out=Vb[:, t0 // 128:t0 // 128 + nsub, 0:130].rearrange("p s (k d) -> p s k d", k=2)[:, :, :, 0:64],
                                                 in_=pv[:, 0:n].rearrange("p (s k d) -> p s k d", k=2, d=64)), reads=[pv], writes=[Vb])

                    if cfg.get('cut') == 6 and it == cfg.get('cutit', 1):
                        s.finish(); s.dead = True
                    for sub in range(nsub):
                        pg = ps[1 + sub % 4]
                        for kc in range(8):
                            s.op("pe", lambda e: e.matmul(pg[:, :], lhsT=hTb[:, kc, sub * 128:(sub + 1) * 128], rhs=winb[:, kc, CGATE:CGATE + 512],
                                                         start=(kc == 0), stop=(kc == 7)), reads=[hTb, winb], writes=[pg])
                        s.op("act", lambda e: e.activation(out=ge[:], in_=pg[:], func=AF.Exp, scale=-1.0), reads=[pg], writes=[ge])
                        s.op("act", lambda e: e.activation(out=ge[:], in_=ge[:], func=AF.Ln, bias=1.0), reads=[ge], writes=[ge])
                        s.op("act", lambda e: e.activation(out=ge[:], in_=ge[:], func=AF.Exp, scale=-1.0), reads=[ge], writes=[ge])
                        gst = gstl[0]
                        s.op("dve", lambda e: e.tensor_tensor(out=gst[:], in0=pg[:], in1=ge[:], op=ALU.mult), reads=[pg, ge], writes=[gst])
                        s.dma(gate_s[b, t0 + sub * 128:t0 + (sub + 1) * 128, :], gst[:], reads=[gst], writes=[("gate_s", b, t0)])

                    if cfg.get('cut') == 7 and it == cfg.get('cutit', 1):
                        s.finish(); s.dead = True
                    pba = ps[5]
                    for sub in range(nsub):
                        for kc in range(8):
                            s.op("pe", lambda e: e.matmul(pba[:, sub * 32:(sub + 1) * 32], lhsT=hTb[:, kc, sub * 128:(sub + 1) * 128], rhs=winb[:, kc, CBA:CBA + 32],
                                                         start=(kc == 0), stop=(kc == 7)), reads=[hTb, winb], writes=[pba])
                    pba3 = pba[:, 0:nsub * 32].rearrange("p (s c) -> p s c", c=32)
                    s.op("act", lambda e: e.activation(out=gb1[:, 0:nsub, :], in_=pba3[:, :, 0:16], func=AF.Exp, scale=-1.0), reads=[pba], writes=[gb1])
                    s.op("dve", lambda e: e.tensor_scalar(out=gb1[:, 0:nsub, :], in0=gb1[:, 0:nsub, :], scalar1=1.0, scalar2=None, op0=ALU.add), reads=[gb1], writes=[gb1])
                    s.op("dve", lambda e: e.reciprocal(out=gbt[:, 0:nsub, 0:16], in_=gb1[:, 0:nsub, :]), reads=[gb1], writes=[gbt])
                    s.op("dve", lambda e: e.tensor_tensor(out=gb2[:, 0:nsub, :], in0=pba3[:, :, 16:32], in1=dtb[:].unsqueeze(1).broadcast_to([128, nsub, 16]), op=ALU.add),
                         reads=[pba, dtb], writes=[gb2])
                    s.op("act", lambda e: e.activation(out=gb2[:, 0:nsub, :], in_=gb2[:, 0:nsub, :], func=AF.Exp), reads=[gb2], writes=[gb2])
                    s.op("act", lambda e: e.activation(out=gb2[:, 0:nsub, :], in_=gb2[:, 0:nsub, :], func=AF.Ln, bias=1.0), reads=[gb2], writes=[gb2])
                    s.op("dve", lambda e: e.tensor_tensor(out=gbt[:, 0:nsub, 16:32], in0=gb2[:, 0:nsub, :], in1=alog[:].unsqueeze(1).broadcast_to([128, nsub, 16]), op=ALU.mult),
                         reads=[gb2, alog, gbt], writes=[gbt])
                    s.dma(gb[b, t0:t0 + n, :].rearrange("(s p) c -> p s c", p=128), gbt[:, 0:nsub, :], reads=[gbt], writes=[("gb", b)])

                    if cfg.get('cut') == 8 and it == cfg.get('cutit', 1):
                        s.finish(); s.dead = True

                if "t_qT" in tapd:
                    if cfg.get('cut') == 9:
                        s.finish(); s.dead = True
                    s.dma(tapd["t_qT"][b], qTb[:], reads=[qTb])
                    s.dma(tapd["t_kT"][b], kTb[:], reads=[kTb])
                    s.dma(tapd["t_V"][b], Vb[:], reads=[Vb])
                if stop == "A":
                    continue
                aoi = 0
                ui = 0
                for kv in range(2):
                    pl = slice(kv * 64, (kv + 1) * 64)
                    for (t0, n) in tiles:
                        kts = list(range(L // 128)) if t0 < L else list(range(NT128))
                        for gp in range(2):
                            accb = [ps[(ui % 2) * 2], ps[(ui % 2) * 2 + 1]]
                            qz = qzl[kv][ui % 2]
                            ui += 1
                            LA = 2
                            s.op("dve", lambda e: e.tensor_copy(out=qz[pl, :].rearrange("p (two c) -> p two c", two=2)[:, :, 0:n], in_=qTb[pl, gp * 2:gp * 2 + 2, t0:t0 + n]),
                                 reads=[qTb], writes=[qz])

                            def emit_s(si):
                                kt = kts[si]
                                big = psS[si % 2]
                                halves = [ps[4 + (si % 2) * 2], ps[5 + (si % 2) * 2]]
                                for hh in range(2):
                                    g = gp * 2 + hh
                                    s.op("pe", lambda e: e.matmul(halves[hh][:, 0:n], lhsT=kTb[:, kt * 128:(kt + 1) * 128], rhs=qz[:, hh * 512:hh * 512 + n], start=True, stop=True),
                                         reads=[kTb, qz], writes=[halves[hh]])
                                pt_ = ptb[si % 3]
                                s.op("act", lambda e: e.activation(out=pt_[:].rearrange("p (two c) -> p two c", two=2)[:, :, 0:n],
                                                                   in_=big.t[:].rearrange("p (two c) -> p two c", two=2)[:, :, 0:n], func=AF.Exp, bias=nshift[:, 0:1], scale=0.125),
                                     reads=[halves[0], halves[1], nshift], writes=[pt_])

                            def emit_pv(si):
                                kt = kts[si]
                                pt_ = ptb[si % 3]
                                for hh in range(2):
                                    s.op("pe", lambda e: e.matmul(accb[hh][:, 0:n], lhsT=Vb[:, kt, kv * 65:kv * 65 + 128], rhs=pt_[:, hh * 512:hh * 512 + n],
                                                                 start=(si == 0), stop=(si == len(kts) - 1)), reads=[Vb, pt_], writes=[accb[hh]])

                            for si in range(len(kts) + LA):
                                if si < len(kts):
                                    emit_s(si)
                                if si - LA >= 0:
                                    emit_pv(si - LA)
                            for hh in range(2):
                                head = kv * 4 + gp * 2 + hh
                                pa_ = accb[hh]
                                a_o = ao[aoi % 2]
                                au = aul[aoi % 2]
                                rrow = rrowl[aoi % 2]
                                aoi += 1
                                s.op("dve", lambda e: e.reciprocal(out=rrow[64:65, 0:n], in_=pa_[64:65, 0:n]), reads=[pa_], writes=[rrow])
                                s.op("dve", lambda e: e.tensor_copy(out=au[:, 0:n], in_=pa_[0:64, 0:n]), reads=[pa_], writes=[au])
                                s.op("pe", lambda e: e.matmul(pa_[0:64, 0:n], lhsT=ones[64:65, 0:64], rhs=rrow[64:65, 0:n], start=True, stop=True), reads=[ones, rrow], writes=[pa_])
                                s.op("dve", lambda e: e.tensor_tensor(out=a_o[:, 0:n], in0=pa_[0:64, 0:n], in1=au[:, 0:n], op=ALU.mult), reads=[pa_, au], writes=[a_o])
                                s.dma(attnT[b, head, :, t0:t0 + n], a_o[:, 0:n], reads=[a_o], writes=[("attnT", b)])
            s.barrier()
        if stop in ("A", "attn"):
            s.finish(); s.close(); return nc

        with contextlib.ExitStack() as ph:
            cw = s.sbuf("cw", [128, 12, 5], stack=ph)
            s.dma(cw[:], conv_wT[l, :, :, :], writes=[cw])
            NR = 4
            rwb = [s.sbuf("rw%d" % i, [128, 516], stack=ph) for i in range(NR)]
            accl = [s.sbuf("cacc%d" % i, [128, 512], stack=ph) for i in range(NR)]
            cel = [s.sbuf("ce%d" % i, [128, 512], stack=ph) for i in range(NR)]
            cyl = [s.sbuf("cy%d" % i, [128, 512], stack=ph) for i in range(NR)]
            csql = [s.sbuf("csq%d" % i, [128, 512], stack=ph) for i in range(NR)]
            crnl = [s.sbuf("crn%d" % i, [128, 512], stack=ph) for i in range(NR)]
            cynl = [s.sbuf("cyn%d" % i, [128, 512], stack=ph) for i in range(NR)]
            cynb = [s.sbuf("cynb%d" % i, [128, 512], BF16, stack=ph) for i in range(NR)]
            ctk = [s.sbuf("ctk%d" % i, [128, 4, 128], stack=ph) for i in range(NR)]
            it = 0
            for b in range(NB):
                for cc in range(12):
                    for (t0, n) in tiles:
                        nsub = n // 128
                        r_ = it % NR
                        rw, ynb, tk = rwb[r_], cynb[r_], ctk[r_]
                        acc, ce, cy, csq, crn, cyn = accl[r_], cel[r_], cyl[r_], csql[r_], crnl[r_], cynl[r_]
                        it += 1
                        lo = t0 if t0 in (0, L) else t0 - 2
                        hi = t0 + n if (t0 + n) in (L, T) else t0 + n + 2
                        s.op("dve", lambda e: e.memset(rw[:, 0:2], 0.0), writes=[rw])
                        s.op("dve", lambda e: e.memset(rw[:, 2 + n:4 + n], 0.0), writes=[rw])
                        s.dma(rw[:, 2 - (t0 - lo):2 + n + (hi - t0 - n)], dq_raw[b, cc * 128:(cc + 1) * 128, lo:hi], reads=[("dq_raw", b)], writes=[rw])
                        s.op("act", lambda e: e.activation(out=acc[:, 0:n], in_=rw[:, 0:n], func=AF.Copy, scale=cw[:, cc, 0:1]), reads=[rw, cw], writes=[acc])
                        for jj in range(1, 5):
                            s.op("dve", lambda e: e.scalar_tensor_tensor(out=acc[:, 0:n], in0=rw[:, jj:jj + n], scalar=cw[:, cc, jj:jj + 1], in1=acc[:, 0:n],
                                                                         op0=ALU.mult, op1=ALU.add), reads=[rw, cw, acc], writes=[acc])
                        s.op("act", lambda e: e.activation(out=ce[:, 0:n], in_=acc[:, 0:n], func=AF.Exp, scale=-1.0), reads=[acc], writes=[ce])
                        s.op("act", lambda e: e.activation(out=ce[:, 0:n], in_=ce[:, 0:n], func=AF.Ln, bias=1.0), reads=[ce], writes=[ce])
                        s.op("act", lambda e: e.activation(out=ce[:, 0:n], in_=ce[:, 0:n], func=AF.Exp, scale=-1.0), reads=[ce], writes=[ce])
                        s.op("dve", lambda e: e.tensor_tensor(out=cy[:, 0:n], in0=acc[:, 0:n], in1=ce[:, 0:n], op=ALU.mult), reads=[acc, ce], writes=[cy])
                        src_tm = cy
                        if cc < 8:
                            pm = ps[it % 4]
                            s.op("act", lambda e: e.activation(out=csq[:, 0:n], in_=cy[:, 0:n], func=AF.Square), reads=[cy], writes=[csq])
                            s.op("pe", lambda e: e.matmul(pm[:, 0:n], lhsT=bd1[:], rhs=csq[:, 0:n], start=True, stop=True), reads=[bd1, csq], writes=[pm])
                            rsqrt_("act", crn[:, 0:n], pm[:, 0:n], [pm], [crn], crn[:, 0:n])
                            sc_ = 0.125 if cc < 4 else 1.0
                            s.op("dve", lambda e: e.scalar_tensor_tensor(out=cyn[:, 0:n], in0=cy[:, 0:n], scalar=sc_, in1=crn[:, 0:n], op0=ALU.mult, op1=ALU.mult),
                                 reads=[cy, crn], writes=[cyn])
                            s.op("dve", lambda e: e.tensor_copy(out=ynb[:, 0:n], in_=cyn[:, 0:n]), reads=[cyn], writes=[ynb])
                            dstT = qnT if cc < 4 else knT
                            s.dma(dstT[b, (cc % 4) * 128:(cc % 4 + 1) * 128, t0:t0 + n], ynb[:, 0:n], reads=[ynb], writes=[("qknT", b)])
                            src_tm = cyn
                        if cc >= 4:
                            pt_ = ps[4 + it % 4]
                            for sub in range(nsub):
                                s.op("pe", lambda e: e.transpose(pt_[:, sub * 128:(sub + 1) * 128], src_tm[:, sub * 128:(sub + 1) * 128], ident[:]), reads=[src_tm, ident], writes=[pt_])
                            s.op("act", lambda e: e.copy(out=tk[:, 0:nsub, :], in_=pt_[:, 0:n].rearrange("p (s c) -> p s c", c=128)), reads=[pt_], writes=[tk])
                            dtk = k_tok if cc < 8 else v_tok
                            s.dma(dtk[b, t0:t0 + n, (cc % 4) * 128:(cc % 4 + 1) * 128].rearrange("(s p) c -> p s c", p=128), tk[:, 0:nsub, :], reads=[tk], writes=[("kvtok", b)])
            s.barrier()
        if stop == "B":
            s.finish(); s.close(); return nc

        with contextlib.ExitStack() as ph:
            mk = [s.sbuf("mk%d" % i, [128, 128], stack=ph) for i in range(6)]
            for i in range(6):
                s.dma(mk[i][:], c_masks[i, :, :], writes=[mk[i]])
            notI = s.sbuf("notI", [128, 128], stack=ph)
            nones = s.sbuf("nones", [128, 128], stack=ph)
            s.op("dve", lambda e: e.tensor_scalar(out=notI[:], in0=ident[:], scalar1=-1.0, scalar2=1.0, op0=ALU.mult, op1=ALU.add), reads=[ident], writes=[notI])
            s.op("dve", lambda e: e.memset(nones[:], -1.0), writes=[nones])
            NC8 = NCH * 8
            gbt = s.sbuf("dgbt", [128, NCH, 32], stack=ph)
            gc = s.sbuf("dgc", [128, NC8], stack=ph)
            glb = s.sbuf("dglb", [128, NC8], stack=ph)
            egc = s.sbuf("degc", [128, NC8], stack=ph)
            egl = s.sbuf("degl", [128, NC8], stack=ph)
            eglmg = s.sbuf("deglmg", [128, NC8], stack=ph)
            nbt = s.sbuf("dnbt", [128, NC8], stack=ph)
            bet = s.sbuf("dbet", [128, NC8], stack=ph)
            begc = s.sbuf("dbegc", [128, NC8], stack=ph)
            Sf = s.sbuf("Sf", [64, 8, 64], stack=ph)
            Sb = s.sbuf("Sb", [64, 8, 64], BF16, stack=ph)
            qcb = [s.sbuf("qc%d" % i, [64, 8, 128], BF16, stack=ph) for i in range(2)]
            kcb = [s.sbuf("kc%d" % i, [64, 8, 128], BF16, stack=ph) for i in range(2)]
            ktb = [s.sbuf("kt%d" % i, [128, 512], stack=ph) for i in range(2)]
            vtb = [s.sbuf("vt%d" % i, [128, 512], stack=ph) for i in range(2)]
            vb_ = s.sbuf("vb", [128, 512], BF16, stack=ph)
            kbg = s.sbuf("kbg", [128, 512], BF16, stack=ph)
            kdec = s.sbuf("kdec", [128, 512], BF16, stack=ph)
            dgb = [s.sbuf("dg%d" % i, [128, 128], stack=ph) for i in range(4)]
            Dtl = [s.sbuf("Dt%d" % i, [128, 512], stack=ph) for i in range(2)]
            Dstl = [s.sbuf("Dst%d" % i, [128, 512], stack=ph) for i in range(2)]
            intral = [s.sbuf("intra%d" % i, [128, 512], BF16, stack=ph) for i in range(2)]
            intraT = s.sbuf("intraT", [128, 1024], BF16, stack=ph)
            Nkl = [[s.sbuf("Nk%d_%d" % (g_, i), [128, 512], stack=ph) for i in range(3)] for g_ in range(2)]
            Pml = [s.sbuf("Pm%d" % g_, [128, 512], stack=ph) for g_ in range(2)]
            Rtl = [s.sbuf("Rt%d" % g_, [128, 512], stack=ph) for g_ in range(2)]
            Qml = [s.sbuf("Qm%d" % g_, [128, 512], stack=ph) for g_ in range(2)]
            NkTl = [[s.sbuf("NkT%d_%d" % (g_, i), [128, 512], stack=ph) for i in range(2)] for g_ in range(2)]
            PTl = [[s.sbuf("PT%d_%d" % (g_, i), [128, 512], stack=ph) for i in range(2)] for g_ in range(2)]
            TTbl = [s.sbuf("TTb%d" % g_, [128, 512], BF16, stack=ph) for g_ in range(2)]
            wTs = s.sbuf("wTs", [64, 8, 128], BF16, stack=ph)
            us = s.sbuf("us", [128, 512], stack=ph)
            vnew = s.sbuf("vnew", [128, 512], BF16, stack=ph)
            tt = s.sbuf("dtt", [128, 512], stack=ph)
            otb = [s.sbuf("ot%d" % i, [128, 512], stack=ph) for i in range(2)]
            ncx = L // CH
            it = 0
            for b in range(NB):
                for dr in range(2):
                    tri, sel, bigm = mk[dr], mk[2 + dr], mk[4 + dr]
                    s.dma(gbt[:], gb[b, :, :].rearrange("(n p) c -> p n c", p=128), reads=[("gb", b)], writes=[gbt])
                    gd = gbt[:, :, 16 + dr * 8:24 + dr * 8]
                    bdv = gbt[:, :, dr * 8:dr * 8 + 8]
                    g3 = lambda t_: t_[:].rearrange("p (n h) -> p n h", h=8)
                    s.op("pe", lambda e: e.matmul(ps[0][:, 0:NC8].rearrange("p (n h) -> p n h", h=8), lhsT=tri[:], rhs=gd, start=True, stop=True), reads=[tri, gbt], writes=[ps[0]])
                    s.op("act", lambda e: e.copy(out=gc[:], in_=ps[0][:, 0:NC8]), reads=[ps[0]], writes=[gc])
                    s.op("pe", lambda e: e.matmul(ps[1][:, 0:NC8], lhsT=sel[:], rhs=gc[:], start=True, stop=True), reads=[sel, gc], writes=[ps[1]])
                    s.op("dve", lambda e: e.tensor_copy(out=glb[:], in_=ps[1][:, 0:NC8]), reads=[ps[1]], writes=[glb])
                    s.op("act", lambda e: e.activation(out=egc[:], in_=gc[:], func=AF.Exp), reads=[gc], writes=[egc])
                    s.op("act", lambda e: e.activation(out=egl[:], in_=glb[:], func=AF.Exp), reads=[glb], writes=[egl])
                    s.op("dve", lambda e: e.tensor_tensor(out=eglmg[:], in0=glb[:], in1=gc[:], op=ALU.subtract), reads=[glb, gc], writes=[eglmg])
                    s.op("act", lambda e: e.activation(out=eglmg[:], in_=eglmg[:], func=AF.Exp), reads=[eglmg], writes=[eglmg])
                    s.op("dve", lambda e: e.tensor_copy(out=g3(bet), in_=bdv), reads=[gbt], writes=[bet])
                    s.op("dve", lambda e: e.tensor_scalar(out=nbt[:], in0=bet[:], scalar1=-1.0, scalar2=None, op0=ALU.mult), reads=[bet], writes=[nbt])
                    s.op("dve", lambda e: e.tensor_tensor(out=begc[:], in0=bet[:], in1=egc[:], op=ALU.mult), reads=[bet, egc], writes=[begc])
                    tap("gc", gc, gc[:], [128, NC8]); tap("glb", glb, glb[:], [128, NC8]); tap("bet", bet, bet[:], [128, NC8])
                    s.op("dve", lambda e: e.memset(Sf[:], 0.0), writes=[Sf])
                    s.op("dve", lambda e: e.memset(Sb[:], 0.0), writes=[Sb])
                    order = list(range(NCH)) if dr == 0 else (list(range(ncx - 1, -1, -1)) + list(range(NCH - 1, ncx - 1, -1)))
                    for n_ in order:
                        c0 = n_ * 8
                        tk0 = n_ * CH
                        qc, kc, kt, vt, ot = qcb[it % 2], kcb[it % 2], ktb[it % 2], vtb[it % 2], otb[it % 2]
                        it += 1
                        s.dma(qc[:], qnT[b, :, tk0:tk0 + CH].rearrange("(h d) t -> d h t", d=64), reads=[("qknT", b)], writes=[qc])
                        s.dma(kc[:], knT[b, :, tk0:tk0 + CH].rearrange("(h d) t -> d h t", d=64), reads=[("qknT", b)], writes=[kc])
                        s.dma(kt[:], k_tok[b, tk0:tk0 + CH, :], reads=[("kvtok", b)], writes=[kt])
                        s.dma(vt[:], v_tok[b, tk0:tk0 + CH, :], reads=[("kvtok", b)], writes=[vt])
                        bc8 = lambda t_: t_[:, c0:c0 + 8].unsqueeze(2).broadcast_to([128, 8, 64])
                        v3 = lambda t_: t_[:].rearrange("p (h e) -> p h e", e=64)
                        s.op("dve", lambda e: e.tensor_tensor(out=v3(vb_), in0=v3(vt), in1=bc8(bet), op=ALU.mult), reads=[vt, bet], writes=[vb_])
                        s.op("dve", lambda e: e.tensor_tensor(out=v3(kbg), in0=v3(kt), in1=bc8(begc), op=ALU.mult), reads=[kt, begc], writes=[kbg])
                        s.op("dve", lambda e: e.tensor_tensor(out=v3(kdec), in0=v3(kt), in1=bc8(eglmg), op=ALU.mult), reads=[kt, eglmg], writes=[kdec])
                        d3 = lambda t_: t_[:].rearrange("p (h j) -> p h j", j=128)
                        pD, pE = ps[6], ps[7]
                        pEb = pE[:].bitcast(BF16)
                        bank = [(ps[0], ps[1], ps[2]), (ps[3], ps[4], ps[5])]
                        cur = [0, 0]
                        curT = [0, 0]
                        pcur = [0, 0]

                        def phase1(hg):
                            pA, pB_, pC = bank[hg]
                            Dt, Dst, intra = Dtl[hg], Dstl[hg], intral[hg]
                            Nk, NkT, PT = Nkl[hg], NkTl[hg], PTl[hg]
                            for hh in range(4):
                                h = hg * 4 + hh
                                s.op("pe", lambda e: e.matmul(pA[:, hh * 128:(hh + 1) * 128], lhsT=kc[:, h, :], rhs=kc[:, h, :], start=True, stop=True), reads=[kc], writes=[pA])
                            for hh in range(4):
                                h = hg * 4 + hh
                                s.op("pe", lambda e: e.matmul(pB_[:, hh * 128:(hh + 1) * 128], lhsT=qc[:, h, :], rhs=kc[:, h, :], start=True, stop=True), reads=[qc, kc], writes=[pB_])
                            for hh in range(4):
                                h = hg * 4 + hh
                                dg = dgb[hg * 2 + hh % 2]
                                s.op("dve", lambda e: e.tensor_scalar(out=dg[:], in0=ident[:], scalar1=gc[:, c0 + h:c0 + h + 1], scalar2=None, op0=ALU.mult), reads=[ident, gc], writes=[dg])
                                o_ = pC[:, hh * 128:(hh + 1) * 128]
                                s.op("pe", lambda e: e.matmul(o_, lhsT=ones[:], rhs=dg[:], start=True, stop=False), reads=[ones, dg], writes=[pC])
                                s.op("pe", lambda e: e.matmul(o_, lhsT=dg[:], rhs=nones[:], start=False, stop=False), reads=[dg, nones], writes=[pC])
                                s.op("pe", lambda e: e.matmul(o_, lhsT=ident[:], rhs=bigm[:], start=False, stop=True), reads=[ident, bigm], writes=[pC])
                            s.op("act", lambda e: e.activation(out=Dt[:], in_=pC[:], func=AF.Exp, scale=-1.0), reads=[pC], writes=[Dt])
                            s.op("dve", lambda e: e.tensor_tensor(out=intra[:], in0=pB_[:], in1=Dt[:], op=ALU.mult), reads=[pB_, Dt], writes=[intra])
                            s.op("dve", lambda e: e.tensor_tensor(out=d3(Dst), in0=d3(Dt), in1=notI[:].unsqueeze(1).broadcast_to([128, 4, 128]), op=ALU.mult), reads=[Dt, notI], writes=[Dst])
                            s.op("dve", lambda e: e.tensor_tensor(out=d3(Dst), in0=d3(Dst), in1=nbt[:, c0 + hg * 4:c0 + hg * 4 + 4].unsqueeze(2).broadcast_to([128, 4, 128]), op=ALU.mult),
                                 reads=[Dst, nbt], writes=[Dst])
                            s.op("dve", lambda e: e.tensor_tensor(out=Nk[2][:], in0=pA[:], in1=Dst[:], op=ALU.mult), reads=[pA, Dst], writes=[Nk[2]])
                            for hh in range(4):
                                s.op("pe", lambda e: e.transpose(pD[:, hh * 128:(hh + 1) * 128], Nk[2][:, hh * 128:(hh + 1) * 128], ident[:]), reads=[Nk[2], ident], writes=[pD])
                            for hh in range(4):
                                s.op("pe", lambda e: e.transpose(pEb[:, hh * 128:(hh + 1) * 128], intra[:, hh * 128:(hh + 1) * 128], identb[:]), reads=[intra, identb], writes=[pE])
                            s.op("act", lambda e: e.copy(out=NkT[0][:], in_=pD[:]), reads=[pD], writes=[NkT[0]])
                            s.op("dve", lambda e: e.tensor_tensor(out=d3(PT[0]), in0=pD[:].rearrange("p (h j) -> p h j", j=128),
                                                                  in1=ident[:].unsqueeze(1).broadcast_to([128, 4, 128]), op=ALU.add), reads=[pD, ident], writes=[PT[0]])
                            s.op("act", lambda e: e.copy(out=intraT[:, hg * 512:(hg + 1) * 512], in_=pEb[:, 0:512]), reads=[pE], writes=[intraT])
                            cur[hg] = 2
                            curT[hg] = 0
                            pcur[hg] = 0

                        def nstep(hg, k_):
                            pA, pB_, pC = bank[hg]
                            Nk, NkT, PT = Nkl[hg], NkTl[hg], PTl[hg]
                            c_, p_ = cur[hg], pcur[hg]
                            nxt = 0 if c_ == 2 else 1 - c_
                            ct_ = curT[hg]
                            for hh in range(4):
                                sl = slice(hh * 128, (hh + 1) * 128)
                                s.op("pe", lambda e: e.matmul(pA[:, sl], lhsT=NkT[ct_][:, sl], rhs=Nk[c_][:, sl], start=True, stop=True), reads=[NkT[ct_], Nk[c_]], writes=[pA])
                            if k_ < 5:
                                for hh in range(4):
                                    sl = slice(hh * 128, (hh + 1) * 128)
                                    s.op("pe", lambda e: e.matmul(pB_[:, sl], lhsT=Nk[c_][:, sl], rhs=NkT[ct_][:, sl], start=True, stop=True), reads=[NkT[ct_], Nk[c_]], writes=[pB_])
                            s.op("act", lambda e: e.copy(out=Nk[nxt][:], in_=pA[:]), reads=[pA], writes=[Nk[nxt]])
                            if k_ < 5:
                                ntx = 1 - ct_
                                s.op("act" if hg else "dve", (lambda e: e.copy(out=NkT[ntx][:], in_=pB_[:])) if hg else (lambda e: e.tensor_copy(out=NkT[ntx][:], in_=pB_[:])),
                                     reads=[pB_], writes=[NkT[ntx]])
                            for hh in range(4):
                                sl = slice(hh * 128, (hh + 1) * 128)
                                s.op("pe", lambda e: e.matmul(pC[:, sl], lhsT=Nk[nxt][:, sl], rhs=PT[p_][:, sl], start=True, stop=True), reads=[Nk[nxt], PT[p_]], writes=[pC])
                            s.op("dve", lambda e: e.tensor_tensor(out=PT[1 - p_][:], in0=pC[:], in1=PT[p_][:], op=ALU.add), reads=[pC, PT[p_]], writes=[PT[1 - p_]])
                            pcur[hg] = 1 - p_
                            cur[hg] = nxt
                            if k_ < 5:
                                curT[hg] = 1 - ct_

                        def tail(hg):
                            PT = PTl[hg]
                            TT = TTbl[hg]
                            pA, pB_, pC = bank[hg]
                            N0s = Nkl[hg][2]
                            Pm, Rt, Qm = Pml[hg], Rtl[hg], Qml[hg]
                            nsteps = cfg.get("newton", 1)
                            for ns_ in range(nsteps):
                                X = PT[pcur[hg]]
                                Xn = PT[1 - pcur[hg]]
                                for hh in range(4):
                                    sl = slice(hh * 128, (hh + 1) * 128)
                                    s.op("pe", lambda e: e.transpose(pA[:, sl], X[:, sl], ident[:]), reads=[X, ident], writes=[pA])
                                    s.op("pe", lambda e: e.matmul(pB_[:, sl], lhsT=N0s[:, sl], rhs=X[:, sl], start=True, stop=True), reads=[N0s, X], writes=[pB_])
                                s.op("act", lambda e: e.copy(out=Pm[:], in_=pA[:]), reads=[pA], writes=[Pm])
                                s.op("dve", lambda e: e.tensor_tensor(out=d3(Qm), in0=ident[:].unsqueeze(1).broadcast_to([128, 4, 128]), in1=d3(X), op=ALU.subtract), reads=[ident, X], writes=[Qm])
                                s.op("dve", lambda e: e.tensor_tensor(out=Rt[:], in0=pB_[:], in1=Qm[:], op=ALU.add), reads=[pB_, Qm], writes=[Rt])
                                for hh in range(4):
                                    sl = slice(hh * 128, (hh + 1) * 128)
                                    s.op("pe", lambda e: e.matmul(pC[:, sl], lhsT=Pm[:, sl], rhs=Rt[:, sl], start=True, stop=True), reads=[Pm, Rt], writes=[pC])
                                s.op("dve", lambda e: e.tensor_tensor(out=Xn[:], in0=pC[:], in1=X[:], op=ALU.add), reads=[pC, X], writes=[Xn])
                                pcur[hg] = 1 - pcur[hg]
                            s.op("act", lambda e: e.copy(out=TT[:], in_=PT[pcur[hg]][:]), reads=[PT[pcur[hg]]], writes=[TT])
                            pU = ps[6]
                            pW = bank[hg][2]
                            for hh in range(4):
                                h = hg * 4 + hh
                                sl = slice(hh * 128, (hh + 1) * 128)
                                s.op("pe", lambda e: e.matmul(pU[:, h * 64:(h + 1) * 64], lhsT=TT[:, sl], rhs=vb_[:, h * 64:(h + 1) * 64], start=True, stop=True), reads=[TT, vb_], writes=[pU])
                                s.op("pe", lambda e: e.matmul(pW[0:64, sl], lhsT=kbg[:, h * 64:(h + 1) * 64], rhs=TT[:, sl], start=True, stop=True), reads=[TT, kbg], writes=[pW])
                            s.op("act", lambda e: e.copy(out=wTs[:, hg * 4:(hg + 1) * 4, :], in_=pW[0:64, :].rearrange("p (h i) -> p h i", i=128)), reads=[pW], writes=[wTs])

                        for hg in range(2):
                            phase1(hg)
                        for k_ in range(6):
                            for hg in range(2):
                                nstep(hg, k_)
                        for hg in range(2):
                            tail(hg)
                        s.op("dve", lambda e: e.tensor_copy(out=us[:], in_=ps[6][:]), reads=[ps[6]], writes=[us])
                        tap("us", us, us[:], [128, 512]); tap("wTs", wTs, wTs[:], [64, 8, 128], BF16)
                        for h in range(8):
                            s.op("pe", lambda e: e.matmul(ps[0][:, h * 64:(h + 1) * 64], lhsT=wTs[:, h, :], rhs=Sb[:, h, :], start=True, stop=True), reads=[wTs, Sb], writes=[ps[0]])
                        s.op("dve", lambda e: e.tensor_tensor(out=vnew[:], in0=us[:], in1=ps[0][:], op=ALU.subtract), reads=[us, ps[0]], writes=[vnew])
                        for h in range(8):
                            s.op("pe", lambda e: e.matmul(ps[1][:, h * 64:(h + 1) * 64], lhsT=qc[:, h, :], rhs=Sb[:, h, :], start=True, stop=True), reads=[qc, Sb], writes=[ps[1]])
                        for h in range(8):
                            s.op("pe", lambda e: e.matmul(ps[2][:, h * 64:(h + 1) * 64], lhsT=intraT[:, h * 128:(h + 1) * 128], rhs=vnew[:, h * 64:(h + 1) * 64], start=True, stop=True),
                                 reads=[intraT, vnew], writes=[ps[2]])
                        for h in range(8):
                            s.op("pe", lambda e: e.matmul(ps[3][0:64, h * 64:(h + 1) * 64], lhsT=kdec[:, h * 64:(h + 1) * 64], rhs=vnew[:, h * 64:(h + 1) * 64], start=True, stop=True),
                                 reads=[kdec, vnew], writes=[ps[3]])
                        s.op("dve", lambda e: e.tensor_tensor(out=v3(tt), in0=ps[1][:].rearrange("p (h e) -> p h e", e=64), in1=bc8(egc), op=ALU.mult), reads=[ps[1], egc], writes=[tt])
                        s.op("dve", lambda e: e.tensor_tensor(out=ot[:], in0=tt[:], in1=ps[2][:], op=ALU.add), reads=[tt, ps[2]], writes=[ot])
                        tap("ot", ot, ot[:], [128, 512]); tap("vnew", vnew, vnew[:], [128, 512], BF16)
                        s.dma(o_dir[dr][b, tk0:tk0 + CH, :], ot[:], reads=[ot], writes=[("o_dir", b)])
                        s.op("dve", lambda e: e.tensor_tensor(out=Sf[:], in0=Sf[:], in1=egl[0:64, c0:c0 + 8].unsqueeze(2).broadcast_to([64, 8, 64]), op=ALU.mult), reads=[Sf, egl], writes=[Sf])
                        s.op("dve", lambda e: e.tensor_tensor(out=Sf[:], in0=Sf[:], in1=ps[3][0:64, :].rearrange("p (h e) -> p h e", e=64), op=ALU.add), reads=[Sf, ps[3]], writes=[Sf])
                        s.op("act", lambda e: e.copy(out=Sb[:], in_=Sf[:]), reads=[Sf], writes=[Sb])
            s.barrier()
        if stop == "DN":
            s.finish(); s.close(); return nc

        with contextlib.ExitStack() as ph:
            wo_a = s.sbuf("wo_a", [64, 8, D], BF16, stack=ph)
            wo_d = s.sbuf("wo_d", [128, 4, D], BF16, stack=ph)
            wst = [s.sbuf("wos%d" % i, [128, D], stack=ph) for i in range(2)]
            for i in range(12):
                st = wst[i % 2]
                if i < 8:
                    s.dma(st[0:64, :], w_out[l, i * 64:(i + 1) * 64, :], writes=[st])
                    s.op("act" if i % 2 else "dve", (lambda e: e.copy(out=wo_a[:, i, :], in_=st[0:64, :])) if i % 2 else (lambda e: e.tensor_copy(out=wo_a[:, i, :], in_=st[0:64, :])),
                         reads=[st], writes=[wo_a])
                else:
                    c = i - 8
                    s.dma(st[:, :], w_out[l, 512 + c * 128:512 + (c + 1) * 128, :], writes=[st])
                    s.op("act" if i % 2 else "dve", (lambda e: e.copy(out=wo_d[:, c, :], in_=st[:, :])) if i % 2 else (lambda e: e.tensor_copy(out=wo_d[:, c, :], in_=st[:, :])),
                         reads=[st], writes=[wo_d])
            wr = s.sbuf("wr", [128, 8, 36], stack=ph)
            rbb = s.sbuf("rbb", [128, 36], stack=ph)
            dnw = s.sbuf("dnw", [128, 64], stack=ph)
            s.dma(wr[:], wr_in[l, :, :].rearrange("(kc p) n -> p kc n", p=128), writes=[wr])
            s.dma(rbb[:], rb_in[l:l + 1, :].partition_broadcast(128), writes=[rbb])
            s.dma(dnw[:], dn_norm_w[l:l + 1, :].partition_broadcast(128), writes=[dnw])
            ofb = [s.sbuf("of%d" % i, [128, 4, 512], stack=ph) for i in range(2)]
            obb = [s.sbuf("ob%d" % i, [128, 4, 512], stack=ph) for i in range(2)]
            gsb = [s.sbuf("gs%d" % i, [128, 4, 512], stack=ph) for i in range(2)]
            atb = [s.sbuf("at%d" % i, [64, 8, 512], BF16, stack=ph) for i in range(2)]
            xtb = [s.sbuf("mxt%d" % i, [128, 8, 512], stack=ph) for i in range(2)]
            ss = s.sbuf("mss", [128, 32], stack=ph)
            dnT = s.sbuf("dnT", [128, 4, 512], BF16, stack=ph)
            sqb = s.sbuf("msq", [128, 8, 512], stack=ph)
            rstd = s.sbuf("mrstd", [128, 512], stack=ph)
            h2b = s.sbuf("h2b", [128, 8, 512], BF16, stack=ph)
            lg = s.sbuf("lg", [128, 4, 36], stack=ph)
            gmx = s.sbuf("gmx", [128, 4], stack=ph)
            oh = s.sbuf("oh", [128, 4, 4], stack=ph)
            eg = s.sbuf("eg", [128, 4, 4], stack=ph)
            sg = s.sbuf("sg", [128, 4], stack=ph)
            ml = s.sbuf("ml", [128, 4, 32], stack=ph)
            top8 = s.sbuf("top8", [128, 4, 8], stack=ph)
            selt = s.sbuf("selt", [128, 4, 32], stack=ph)
            ex = s.sbuf("ex", [128, 4, 32], stack=ph)
            se = s.sbuf("se", [128, 4], stack=ph)
            wts = s.sbuf("wts", [32, 512], stack=ph)
            it = 0
            for b in range(NB):
                for (t0, n) in tiles:
                    j = NB if t0 < L else b
                    nsub = n // 128
                    of_, ob_, gs_, at_, xt = ofb[it % 2], obb[it % 2], gsb[it % 2], atb[it % 2], xtb[it % 2]
                    it += 1
                    tm = lambda ap_: ap_.rearrange("(s p) f -> p s f", p=128)
                    s.dma(of_[:, 0:nsub, :], tm(o_dir[0][b, t0:t0 + n, :]), reads=[("o_dir", b)], writes=[of_])
                    s.dma(ob_[:, 0:nsub, :], tm(o_dir[1][b, t0:t0 + n, :]), reads=[("o_dir", b)], writes=[ob_])
                    s.dma(gs_[:, 0:nsub, :], tm(gate_s[b, t0:t0 + n, :]), reads=[("gate_s", b, t0)], writes=[gs_])
                    s.dma(at_[:, :, 0:n], attnT[b, :, :, t0:t0 + n].rearrange("h d t -> d h t"), reads=[("attnT", b)], writes=[at_])
                    s.dma(xt[:, :, 0:n], xT[b, :, t0:t0 + n].rearrange("(c p) t -> p c t", p=128), reads=[("xT", b, t0)], writes=[xt])
                    o4 = lambda t_: t_[:, 0:nsub, :].rearrange("p s (h e) -> p (s h) e", e=64)
                    s.op("dve", lambda e: e.tensor_tensor(out=of_[:, 0:nsub, :], in0=of_[:, 0:nsub, :], in1=ob_[:, 0:nsub, :], op=ALU.add), reads=[of_, ob_], writes=[of_])
                    s.op("act", lambda e: e.activation(out=ob_[:, 0:nsub, :], in_=of_[:, 0:nsub, :], func=AF.Square), reads=[of_], writes=[ob_])
                    s.op("dve", lambda e: e.tensor_reduce(out=ss[:, 0:nsub * 8], in_=o4(ob_), axis=AX.X, op=ALU.add), reads=[ob_], writes=[ss])
                    rsqrt_("act", ss[:, 0:nsub * 8], ss[:, 0:nsub * 8], [ss], [ss], ss[:, 0:nsub * 8], scale=1.0 / 64)
                    s.op("dve", lambda e: e.tensor_tensor(out=o4(of_), in0=o4(of_), in1=ss[:, 0:nsub * 8].unsqueeze(2).broadcast_to([128, nsub * 8, 64]), op=ALU.mult), reads=[of_, ss], writes=[of_])
                    s.op("dve", lambda e: e.tensor_tensor(out=o4(of_), in0=o4(of_), in1=dnw[:].unsqueeze(1).broadcast_to([128, nsub * 8, 64]), op=ALU.mult), reads=[of_, dnw], writes=[of_])
                    s.op("dve", lambda e: e.tensor_tensor(out=of_[:, 0:nsub, :], in0=of_[:, 0:nsub, :], in1=gs_[:, 0:nsub, :], op=ALU.mult), reads=[of_, gs_], writes=[of_])
                    for c in range(4):
                        pT_ = ps[c % 2]
                        for sub in range(nsub):
                            s.op("pe", lambda e: e.transpose(pT_[:, sub * 128:(sub + 1) * 128], of_[:, sub, c * 128:(c + 1) * 128], ident[:]), reads=[of_, ident], writes=[pT_])
                        if c % 2:
                            s.op("act", lambda e: e.copy(out=dnT[:, c, 0:n], in_=pT_[:, 0:n]), reads=[pT_], writes=[dnT])
                        else:
                            s.op("dve", lambda e: e.tensor_copy(out=dnT[:, c, 0:n], in_=pT_[:, 0:n]), reads=[pT_], writes=[dnT])
                    for oc in range(8):
                        py = ps[2 + oc % 4]
                        osl = slice(oc * 128, (oc + 1) * 128)
                        for h in range(8):
                            s.op("pe", lambda e: e.matmul(py[:, 0:n], lhsT=wo_a[0:64, h, osl], rhs=at_[0:64, h, 0:n], start=(h == 0), stop=False), reads=[wo_a, at_], writes=[py])
                        for c in range(4):
                            s.op("pe", lambda e: e.matmul(py[:, 0:n], lhsT=wo_d[:, c, osl], rhs=dnT[:, c, 0:n], start=False, stop=(c == 3)), reads=[wo_d, dnT], writes=[py])
                        s.op("dve", lambda e: e.scalar_tensor_tensor(out=xt[:, oc, 0:n], in0=py[:, 0:n], scalar=mod[l][:, 16 + oc, j:j + 1], in1=xt[:, oc, 0:n], op0=ALU.mult, op1=ALU.add),
                             reads=[py, mod[l], xt], writes=[xt])
                    s.dma(xT[b, :, t0:t0 + n].rearrange("(c p) t -> p c t", p=128), xt[:, :, 0:n], reads=[xt], writes=[("xT", b, t0)])
                    s.op("act", lambda e: e.activation(out=sqb[:, :, 0:n], in_=xt[:, :, 0:n], func=AF.Square), reads=[xt], writes=[sqb])
                    for c in range(8):
                        s.op("pe", lambda e: e.matmul(ps[6][:, 0:n], lhsT=onesm[:], rhs=sqb[:, c, 0:n], start=(c == 0), stop=(c == 7)), reads=[onesm, sqb], writes=[ps[6]])
                    rsqrt_("act", rstd[:, 0:n], ps[6][:, 0:n], [ps[6]], [rstd], rstd[:, 0:n])
                    s.op("dve", lambda e: e.tensor_tensor(out=sqb[:, :, 0:n], in0=xt[:, :, 0:n], in1=rstd[:, 0:n].unsqueeze(1).broadcast_to([128, 8, n]), op=ALU.mult),
                         reads=[xt, rstd], writes=[sqb])
                    for c in range(8):
                        s.op("dve", lambda e: e.tensor_scalar(out=sqb[:, c, 0:n], in0=sqb[:, c, 0:n], scalar1=affn[l][:, c, j:j + 1], scalar2=mod[l][:, 24 + c, j:j + 1],
                                                              op0=ALU.mult, op1=ALU.add), reads=[sqb, affn[l], mod[l]], writes=[sqb])
                    s.op("act", lambda e: e.copy(out=h2b[:, :, 0:n], in_=sqb[:, :, 0:n]), reads=[sqb], writes=[h2b])
                    s.dma(h2T[b, :, t0:t0 + n].rearrange("(c p) t -> p c t", p=128), h2b[:, :, 0:n], reads=[h2b], writes=[("h2T", b)])
                    pl_ = ps[7]
                    for sub in range(nsub):
                        for kc in range(8):
                            s.op("pe", lambda e: e.matmul(pl_[:, sub * 36:(sub + 1) * 36], lhsT=sqb[:, kc, sub * 128:(sub + 1) * 128], rhs=wr[:, kc, :], start=(kc == 0), stop=(kc == 7)),
                                 reads=[sqb, wr], writes=[pl_])
                    L3 = lg[:, 0:nsub, :]
                    s.op("dve", lambda e: e.tensor_tensor(out=L3, in0=pl_[:, 0:nsub * 36].rearrange("p (s c) -> p s c", c=36), in1=rbb[:].unsqueeze(1).broadcast_to([128, nsub, 36]), op=ALU.add),
                         reads=[pl_, rbb], writes=[lg])
                    s.op("dve", lambda e: e.tensor_reduce(out=gmx[:, 0:nsub], in_=lg[:, 0:nsub, 0:4], axis=AX.X, op=ALU.max), reads=[lg], writes=[gmx])
                    gmb = gmx[:, 0:nsub].unsqueeze(2).broadcast_to([128, nsub, 4])
                    s.op("dve", lambda e: e.tensor_tensor(out=oh[:, 0:nsub, :], in0=lg[:, 0:nsub, 0:4], in1=gmb, op=ALU.is_equal), reads=[lg, gmx], writes=[oh])
                    s.op("dve", lambda e: e.tensor_tensor(out=eg[:, 0:nsub, :], in0=lg[:, 0:nsub, 0:4], in1=gmb, op=ALU.subtract), reads=[lg, gmx], writes=[eg])
                    s.op("act", lambda e: e.activation(out=eg[:, 0:nsub, :], in_=eg[:, 0:nsub, :], func=AF.Exp), reads=[eg], writes=[eg])
                    s.op("dve", lambda e: e.tensor_reduce(out=sg[:, 0:nsub], in_=eg[:, 0:nsub, :], axis=AX.X, op=ALU.add), reads=[eg], writes=[sg])
                    s.op("dve", lambda e: e.tensor_scalar(out=oh[:, 0:nsub, :], in0=oh[:, 0:nsub, :], scalar1=1.0e30, scalar2=-1.0e30, op0=ALU.mult, op1=ALU.add), reads=[oh], writes=[oh])
                    s.op("dve", lambda e: e.tensor_tensor(out=ml[:, 0:nsub, :].rearrange("p s (g x) -> p s g x", x=8), in0=lg[:, 0:nsub, 4:36].rearrange("p s (g x) -> p s g x", x=8),
                                                          in1=oh[:, 0:nsub, :].unsqueeze(3).broadcast_to([128, nsub, 4, 8]), op=ALU.add), reads=[lg, oh], writes=[ml])
                    for sub in range(nsub):
                        s.op("dve", lambda e: e.max(out=top8[:, sub, :], in_=ml[:, sub, :]), reads=[ml], writes=[top8])
                    s.op("dve", lambda e: e.tensor_tensor(out=selt[:, 0:nsub, :], in0=ml[:, 0:nsub, :], in1=top8[:, 0:nsub, 1:2].broadcast_to([128, nsub, 32]), op=ALU.is_ge), reads=[ml, top8], writes=[selt])
                    s.op("dve", lambda e: e.tensor_tensor(out=ex[:, 0:nsub, :], in0=ml[:, 0:nsub, :], in1=top8[:, 0:nsub, 0:1].broadcast_to([128, nsub, 32]), op=ALU.subtract), reads=[ml, top8], writes=[ex])
                    s.op("act", lambda e: e.activation(out=ex[:, 0:nsub, :], in_=ex[:, 0:nsub, :], func=AF.Exp), reads=[ex], writes=[ex])
                    s.op("dve", lambda e: e.tensor_tensor(out=ex[:, 0:nsub, :], in0=ex[:, 0:nsub, :], in1=selt[:, 0:nsub, :], op=ALU.mult), reads=[ex, selt], writes=[ex])
                    s.op("dve", lambda e: e.tensor_reduce(out=se[:, 0:nsub], in_=ex[:, 0:nsub, :], axis=AX.X, op=ALU.add), reads=[ex], writes=[se])
                    s.op("dve", lambda e: e.tensor_tensor(out=se[:, 0:nsub], in0=se[:, 0:nsub], in1=sg[:, 0:nsub], op=ALU.mult), reads=[se, sg], writes=[se])
                    s.op("dve", lambda e: e.reciprocal(out=se[:, 0:nsub], in_=se[:, 0:nsub]), reads=[se], writes=[se])
                    s.op("dve", lambda e: e.tensor_tensor(out=ex[:, 0:nsub, :], in0=ex[:, 0:nsub, :], in1=se[:, 0:nsub].unsqueeze(2).broadcast_to([128, nsub, 32]), op=ALU.mult), reads=[ex, se], writes=[ex])
                    pw_ = ps[0]
                    for sub in range(nsub):
                        s.op("pe", lambda e: e.transpose(pw_[0:32, sub * 128:(sub + 1) * 128], ex[:, sub, :], ident[:]), reads=[ex, ident], writes=[pw_])
                    s.op("act", lambda e: e.copy(out=wts[:, 0:n], in_=pw_[0:32, 0:n]), reads=[pw_], writes=[wts])
                    s.dma(WtT[b, :, t0:t0 + n], wts[:, 0:n], reads=[wts], writes=[("WtT", b)])
            s.barrier()
        if stop == "M":
            s.finish(); s.close(); return nc

        half = (len(tiles) + 1) // 2
        groups = [tiles[:half], tiles[half:]]
        GMAX = max(sum(n for (_, n) in g) for g in groups)
        with contextlib.ExitStack() as ph:
            h2g = s.sbuf("h2g", [128, 8, GMAX], BF16, stack=ph)
            acc = s.sbuf("eacc", [128, 8, GMAX], stack=ph)
            for b in range(NB):
                for grp in groups:
                    g0 = grp[0][0]
                    G = sum(n for (_, n) in grp)
                    s.dma(h2g[:, :, 0:G], h2T[b, :, g0:g0 + G].rearrange("(c p) t -> p c t", p=128), reads=[("h2T", b)], writes=[h2g])
                    with contextlib.ExitStack() as ph2:
                        stg = [s.sbuf("stg%d" % i, [128, 2048], stack=ph2) for i in range(2)]
                        w1b = [s.sbuf("w1b%d" % i, [128, 8, FF], BF16, stack=ph2) for i in range(2)]
                        w3b = [s.sbuf("w3b%d" % i, [128, 8, FF], BF16, stack=ph2) for i in range(2)]
                        w2b = [s.sbuf("w2b%d" % i, [128, 2, D], BF16, stack=ph2) for i in range(2)]
                        wbc = [s.sbuf("wbc%d" % i, [128, GMAX], stack=ph2) for i in range(2)]
                        hhb2 = [[s.sbuf("hhc%d_%d" % (i, k_), [128, 512], BF16, stack=ph2) for k_ in range(2)] for i in range(2)]
                        eel = [s.sbuf("eel%d" % i, [128, 512], stack=ph2) for i in range(2)]
                        hhl = [s.sbuf("hhl%d" % i, [128, 512], stack=ph2) for i in range(2)]
                        sicnt = [0]

                        def load_w(ex_):
                            st_ = ex_ % 2
                            for (dstw, srcw) in ((w1b[st_], w1_in[l, ex_, :, :].rearrange("(kc p) f -> p kc f", p=128)),
                                                 (w3b[st_], w3_in[l, ex_, :, :].rearrange("(kc p) f -> p kc f", p=128)),
                                                 (w2b[st_], w2_in[l, ex_, :, :].rearrange("(fc p) n -> p fc n", p=128))):
                                sg_ = stg[sicnt[0] % 2]
                                sicnt[0] += 1
                                a_ = dstw.t.shape[1]
                                s.dma(sg_[:].rearrange("p (a b) -> p a b", a=a_), srcw, writes=[sg_])
                                s.op("act", lambda e: e.copy(out=dstw[:], in_=sg_[:].rearrange("p (a b) -> p a b", a=a_)), reads=[sg_], writes=[dstw])
                            s.dma(wbc[st_][:, 0:G], WtT[b, ex_:ex_ + 1, g0:g0 + G].partition_broadcast(128), reads=[("WtT", b)], writes=[wbc[st_]])

                        items = [(ex_, ti) for ex_ in range(NEXP) for ti in range(len(grp))]

                        def stage1(idx):
                            ex_, ti = items[idx]
                            st_ = ex_ % 2
                            t0, n = grp[ti]
                            u0 = t0 - g0
                            for fc in range(2):
                                pa, pb = ps[fc * 2], ps[fc * 2 + 1]
                                ee, hh = eel[fc], hhl[fc]
                                for kc in range(8):
                                    s.op("pe", lambda e: e.matmul(pa[:, 0:n], lhsT=w1b[st_][:, kc, fc * 128:(fc + 1) * 128], rhs=h2g[:, kc, u0:u0 + n], start=(kc == 0), stop=(kc == 7)),
                                         reads=[w1b[st_], h2g], writes=[pa])
                                for kc in range(8):
                                    s.op("pe", lambda e: e.matmul(pb[:, 0:n], lhsT=w3b[st_][:, kc, fc * 128:(fc + 1) * 128], rhs=h2g[:, kc, u0:u0 + n], start=(kc == 0), stop=(kc == 7)),
                                         reads=[w3b[st_], h2g], writes=[pb])
                                s.op("act", lambda e: e.activation(out=ee[:, 0:n], in_=pa[:, 0:n], func=AF.Exp, scale=-1.0), reads=[pa], writes=[ee])
                                s.op("act", lambda e: e.activation(out=ee[:, 0:n], in_=ee[:, 0:n], func=AF.Ln, bias=1.0), reads=[ee], writes=[ee])
                                s.op("act", lambda e: e.activation(out=ee[:, 0:n], in_=ee[:, 0:n], func=AF.Exp, scale=-1.0), reads=[ee], writes=[ee])
                                s.op("dve", lambda e: e.tensor_tensor(out=hh[:, 0:n], in0=pa[:, 0:n], in1=ee[:, 0:n], op=ALU.mult), reads=[pa, ee], writes=[hh])
                                s.op("dve", lambda e: e.tensor_tensor(out=hh[:, 0:n], in0=pb[:, 0:n], in1=hh[:, 0:n], op=ALU.mult), reads=[pb, hh], writes=[hh])
                                s.op("dve", lambda e: e.tensor_tensor(out=hhb2[idx % 2][fc][:, 0:n], in0=hh[:, 0:n], in1=wbc[st_][:, u0:u0 + n], op=ALU.mult),
                                     reads=[hh, wbc[st_]], writes=[hhb2[idx % 2][fc]])

                        def stage2(idx):
                            ex_, ti = items[idx]
                            st_ = ex_ % 2
                            t0, n = grp[ti]
                            u0 = t0 - g0
                            for oc in range(8):
                                py = ps[4 + oc % 4]
                                for fc in range(2):
                                    s.op("pe", lambda e: e.matmul(py[:, 0:n], lhsT=w2b[st_][:, fc, oc * 128:(oc + 1) * 128], rhs=hhb2[idx % 2][fc][:, 0:n], start=(fc == 0), stop=(fc == 1)),
                                         reads=[w2b[st_], hhb2[idx % 2][fc]], writes=[py])
                                if ex_ == 0:
                                    s.op("act", lambda e: e.copy(out=acc[:, oc, u0:u0 + n], in_=py[:, 0:n]), reads=[py], writes=[acc])
                                else:
                                    s.op("dve", lambda e: e.tensor_tensor(out=acc[:, oc, u0:u0 + n], in0=py[:, 0:n], in1=acc[:, oc, u0:u0 + n], op=ALU.add), reads=[py, acc], writes=[acc])

                        load_w(0)
                        for idx in range(len(items) + 1):
                            if idx < len(items):
                                stage1(idx)
                            if idx >= 1:
                                stage2(idx - 1)
                            if idx < len(items) and items[idx][1] == 0 and items[idx][0] + 1 < NEXP:
                                load_w(items[idx][0] + 1)
                        s.barrier()
                    with contextlib.ExitStack() as ph2:
                        xtb = [s.sbuf("ext%d" % i, [128, 8, 512], stack=ph2) for i in range(2)]
                        for ti, (t0, n) in enumerate(grp):
                            j = NB if t0 < L else b
                            u0 = t0 - g0
                            xt = xtb[ti % 2]
                            s.dma(xt[:, :, 0:n], xT[b, :, t0:t0 + n].rearrange("(c p) t -> p c t", p=128), reads=[("xT", b, t0)], writes=[xt])
                            for oc in range(8):
                                s.op("dve", lambda e: e.scalar_tensor_tensor(out=xt[:, oc, 0:n], in0=acc[:, oc, u0:u0 + n], scalar=mod[l][:, 40 + oc, j:j + 1], in1=xt[:, oc, 0:n],
                                                                             op0=ALU.mult, op1=ALU.add), reads=[acc, mod[l], xt], writes=[xt])
                            s.dma(xT[b, :, t0:t0 + n].rearrange("(c p) t -> p c t", p=128), xt[:, :, 0:n], reads=[xt], writes=[("xT", b, t0)])
                        s.barrier()
        if stop == "E":
            s.finish(); s.close(); return nc

    with contextlib.ExitStack() as ph:
        fnw = s.sbuf("fnw", [128, 8], stack=ph)
        s.dma(fnw[:], fnwT[:, :], writes=[fnw])
        xtb = [s.sbuf("fxt%d" % i, [128, 8, 512], stack=ph) for i in range(2)]
        sqb = s.sbuf("fsq", [128, 8, 512], stack=ph)
        rstd = s.sbuf("frstd", [128, 512], stack=ph)
        otb = [s.sbuf("fot%d" % i, [128, 4, D], stack=ph) for i in range(2)]
        it = 0
        for b in range(NB):
            for (t0, n) in tiles:
                if t0 < L:
                    continue
                nsub = n // 128
                xt, ot = xtb[it % 2], otb[it % 2]
                it += 1
                s.dma(xt[:, :, 0:n], xT[b, :, t0:t0 + n].rearrange("(c p) t -> p c t", p=128), reads=[("xT", b, t0)], writes=[xt])
                s.op("act", lambda e: e.activation(out=sqb[:, :, 0:n], in_=xt[:, :, 0:n], func=AF.Square), reads=[xt], writes=[sqb])
                for c in range(8):
                    s.op("pe", lambda e: e.matmul(ps[0][:, 0:n], lhsT=onesm[:], rhs=sqb[:, c, 0:n], start=(c == 0), stop=(c == 7)), reads=[onesm, sqb], writes=[ps[0]])
                rsqrt_("act", rstd[:, 0:n], ps[0][:, 0:n], [ps[0]], [rstd], rstd[:, 0:n])
                s.op("dve", lambda e: e.tensor_tensor(out=sqb[:, :, 0:n], in0=xt[:, :, 0:n], in1=rstd[:, 0:n].unsqueeze(1).broadcast_to([128, 8, n]), op=ALU.mult), reads=[xt, rstd], writes=[sqb])
                s.op("dve", lambda e: e.tensor_tensor(out=sqb[:, :, 0:n], in0=sqb[:, :, 0:n], in1=fnw[:].unsqueeze(2).broadcast_to([128, 8, n]), op=ALU.mult), reads=[sqb, fnw], writes=[sqb])
                for sub in range(nsub):
                    for hf in range(2):
                        pT_ = ps[1 + (sub * 2 + hf) % 4]
                        for c4 in range(4):
                            c = hf * 4 + c4
                            s.op("pe", lambda e: e.transpose(pT_[:, c4 * 128:(c4 + 1) * 128], sqb[:, c, sub * 128:(sub + 1) * 128], ident[:]), reads=[sqb, ident], writes=[pT_])
                        if hf:
                            s.op("act", lambda e: e.copy(out=ot[:, sub, hf * 512:(hf + 1) * 512], in_=pT_[:, :]), reads=[pT_], writes=[ot])
                        else:
                            s.op("dve", lambda e: e.tensor_copy(out=ot[:, sub, hf * 512:(hf + 1) * 512], in_=pT_[:, :]), reads=[pT_], writes=[ot])
                s.dma(out_hbm[b, t0 - L:t0 - L + n, :].rearrange("(s p) f -> p s f", p=128), ot[:, 0:nsub, :], reads=[ot])
    s.finish()
    s.close()
    return nc


def _partner():
    d = np.arange(64)
    return np.where((d % 32) < 16, d + 16, d - 16)


def host_consts(S, L):
    T = S + L
    c = {}
    c["c_ident"] = np.eye(128, dtype=np.float32)
    bd = np.zeros((128, 128), np.float32)
    bd[:64, :64] = 1.0
    bd[64:, 64:] = 1.0
    c["c_bd"] = bd
    d = np.arange(128) % 64
    axis = d // 32
    f = d % 16
    inv = (10000.0 ** (-(np.arange(16, dtype=np.float32)) / 16.0)).astype(np.float32)
    tl = np.arange(S)
    pos = np.stack([(tl // 64).astype(np.float32), (tl % 64).astype(np.float32)], 0)
    ang = pos[axis, :] * inv[f][:, None]
    cos = np.ones((128, T), np.float32)
    sin = np.zeros((128, T), np.float32)
    cos[:, L:] = np.cos(ang.astype(np.float32))
    sgn = np.where((d % 32) < 16, -1.0, 1.0).astype(np.float32)
    sin[:, L:] = np.sin(ang.astype(np.float32)) * sgn[:, None]
    c["c_cos"] = cos
    c["c_sin"] = sin
    p = np.arange(128)[:, None]
    i = np.arange(128)[None, :]
    m = np.zeros((6, 128, 128), np.float32)
    m[0] = (p <= i)
    m[1] = (p >= i)
    m[2] = (p == 127)
    m[3] = (p == 0)
    m[4] = np.where(i > p, BIG, 0.0)
    m[5] = np.where(i < p, BIG, 0.0)
    c["c_masks"] = m
    return c


def host_weights(inp):
    DEPTH = inp["w_in"].shape[0]
    o = {}
    o["ada_w"] = np.ascontiguousarray(inp["ada_w"])
    o["ada_bT"] = np.ascontiguousarray(inp["ada_b"].reshape(DEPTH, 48, 128).transpose(0, 2, 1))
    o["nmixT"] = np.ascontiguousarray(inp["norm_mix_w"].reshape(DEPTH, 8, 128).transpose(0, 2, 1))
    o["nffnT"] = np.ascontiguousarray(inp["norm_ffn_w"].reshape(DEPTH, 8, 128).transpose(0, 2, 1))
    o["fnwT"] = np.ascontiguousarray(inp["final_norm_w"].reshape(8, 128).T)
    w = inp["w_in"]
    pt = _partner()
    ext = np.empty((DEPTH, D, WEXT), np.float32)
    ext[:, :, CQ:WEXT - 640] = w[:, :, 0:2848]
    qcols = np.empty(512, np.int64)
    rqcols = np.empty(512, np.int64)
    for c in range(4):
        for two in range(2):
            h = two * 4 + c
            qcols[c * 128 + two * 64:c * 128 + two * 64 + 64] = h * 64 + np.arange(64)
            rqcols[c * 128 + two * 64:c * 128 + two * 64 + 64] = h * 64 + pt
    ext[:, :, CQ:CQ + 512] = w[:, :, qcols]
    ext[:, :, CRQ:CRQ + 512] = w[:, :, rqcols]
    rk = np.concatenate([512 + pt, 512 + 64 + pt])
    ext[:, :, CRK:CRK + 128] = w[:, :, rk]
    o["w_in_ext"] = ext
    qw, kw = inp["q_norm_w"], inp["k_norm_w"]
    dd = np.arange(128) % 64
    o["qkw"] = np.ascontiguousarray(np.stack([qw[:, dd], qw[:, pt[dd]], kw[:, dd], kw[:, pt[dd]]], -1))
    o["qkw_row"] = np.ascontiguousarray(np.stack([qw, kw], 1))
    o["conv_wT"] = np.ascontiguousarray(inp["conv_w"].reshape(DEPTH, 5, 12, 128).transpose(0, 3, 2, 1))
    o["dn_A_log"] = np.ascontiguousarray(inp["dn_A_log"].reshape(DEPTH, 16))
    o["dn_dt_bias"] = np.ascontiguousarray(inp["dn_dt_bias"].reshape(DEPTH, 16))
    o["dn_norm_w"] = np.ascontiguousarray(inp["dn_norm_w"])
    o["w_out"] = np.ascontiguousarray(inp["w_out"])
    o["wr"] = np.ascontiguousarray(np.concatenate([inp["rg_w"], inp["re_w"]], -1))
    o["rb"] = np.ascontiguousarray(np.concatenate([inp["rg_b"], inp["re_b"]], -1))
    o["w1"] = np.ascontiguousarray(inp["w1"])
    o["w3"] = np.ascontiguousarray(inp["w3"])
    o["w2"] = np.ascontiguousarray(inp["w2"])
    return o


def host_core_inputs(inp, core, NB):
    b0 = core * NB
    o = {}
    o["x"] = np.ascontiguousarray(inp["x"][b0:b0 + NB])
    o["ctx"] = np.ascontiguousarray(inp["ctx"][b0:b0 + NB])
    vecs = [inp["c"][b0 + j] for j in range(NB)] + [inp["c_ctx"]]
    o["cT"] = np.ascontiguousarray(np.stack(vecs, -1).reshape(8, 128, NB + 1).transpose(1, 0, 2))
    return o


def kernel(**inputs):
    inputs = {k: np.asarray(v, dtype=np.float32) for k, v in inputs.items()}
    B, S, _ = inputs["x"].shape
    L = inputs["ctx"].shape[1]
    DEPTH = inputs["w_in"].shape[0]
    ncores = 8
    NB = B // ncores
    cfg = dict(NB=NB, S=S, L=L, DEPTH=DEPTH)
    nc = build(cfg)
    shared = host_weights(inputs)
    shared.update(host_consts(S, L))
    in_maps = []
    for core in range(ncores):
        m = dict(shared)
        m.update(host_core_inputs(inputs, core, NB))
        in_maps.append(m)
    res = run_bass_kernel_spmd(nc, in_maps, core_ids=list(range(ncores)))
    return np.concatenate([r["out"] for r in res.results], axis=0)
```

```python
import contextlib
import math
import numpy as np
import concourse.bass as bass
import concourse.mybir as mybir
from concourse.bass_utils import run_bass_kernel_spmd

F32 = mybir.dt.float32
BF16 = mybir.dt.bfloat16
AF = mybir.ActivationFunctionType
ALU = mybir.AluOpType
AX = mybir.AxisListType

D = 1024
NH = 8
HD = 64
EPS = 1e-6
CQ, CK, CV, CDQKV, CGATE, CBA, CRQ, CRK, WEXT = 0, 512, 640, 768, 2304, 2816, 2848, 3360, 3488
NEXP = 32
FF = 256
BIG = 1.0e5
CH = 128


class Buf:
    def __init__(self, t, key):
        self.t = t
        self.key = key

    def __getitem__(self, idx):
        return self.t[idx]


class Sched:
    NSLOT = 16

    def __init__(self, nc):
        self.nc = nc
        self.es = contextlib.ExitStack()
        self.engs = {"pe": nc.tensor, "act": nc.scalar, "dve": nc.vector, "pool": nc.gpsimd, "sp": nc.sync}
        self.sem = {}
        self.cnt = {}
        for n in self.engs:
            self.sem[n] = self.es.enter_context(nc.semaphore("s_" + n))
            self.cnt[n] = 0
        for i in range(self.NSLOT):
            k = ("d", i)
            self.sem[k] = self.es.enter_context(nc.semaphore("s_d%d" % i))
            self.cnt[k] = 0
        self.waited = {n: {} for n in self.engs}
        self.res = {}
        self.slot = 0
        self.ninst = 0
        self.dead = False
        self.excl = set()

    def sbuf(self, name, shape, dtype=F32, stack=None):
        self.nbuf = getattr(self, "nbuf", 0) + 1
        name = "%s_u%d" % (name, self.nbuf)
        t = (stack or self.es).enter_context(self.nc.sbuf_tensor(name, list(shape), dtype))
        return Buf(t, name)

    def psum(self, name, shape, dtype=F32):
        t = self.es.enter_context(self.nc.psum_tensor(name, list(shape), dtype))
        self.excl.add(name)
        return Buf(t, name)

    def _val(self, k, c):
        return c * 16 if isinstance(k, tuple) else c

    def _keys(self, xs):
        return [x.key if isinstance(x, Buf) else x for x in xs]

    def _deps(self, me, reads, writes):
        deps = {}
        for r in reads:
            st = self.res.get(r)
            if st is None:
                continue
            for k, c in st[0].items():
                if k == me and me == "pe":
                    continue
                if c > deps.get(k, 0):
                    deps[k] = c
            if r in self.excl:
                for k, c in st[1].items():
                    if k != me and c > deps.get(k, 0):
                        deps[k] = c
        for w in writes:
            st = self.res.get(w)
            if st is None:
                continue
            for dd in st:
                for k, c in dd.items():
                    if (k != me or me == "pool") and c > deps.get(k, 0):
                        deps[k] = c
        return deps

    def _wait(self, eng, deps):
        wd = self.waited[eng]
        e = self.engs[eng]
        for k, c in deps.items():
            v = self._val(k, c)
            if wd.get(k, 0) >= v:
                continue
            e.wait_ge(self.sem[k], v)
            wd[k] = v

    def _commit(self, me, reads, writes):
        c = self.cnt[me]
        for r in reads:
            st = self.res.setdefault(r, [{}, {}])
            st[1][me] = c
        for w in writes:
            st = self.res.setdefault(w, [{}, {}])
            st[0] = {me: c}
            st[1] = {}

    def op(self, eng, fn, reads=(), writes=()):
        if self.dead:
            return None
        reads = self._keys(reads)
        writes = self._keys(writes)
        self._wait(eng, self._deps(eng, reads, writes))
        ins = fn(self.engs[eng])
        ins.then_inc(self.sem[eng], 1)
        self.cnt[eng] += 1
        self.ninst += 1
        self._commit(eng, reads, writes)
        return ins

    def dma(self, out, in_, reads=(), writes=(), eng="sp", **kw):
        if self.dead:
            return None
        reads = self._keys(reads)
        writes = self._keys(writes)
        k = ("d", self.slot)
        self.slot = (self.slot + 1) % self.NSLOT
        deps = self._deps(k, reads, writes)
        if self.cnt[k] > 0:
            deps[k] = max(deps.get(k, 0), self.cnt[k])
        self._wait(eng, deps)
        ins = self.engs[eng].dma_start(out=out, in_=in_, **kw)
        ins.then_inc(self.sem[k], 16)
        self.cnt[k] += 1
        self.ninst += 1
        self._commit(k, reads, writes)
        return ins

    def barrier(self):
        if self.dead:
            return
        deps = {}
        for k, c in self.cnt.items():
            if c > 0 and k != "sp":
                deps[k] = c
        for n in self.engs:
            d = {k: c for k, c in deps.items() if k != n}
            self._wait(n, d)
        self.res = {}

    def finish(self):
        if self.dead:
            return
        deps = {k: c for k, c in self.cnt.items() if c > 0 and k != "sp"}
        self._wait("sp", deps)

    def close(self):
        self.es.close()


def _tiles(L, S):
    ts = [(0, L)]
    for i in range(S // 512):
        ts.append((L + i * 512, 512))
    return ts


def build(cfg):
    NB, S, L, DEPTH = cfg["NB"], cfg["S"], cfg["L"], cfg["DEPTH"]
    taps = cfg.get("taps", ())
    stop = cfg.get("stop", "end")
    T = S + L
    TP = T
    NT128 = T // 128
    NCH = T // CH
    tiles = _tiles(L, S)
    PL = cfg.get('pooleng', 'dve')
    nc = bass.Bass("TRN2", target_bir_lowering=False)

    def din(name, shape, dt=F32):
        return nc.dram_tensor(name, list(shape), dt, kind="ExternalInput").ap()

    dbg_kind = "ExternalOutput" if taps else "Internal"

    def dscr(name, shape, dt=F32):
        kind = "ExternalOutput" if name in taps else "Internal"
        return nc.dram_tensor(name, list(shape), dt, kind=kind).ap()

    x_in = din("x", [NB, S, D])
    ctx_in = din("ctx", [NB, L, D])
    cT_in = din("cT", [128, 8, NB + 1])
    ada_w = din("ada_w", [DEPTH, D, 6 * D])
    ada_bT = din("ada_bT", [DEPTH, 128, 48])
    nmixT = din("nmixT", [DEPTH, 128, 8])
    nffnT = din("nffnT", [DEPTH, 128, 8])
    w_in_ext = din("w_in_ext", [DEPTH, D, WEXT])
    qkw = din("qkw", [DEPTH, 128, 4])
    qkw_row = din("qkw_row", [DEPTH, 2, 64])
    conv_wT = din("conv_wT", [DEPTH, 128, 12, 5])
    dn_A_log = din("dn_A_log", [DEPTH, 16])
    dn_dt_bias = din("dn_dt_bias", [DEPTH, 16])
    dn_norm_w = din("dn_norm_w", [DEPTH, 64])
    w_out = din("w_out", [DEPTH, D, D])
    wr_in = din("wr", [DEPTH, D, 36])
    rb_in = din("rb", [DEPTH, 36])
    w1_in = din("w1", [DEPTH, NEXP, D, FF])
    w3_in = din("w3", [DEPTH, NEXP, D, FF])
    w2_in = din("w2", [DEPTH, NEXP, FF, D])
    fnwT = din("fnwT", [128, 8])
    c_ident = din("c_ident", [128, 128])
    c_bd = din("c_bd", [128, 128])
    c_cos = din("c_cos", [128, T])
    c_sin = din("c_sin", [128, T])
    c_masks = din("c_masks", [6, 128, 128])
    c_lmask = din("c_lmask", [2, 7, 128, 128])
    out_hbm = nc.dram_tensor("out", [NB, S, D], F32, kind="ExternalOutput").ap()

    xT = dscr("xT", [NB, D, T])
    dq_raw = dscr("dq_raw", [NB, 1536, TP])
    gate_s = dscr("gate_s", [NB, T, 512])
    gb = dscr("gb", [NB, T, 32])
    attnT = dscr("attnT", [NB, NH, HD, T], BF16)
    qnT = dscr("qnT", [NB, 512, T], BF16)
    knT = dscr("knT", [NB, 512, T], BF16)
    k_tok = dscr("k_tok", [NB, T, 512])
    v_tok = dscr("v_tok", [NB, T, 512])
    o_dir = [dscr("o_f", [NB, T, 512]), dscr("o_b", [NB, T, 512])]
    h2T = dscr("h2T", [NB, D, T], BF16)
    WtT = dscr("WtT", [NB, NEXP, T])
    tapd = {}
    for nm, shp, dt in [("t_qT", [NB, 128, 4, T], BF16), ("t_kT", [NB, 128, T], BF16), ("t_V", [NB, 128, NT128, 200], BF16),
                        ("t_mod", [DEPTH, 128, 48, NB + 1], F32)]:
        if nm in taps:
            tapd[nm] = nc.dram_tensor(nm, shp, dt, kind="ExternalOutput").ap()

    s = Sched(nc)
    _tapped = set()

    def tap(name, buf, ap, shape, dt=F32):
        if ("dbg_" + name) not in taps or name in _tapped:
            return
        _tapped.add(name)
        dtens = nc.dram_tensor("dbg_" + name, list(shape), dt, kind="ExternalOutput").ap()
        s.dma(dtens, ap, reads=[buf])

    ps = [s.psum("ps%d" % i, [128, 512]) for i in range(4)]
    psS = [s.psum("psS%d" % i, [128, 1024]) for i in range(2)]
    for i in range(2):
        for hf in range(2):
            kname = "psv%d" % (4 + i * 2 + hf)
            s.excl.add(kname)
            ps.append(Buf(psS[i].t[:, hf * 512:(hf + 1) * 512], kname))

    ident = s.sbuf("ident", [128, 128])
    identb = s.sbuf("identb", [128, 128], BF16)
    ones = s.sbuf("ones", [128, 128])
    onesm = s.sbuf("onesm", [128, 128])
    bd64 = s.sbuf("bd64", [128, 128])
    bd1 = s.sbuf("bd1", [128, 128])
    epsc = s.sbuf("epsc", [128, 1])
    mod = [s.sbuf("mod%d" % l, [128, 48, NB + 1]) for l in range(DEPTH)]
    amix = [s.sbuf("amix%d" % l, [128, 8, NB + 1]) for l in range(DEPTH)]
    affn = [s.sbuf("affn%d" % l, [128, 8, NB + 1]) for l in range(DEPTH)]
    s.dma(ident[:], c_ident[:, :], writes=[ident])
    s.dma(bd1[:], c_bd[:, :], writes=[bd1])
    s.op("dve", lambda e: e.tensor_copy(out=identb[:], in_=ident[:]), reads=[ident], writes=[identb])
    s.op("dve", lambda e: e.memset(ones[:], 1.0), writes=[ones])
    s.op("dve", lambda e: e.memset(onesm[:], 1.0 / D), writes=[onesm])
    s.op("dve", lambda e: e.memset(epsc[:], EPS), writes=[epsc])
    s.op("dve", lambda e: e.tensor_scalar(out=bd64[:], in0=bd1[:], scalar1=1.0 / 64, scalar2=None, op0=ALU.mult), reads=[bd1], writes=[bd64])

    def rsqrt_(eng_ln, out_ap, in_ap, reads, writes, tmp, scale=1.0):
        s.op("act", lambda e: e.activation(out=tmp, in_=in_ap, func=AF.Ln, bias=epsc[:, 0:1], scale=scale), reads=list(reads) + [epsc], writes=writes)
        s.op("act", lambda e: e.activation(out=out_ap, in_=tmp, func=AF.Exp, scale=-0.5), reads=writes, writes=writes)

    with contextlib.ExitStack() as ph:
        scT = s.sbuf("scT", [128, 8, NB + 1], stack=ph)
        sct = s.sbuf("sct", [128, 8, NB + 1], stack=ph)
        adab = s.sbuf("adab", [128, 48], stack=ph)
        nw = s.sbuf("nw", [128, 8], stack=ph)
        nw2 = s.sbuf("nw2", [128, 8], stack=ph)
        awp = [s.sbuf("awp%d" % i, [128, 8, 1024], stack=ph) for i in range(2)]
        s.dma(scT[:], cT_in[:, :, :], writes=[scT])
        s.op("act", lambda e: e.activation(out=sct[:], in_=scT[:], func=AF.Exp, scale=-1.0), reads=[scT], writes=[sct])
        s.op("dve", lambda e: e.tensor_scalar(out=sct[:], in0=sct[:], scalar1=1.0, scalar2=None, op0=ALU.add), reads=[sct], writes=[sct])
        s.op("dve", lambda e: e.reciprocal(out=sct[:], in_=sct[:]), reads=[sct], writes=[sct])
        s.op("dve", lambda e: e.tensor_tensor(out=scT[:], in0=scT[:], in1=sct[:], op=ALU.mult), reads=[scT, sct], writes=[scT])
        NJ = NB + 1
        for l in range(DEPTH):
            s.dma(adab[:], ada_bT[l, :, :], writes=[adab])
            s.dma(nw[:], nmixT[l, :, :], writes=[nw])
            s.dma(nw2[:], nffnT[l, :, :], writes=[nw2])
            for piece in range(6):
                aw = awp[piece % 2]
                s.dma(aw[:], ada_w[l, :, piece * 1024:(piece + 1) * 1024].rearrange("(kc p) n -> p kc n", p=128), writes=[aw])
                pb = ps[piece % 2]
                for oc in range(8):
                    for kc in range(8):
                        s.op("pe", lambda e: e.matmul(pb[:, oc * NJ:(oc + 1) * NJ], lhsT=aw[:, kc, oc * 128:(oc + 1) * 128], rhs=scT[:, kc, :],
                                                     start=(kc == 0), stop=(kc == 7)), reads=[aw, scT], writes=[pb])
                s.op("dve", lambda e: e.tensor_tensor(out=mod[l][:, piece * 8:(piece + 1) * 8, :],
                                                      in0=pb[:, 0:8 * NJ].rearrange("p (a b) -> p a b", b=NJ),
                                                      in1=adab[:, piece * 8:(piece + 1) * 8].unsqueeze(2).broadcast_to([128, 8, NJ]), op=ALU.add),
                     reads=[pb, adab], writes=[mod[l]])
            for (dst, wv, off) in ((amix[l], nw, 8), (affn[l], nw2, 32)):
                s.op("dve", lambda e: e.tensor_scalar(out=dst[:], in0=mod[l][:, off:off + 8, :], scalar1=1.0, scalar2=None, op0=ALU.add), reads=[mod[l]], writes=[dst])
                s.op("dve", lambda e: e.tensor_tensor(out=dst[:], in0=dst[:], in1=wv[:].unsqueeze(2).broadcast_to([128, 8, NJ]), op=ALU.mult), reads=[dst, wv], writes=[dst])
            if "t_mod" in tapd:
                s.dma(tapd["t_mod"][l], mod[l][:], reads=[mod[l]])
        s.barrier()
    if stop == "prep":
        s.finish(); s.close(); return nc

    with contextlib.ExitStack() as ph:
        xin = [s.sbuf("xin%d" % i, [128, 4, D], stack=ph) for i in range(2)]
        xo = [s.sbuf("xo%d" % i, [128, 8, 512], stack=ph) for i in range(2)]
        zt = s.sbuf("zt", [128, 2], stack=ph)
        s.op("dve", lambda e: e.memset(zt[:], 0.0), writes=[zt])
        it = 0
        for b in range(NB):
            for (t0, n) in tiles:
                xi, xb_ = xin[it % 2], xo[it % 2]
                nsub = n // 128
                src = ctx_in[b, t0:t0 + n, :] if t0 < L else x_in[b, t0 - L:t0 - L + n, :]
                s.dma(xi[:, 0:nsub, :], src.rearrange("(s p) f -> p s f", p=128), writes=[xi])
                for fc in range(8):
                    pb = ps[fc % 4]
                    for sub in range(nsub):
                        s.op("pe", lambda e: e.transpose(pb[:, sub * 128:(sub + 1) * 128], xi[:, sub, fc * 128:(fc + 1) * 128], ident[:]), reads=[xi, ident], writes=[pb])
                    if fc % 2 == 0:
                        s.op("act", lambda e: e.copy(out=xb_[:, fc, 0:n], in_=pb[:, 0:n]), reads=[pb], writes=[xb_])
                    else:
                        s.op("dve", lambda e: e.tensor_copy(out=xb_[:, fc, 0:n], in_=pb[:, 0:n]), reads=[pb], writes=[xb_])
                s.dma(xT[b, :, t0:t0 + n].rearrange("(c p) t -> p c t", p=128), xb_[:, :, 0:n], reads=[xb_], writes=[("xT", b, t0)])
                it += 1
        s.barrier()

    if stop == "s0":
        s.finish(); s.close(); return nc

    for l in range(DEPTH):
        last = (l == DEPTH - 1)
        with contextlib.ExitStack() as ph:
            winb = s.sbuf("winb", [128, 8, WEXT], BF16, stack=ph)
            qkwt = s.sbuf("qkwt", [128, 4], stack=ph)
            qkrow = s.sbuf("qkrow", [128, 128], stack=ph)
            nshift = s.sbuf("nshift", [128, 1], stack=ph)
            mx = s.sbuf("mx", [128, 2], stack=ph)
            alog = s.sbuf("alog", [128, 16], stack=ph)
            dtb = s.sbuf("dtb", [128, 16], stack=ph)
            qTb = s.sbuf("qTb", [128, 4, T], BF16, stack=ph)
            kTb = s.sbuf("kTb", [128, T], BF16, stack=ph)
            Vb = s.sbuf("Vb", [128, NT128, 200], BF16, stack=ph)
            phw = contextlib.ExitStack()
            wst = [s.sbuf("wst%d" % i, [128, 872], stack=phw) for i in range(2)]
            ci = 0
            for kc in range(8):
                for q4 in range(4):
                    st = wst[ci % 2]
                    s.dma(st[:], w_in_ext[l, kc * 128:(kc + 1) * 128, q4 * 872:(q4 + 1) * 872], writes=[st])
                    eng = "act" if ci % 2 == 0 else "dve"
                    if eng == "act":
                        s.op("act", lambda e: e.copy(out=winb[:, kc, q4 * 872:(q4 + 1) * 872], in_=st[:]), reads=[st], writes=[winb])
                    else:
                        s.op("dve", lambda e: e.tensor_copy(out=winb[:, kc, q4 * 872:(q4 + 1) * 872], in_=st[:]), reads=[st], writes=[winb])
                    ci += 1
            s.barrier()
            phw.close()
            s.dma(qkwt[:], qkw[l, :, :], writes=[qkwt])
            s.dma(qkrow[:], qkw_row[l:l + 1, :, :].rearrange("o a b -> o (a b)").partition_broadcast(128), writes=[qkrow])
            s.dma(alog[:], dn_A_log[l:l + 1, :].partition_broadcast(128), writes=[alog])
            s.dma(dtb[:], dn_dt_bias[l:l + 1, :].partition_broadcast(128), writes=[dtb])
            s.op("act", lambda e: e.activation(out=alog[:], in_=alog[:], func=AF.Exp), reads=[alog], writes=[alog])
            s.op("dve", lambda e: e.tensor_scalar(out=alog[:], in0=alog[:], scalar1=-1.0, scalar2=None, op0=ALU.mult), reads=[alog], writes=[alog])
            s.op("dve", lambda e: e.tensor_reduce(out=mx[:, 0:1], in_=qkrow[:, 0:64], axis=AX.X, op=ALU.max, apply_absolute_value=True), reads=[qkrow], writes=[mx])
            s.op("dve", lambda e: e.tensor_reduce(out=mx[:, 1:2], in_=qkrow[:, 64:128], axis=AX.X, op=ALU.max, apply_absolute_value=True), reads=[qkrow, mx], writes=[mx])
            s.op("dve", lambda e: e.tensor_tensor(out=nshift[:], in0=mx[:, 0:1], in1=mx[:, 1:2], op=ALU.mult), reads=[mx], writes=[nshift])
            s.op("dve", lambda e: e.tensor_scalar(out=nshift[:], in0=nshift[:], scalar1=-8.0, scalar2=None, op0=ALU.mult), reads=[nshift], writes=[nshift])

            if cfg.get('cut') == 1:
                s.finish(); s.dead = True

            s.op("dve", lambda e: e.memset(Vb[:], 1.0), writes=[Vb])

            if cfg.get('cut') == 2:
                s.finish(); s.dead = True
            xt2 = [s.sbuf("xt%d" % i, [128, 8, 512], stack=ph) for i in range(1)]
            sqb = s.sbuf("sqb", [128, 8, 512], stack=ph)
            hTb = s.sbuf("hTb", [128, 8, 512], BF16, stack=ph)
            rstd = s.sbuf("rstd", [128, 512], stack=ph)
            cst = [s.sbuf("cst%d" % i, [128, 2, 512], stack=ph) for i in range(1)]
            rq = s.sbuf("rq", [128, 512], stack=ph)
            t1 = s.sbuf("t1", [128, 512], stack=ph)
            t2 = s.sbuf("t2", [128, 512], stack=ph)
            sqq = t2
            dstl = [s.sbuf("dqst%d" % i, [128, 4, 512], stack=ph) for i in range(1)]
            qzl = [[s.sbuf("qz%d_%d" % (k_, r_), [128, 1024], BF16, stack=ph) for r_ in range(2)] for k_ in range(2)]
            for k_ in range(2):
                for r_ in range(2):
                    s.op("dve", lambda e: e.memset(qzl[k_][r_][:], 0.0), writes=[qzl[k_][r_]])
            gstl = [s.sbuf("gst%d" % i, [128, 512], stack=ph) for i in range(1)]
            ge = s.sbuf("ge", [128, 512], stack=ph)
            gbt = s.sbuf("gbt", [128, 4, 32], stack=ph)
            gb1 = s.sbuf("gb1", [128, 4, 16], stack=ph)
            gb2 = s.sbuf("gb2", [128, 4, 16], stack=ph)
            ptb = [s.sbuf("ptb%d" % i, [128, 1024], BF16, stack=ph) for i in range(3)]
            rrowl = [s.sbuf("rrow%d" % i, [128, 512], stack=ph) for i in range(2)]
            aul = [s.sbuf("au%d" % i, [64, 512], stack=ph) for i in range(2)]
            ao = [s.sbuf("ao%d" % i, [64, 512], BF16, stack=ph) for i in range(2)]

            def proj(pb, col0, ncol, n):
                for kc in range(8):
                    s.op("pe", lambda e: e.matmul(pb[0:ncol, 0:n], lhsT=winb[:, kc, col0:col0 + ncol], rhs=hTb[:, kc, 0:n], start=(kc == 0), stop=(kc == 7)),
                         reads=[winb, hTb], writes=[pb])

            it = 0
            for b in range(NB):
                for (t0, n) in tiles:
                    j = NB if t0 < L else b
                    nsub = n // 128
                    xt = xt2[0]
                    cs = cst[0]
                    it += 1
                    s.dma(xt[:, :, 0:n], xT[b, :, t0:t0 + n].rearrange("(c p) t -> p c t", p=128), reads=[("xT", b, t0)], writes=[xt])
                    s.dma(cs[:, 0, 0:n], c_cos[:, t0:t0 + n], writes=[cs])
                    s.dma(cs[:, 1, 0:n], c_sin[:, t0:t0 + n], writes=[cs])
                    s.op("act", lambda e: e.activation(out=sqb[:, :, 0:n], in_=xt[:, :, 0:n], func=AF.Square), reads=[xt], writes=[sqb])
                    for c in range(8):
                        s.op("pe", lambda e: e.matmul(ps[0][:, 0:n], lhsT=onesm[:], rhs=sqb[:, c, 0:n], start=(c == 0), stop=(c == 7)), reads=[onesm, sqb], writes=[ps[0]])
                    rsqrt_("act", rstd[:, 0:n], ps[0][:, 0:n], [ps[0]], [rstd], rstd[:, 0:n])
                    s.op("dve", lambda e: e.tensor_tensor(out=sqb[:, :, 0:n], in0=xt[:, :, 0:n], in1=rstd[:, 0:n].unsqueeze(1).broadcast_to([128, 8, n]), op=ALU.mult),
                         reads=[xt, rstd], writes=[sqb])
                    for c in range(8):
                        s.op("dve", lambda e: e.tensor_scalar(out=hTb[:, c, 0:n], in0=sqb[:, c, 0:n], scalar1=amix[l][:, c, j:j + 1], scalar2=mod[l][:, c, j:j + 1],
                                                              op0=ALU.mult, op1=ALU.add), reads=[sqb, amix[l], mod[l]], writes=[hTb])

                    if cfg.get('cut') == 3 and it == cfg.get('cutit', 1):
                        s.finish(); s.dead = True
                    for c in range(5):
                        isq = c < 4
                        col = CQ + c * 128 if isq else CK
                        rcol = CRQ + c * 128 if isq else CRK
                        wi = 0 if isq else 2
                        pq, prq, pm = ps[1 + (c % 2) * 3], ps[2 + (c % 2) * 3], ps[3 + (c % 2) * 3]
                        proj(pq, col, 128, n)
                        if cfg.get('cut') == 11 and it == cfg.get('cutit', 1):
                            s.finish(); s.dead = True
                        proj(prq, rcol, 128, n)
                        if cfg.get('cut') == 10 and it == cfg.get('cutit', 1):
                            s.finish(); s.dead = True
                        s.op("act", lambda e: e.activation(out=sqq[:, 0:n], in_=pq[:, 0:n], func=AF.Square), reads=[pq], writes=[sqq])
                        if cfg.get('cut') == 12 and it == cfg.get('cutit', 1):
                            s.finish(); s.dead = True
                        if cfg.get('exp') == 1:
                            s.op("dve", lambda e: e.memset(rq[:, 0:n], 1.0), writes=[rq])
                        else:
                            s.op("pe", lambda e: e.matmul(pm[:, 0:n], lhsT=bd64[:], rhs=sqq[:, 0:n], start=True, stop=True), reads=[bd64, sqq], writes=[pm])
                            rsqrt_("act", rq[:, 0:n], pm[:, 0:n], [pm], [rq], rq[:, 0:n])
                        s.op("dve", lambda e: e.scalar_tensor_tensor(out=t1[:, 0:n], in0=pq[:, 0:n], scalar=qkwt[:, wi:wi + 1], in1=cs[:, 0, 0:n], op0=ALU.mult, op1=ALU.mult),
                             reads=[pq, qkwt, cs], writes=[t1])
                        if cfg.get('cut') == 13 and it == cfg.get('cutit', 1):
                            s.finish(); s.dead = True
                        s.op("dve", lambda e: e.scalar_tensor_tensor(out=t2[:, 0:n], in0=prq[:, 0:n], scalar=qkwt[:, wi + 1:wi + 2], in1=cs[:, 1, 0:n], op0=ALU.mult, op1=ALU.mult),
                             reads=[prq, qkwt, cs], writes=[t2])
                        if cfg.get('cut') == 14 and it == cfg.get('cutit', 1):
                            s.finish(); s.dead = True
                        s.op(PL, lambda e: e.tensor_tensor(out=t1[:, 0:n], in0=t1[:, 0:n], in1=t2[:, 0:n], op=ALU.add), reads=[t1, t2], writes=[t1])
                        dstb = qTb[:, c, t0:t0 + n] if isq else kTb[:, t0:t0 + n]
                        s.op(PL, lambda e: e.tensor_tensor(out=dstb, in0=t1[:, 0:n], in1=rq[:, 0:n], op=ALU.mult), reads=[t1, rq], writes=[qTb if isq else kTb])

                    if cfg.get('cut') == 4 and it == cfg.get('cutit', 1):
                        s.finish(); s.dead = True
                    for cc in range(12):
                        pb = ps[1 + cc % 6]
                        dst_ = dstl[0]
                        proj(pb, CDQKV + cc * 128, 128, n)
                        if cc % 2 == 0:
                            s.op("act", lambda e: e.copy(out=dst_[:, cc % 4, 0:n], in_=pb[:, 0:n]), reads=[pb], writes=[dst_])
                        else:
                            s.op("dve", lambda e: e.tensor_copy(out=dst_[:, cc % 4, 0:n], in_=pb[:, 0:n]), reads=[pb], writes=[dst_])
                        if cc % 4 == 3:
                            c4 = cc // 4
                            s.dma(dq_raw[b, c4 * 512:(c4 + 1) * 512, t0:t0 + n].rearrange("(c p) t -> p c t", p=128), dst_[:, :, 0:n], reads=[dst_], writes=[("dq_raw", b)])

                    if cfg.get('cut') == 5 and it == cfg.get('cutit', 1):
                        s.finish(); s.dead = True
                    pv = ps[7]
                    for sub in range(nsub):
                        for kc in range(8):
                            s.op("pe", lambda e: e.matmul(pv[:, sub * 128:(sub + 1) * 128], lhsT=hTb[:, kc, sub * 128:(sub + 1) * 128], rhs=winb[:, kc, CV:CV + 128],
                                                         start=(kc == 0), stop=(kc == 7)), reads=[hTb, winb], writes=[pv])
                    s.op("act", lambda e: e.copy(out=Vb[:, t0 // 128:t0 // 128 + nsub, 0:130].rearrange("p s (k d) -> p s k d", k=2)[:, :, :, 0:64],
                                                 in_=pv[:, 0:n].rearrange("p (s k d) -> p s k d", k=2, d=64)), reads=[pv], writes=[Vb])

                    if cfg.get('cut') == 6 and it == cfg.get('cutit', 1):
                        s.finish(); s.dead = True
                    for sub in range(nsub):
                        pg = ps[1 + sub % 4]
                        for kc in range(8):
                            s.op("pe", lambda e: e.matmul(pg[:, :], lhsT=hTb[:, kc, sub * 128:(sub + 1) * 128], rhs=winb[:, kc, CGATE:CGATE + 512],
                                                         start=(kc == 0), stop=(kc == 7)), reads=[hTb, winb], writes=[pg])
                        s.op("act", lambda e: e.activation(out=ge[:], in_=pg[:], func=AF.Exp, scale=-1.0), reads=[pg], writes=[ge])
                        s.op("act", lambda e: e.activation(out=ge[:], in_=ge[:], func=AF.Ln, bias=1.0), reads=[ge], writes=[ge])
                        s.op("act", lambda e: e.activation(out=ge[:], in_=ge[:], func=AF.Exp, scale=-1.0), reads=[ge], writes=[ge])
                        gst = gstl[0]
                        s.op("dve", lambda e: e.tensor_tensor(out=gst[:], in0=pg[:], in1=ge[:], op=ALU.mult), reads=[pg, ge], writes=[gst])
                        s.dma(gate_s[b, t0 + sub * 128:t0 + (sub + 1) * 128, :], gst[:], reads=[gst], writes=[("gate_s", b, t0)])

                    if cfg.get('cut') == 7 and it == cfg.get('cutit', 1):
                        s.finish(); s.dead = True
                    pba = ps[5]
                    for sub in range(nsub):
                        for kc in range(8):
                            s.op("pe", lambda e: e.matmul(pba[:, sub * 32:(sub + 1) * 32], lhsT=hTb[:, kc, sub * 128:(sub + 1) * 128], rhs=winb[:, kc, CBA:CBA + 32],
                                                         start=(kc == 0), stop=(kc == 7)), reads=[hTb, winb], writes=[pba])
                    pba3 = pba[:, 0:nsub * 32].rearrange("p (s c) -> p s c", c=32)
                    s.op("act", lambda e: e.activation(out=gb1[:, 0:nsub, :], in_=pba3[:, :, 0:16], func=AF.Exp, scale=-1.0), reads=[pba], writes=[gb1])
                    s.op("dve", lambda e: e.tensor_scalar(out=gb1[:, 0:nsub, :], in0=gb1[:, 0:nsub, :], scalar1=1.0, scalar2=None, op0=ALU.add), reads=[gb1], writes=[gb1])
                    s.op("dve", lambda e: e.reciprocal(out=gbt[:, 0:nsub, 0:16], in_=gb1[:, 0:nsub, :]), reads=[gb1], writes=[gbt])
                    s.op("dve", lambda e: e.tensor_tensor(out=gb2[:, 0:nsub, :], in0=pba3[:, :, 16:32], in1=dtb[:].unsqueeze(1).broadcast_to([128, nsub, 16]), op=ALU.add),
                         reads=[pba, dtb], writes=[gb2])
                    s.op("act", lambda e: e.activation(out=gb2[:, 0:nsub, :], in_=gb2[:, 0:nsub, :], func=AF.Exp), reads=[gb2], writes=[gb2])
                    s.op("act", lambda e: e.activation(out=gb2[:, 0:nsub, :], in_=gb2[:, 0:nsub, :], func=AF.Ln, bias=1.0), reads=[gb2], writes=[gb2])
                    s.op("dve", lambda e: e.tensor_tensor(out=gbt[:, 0:nsub, 16:32], in0=gb2[:, 0:nsub, :], in1=alog[:].unsqueeze(1).broadcast_to([128, nsub, 16]), op=ALU.mult),
                         reads=[gb2, alog, gbt], writes=[gbt])
                    s.dma(gb[b, t0:t0 + n, :].rearrange("(s p) c -> p s c", p=128), gbt[:, 0:nsub, :], reads=[gbt], writes=[("gb", b)])

                    if cfg.get('cut') == 8 and it == cfg.get('cutit', 1):
                        s.finish(); s.dead = True

                if "t_qT" in tapd:
                    if cfg.get('cut') == 9:
                        s.finish(); s.dead = True
                    s.dma(tapd["t_qT"][b], qTb[:], reads=[qTb])
                    s.dma(tapd["t_kT"][b], kTb[:], reads=[kTb])
                    s.dma(tapd["t_V"][b], Vb[:], reads=[Vb])
                if stop == "A":
                    continue
                aoi = 0
                ui = 0
                for kv in range(2):
                    pl = slice(kv * 64, (kv + 1) * 64)
                    for (t0, n) in tiles:
                        kts = list(range(L // 128)) if t0 < L else list(range(NT128))
                        for gp in range(2):
                            accb = [ps[(ui % 2) * 2], ps[(ui % 2) * 2 + 1]]
                            qz = qzl[kv][ui % 2]
                            ui += 1
                            LA = 2
                            s.op("dve", lambda e: e.tensor_copy(out=qz[pl, :].rearrange("p (two c) -> p two c", two=2)[:, :, 0:n], in_=qTb[pl, gp * 2:gp * 2 + 2, t0:t0 + n]),
                                 reads=[qTb], writes=[qz])

                            def emit_s(si):
                                kt = kts[si]
                                big = psS[si % 2]
                                halves = [ps[4 + (si % 2) * 2], ps[5 + (si % 2) * 2]]
                                for hh in range(2):
                                    g = gp * 2 + hh
                                    s.op("pe", lambda e: e.matmul(halves[hh][:, 0:n], lhsT=kTb[:, kt * 128:(kt + 1) * 128], rhs=qz[:, hh * 512:hh * 512 + n], start=True, stop=True),
                                         reads=[kTb, qz], writes=[halves[hh]])
                                pt_ = ptb[si % 3]
                                s.op("act", lambda e: e.activation(out=pt_[:].rearrange("p (two c) -> p two c", two=2)[:, :, 0:n],
                                                                   in_=big.t[:].rearrange("p (two c) -> p two c", two=2)[:, :, 0:n], func=AF.Exp, bias=nshift[:, 0:1], scale=0.125),
                                     reads=[halves[0], halves[1], nshift], writes=[pt_])

                            def emit_pv(si):
                                kt = kts[si]
                                pt_ = ptb[si % 3]
                                for hh in range(2):
                                    s.op("pe", lambda e: e.matmul(accb[hh][:, 0:n], lhsT=Vb[:, kt, kv * 65:kv * 65 + 128], rhs=pt_[:, hh * 512:hh * 512 + n],
                                                                 start=(si == 0), stop=(si == len(kts) - 1)), reads=[Vb, pt_], writes=[accb[hh]])

                            for si in range(len(kts) + LA):
                                if si < len(kts):
                                    emit_s(si)
                                if si - LA >= 0:
                                    emit_pv(si - LA)
                            for hh in range(2):
                                head = kv * 4 + gp * 2 + hh
                                pa_ = accb[hh]
                                a_o = ao[aoi % 2]
                                au = aul[aoi % 2]
                                rrow = rrowl[aoi % 2]
                                aoi += 1
                                s.op("dve", lambda e: e.reciprocal(out=rrow[64:65, 0:n], in_=pa_[64:65, 0:n]), reads=[pa_], writes=[rrow])
                                s.op("dve", lambda e: e.tensor_copy(out=au[:, 0:n], in_=pa_[0:64, 0:n]), reads=[pa_], writes=[au])
                                s.op("pe", lambda e: e.matmul(pa_[0:64, 0:n], lhsT=ones[64:65, 0:64], rhs=rrow[64:65, 0:n], start=True, stop=True), reads=[ones, rrow], writes=[pa_])
                                s.op("dve", lambda e: e.tensor_tensor(out=a_o[:, 0:n], in0=pa_[0:64, 0:n], in1=au[:, 0:n], op=ALU.mult), reads=[pa_, au], writes=[a_o])
                                s.dma(attnT[b, head, :, t0:t0 + n], a_o[:, 0:n], reads=[a_o], writes=[("attnT", b)])
            s.barrier()
        if stop in ("A", "attn"):
            s.finish(); s.close(); return nc

        with contextlib.ExitStack() as ph:
            cw = s.sbuf("cw", [128, 12, 5], stack=ph)
            s.dma(cw[:], conv_wT[l, :, :, :], writes=[cw])
            NR = 4
            rwb = [s.sbuf("rw%d" % i, [128, 516], stack=ph) for i in range(NR)]
            accl = [s.sbuf("cacc%d" % i, [128, 512], stack=ph) for i in range(NR)]
            cel = [s.sbuf("ce%d" % i, [128, 512], stack=ph) for i in range(NR)]
            cyl = [s.sbuf("cy%d" % i, [128, 512], stack=ph) for i in range(NR)]
            csql = [s.sbuf("csq%d" % i, [128, 512], stack=ph) for i in range(NR)]
            crnl = [s.sbuf("crn%d" % i, [128, 512], stack=ph) for i in range(NR)]
            cynl = [s.sbuf("cyn%d" % i, [128, 512], stack=ph) for i in range(NR)]
            cynb = [s.sbuf("cynb%d" % i, [128, 512], BF16, stack=ph) for i in range(NR)]
            ctk = [s.sbuf("ctk%d" % i, [128, 4, 128], stack=ph) for i in range(NR)]
            it = 0
            for b in range(NB):
                for cc in range(12):
                    for (t0, n) in tiles:
                        nsub = n // 128
                        r_ = it % NR
                        rw, ynb, tk = rwb[r_], cynb[r_], ctk[r_]
                        acc, ce, cy, csq, crn, cyn = accl[r_], cel[r_], cyl[r_], csql[r_], crnl[r_], cynl[r_]
                        it += 1
                        lo = t0 if t0 in (0, L) else t0 - 2
                        hi = t0 + n if (t0 + n) in (L, T) else t0 + n + 2
                        s.op("dve", lambda e: e.memset(rw[:, 0:2], 0.0), writes=[rw])
                        s.op("dve", lambda e: e.memset(rw[:, 2 + n:4 + n], 0.0), writes=[rw])
                        s.dma(rw[:, 2 - (t0 - lo):2 + n + (hi - t0 - n)], dq_raw[b, cc * 128:(cc + 1) * 128, lo:hi], reads=[("dq_raw", b)], writes=[rw])
                        s.op("act", lambda e: e.activation(out=acc[:, 0:n], in_=rw[:, 0:n], func=AF.Copy, scale=cw[:, cc, 0:1]), reads=[rw, cw], writes=[acc])
                        for jj in range(1, 5):
                            s.op("dve", lambda e: e.scalar_tensor_tensor(out=acc[:, 0:n], in0=rw[:, jj:jj + n], scalar=cw[:, cc, jj:jj + 1], in1=acc[:, 0:n],
                                                                         op0=ALU.mult, op1=ALU.add), reads=[rw, cw, acc], writes=[acc])
                        s.op("act", lambda e: e.activation(out=ce[:, 0:n], in_=acc[:, 0:n], func=AF.Exp, scale=-1.0), reads=[acc], writes=[ce])
                        s.op("act", lambda e: e.activation(out=ce[:, 0:n], in_=ce[:, 0:n], func=AF.Ln, bias=1.0), reads=[ce], writes=[ce])
                        s.op("act", lambda e: e.activation(out=ce[:, 0:n], in_=ce[:, 0:n], func=AF.Exp, scale=-1.0), reads=[ce], writes=[ce])
                        s.op("dve", lambda e: e.tensor_tensor(out=cy[:, 0:n], in0=acc[:, 0:n], in1=ce[:, 0:n], op=ALU.mult), reads=[acc, ce], writes=[cy])
                        src_tm = cy
                        if cc < 8:
                            pm = ps[it % 4]
                            s.op("act", lambda e: e.activation(out=csq[:, 0:n], in_=cy[:, 0:n], func=AF.Square), reads=[cy], writes=[csq])
                            s.op("pe", lambda e: e.matmul(pm[:, 0:n], lhsT=bd1[:], rhs=csq[:, 0:n], start=True, stop=True), reads=[bd1, csq], writes=[pm])
                            rsqrt_("act", crn[:, 0:n], pm[:, 0:n], [pm], [crn], crn[:, 0:n])
                            sc_ = 0.125 if cc < 4 else 1.0
                            s.op("dve", lambda e: e.scalar_tensor_tensor(out=cyn[:, 0:n], in0=cy[:, 0:n], scalar=sc_, in1=crn[:, 0:n], op0=ALU.mult, op1=ALU.mult),
                                 reads=[cy, crn], writes=[cyn])
                            s.op("dve", lambda e: e.tensor_copy(out=ynb[:, 0:n], in_=cyn[:, 0:n]), reads=[cyn], writes=[ynb])
                            dstT = qnT if cc < 4 else knT
                            s.dma(dstT[b, (cc % 4) * 128:(cc % 4 + 1) * 128, t0:t0 + n], ynb[:, 0:n], reads=[ynb], writes=[("qknT", b)])
                            src_tm = cyn
                        if cc >= 4:
                            pt_ = ps[4 + it % 4]
                            for sub in range(nsub):
                                s.op("pe", lambda e: e.transpose(pt_[:, sub * 128:(sub + 1) * 128], src_tm[:, sub * 128:(sub + 1) * 128], ident[:]), reads=[src_tm, ident], writes=[pt_])
                            s.op("act", lambda e: e.copy(out=tk[:, 0:nsub, :], in_=pt_[:, 0:n].rearrange("p (s c) -> p s c", c=128)), reads=[pt_], writes=[tk])
                            dtk = k_tok if cc < 8 else v_tok
                            s.dma(dtk[b, t0:t0 + n, (cc % 4) * 128:(cc % 4 + 1) * 128].rearrange("(s p) c -> p s c", p=128), tk[:, 0:nsub, :], reads=[tk], writes=[("kvtok", b)])
            s.barrier()
        if stop == "B":
            s.finish(); s.close(); return nc

        with contextlib.ExitStack() as ph:
            mk = [s.sbuf("mk%d" % i, [128, 128], stack=ph) for i in range(6)]
            for i in range(6):
                s.dma(mk[i][:], c_masks[i, :, :], writes=[mk[i]])
            lmk = [[s.sbuf("lmk%d_%d" % (a_, l_), [128, 128], BF16, stack=ph) for l_ in range(7)] for a_ in range(2)]
            lmst = s.sbuf("lmst", [128, 128], stack=ph)
            for a_ in range(2):
                for l_ in range(7):
                    s.dma(lmst[:], c_lmask[a_, l_, :, :], writes=[lmst])
                    s.op("act", lambda e: e.copy(out=lmk[a_][l_][:], in_=lmst[:]), reads=[lmst], writes=[lmk[a_][l_]])
            N0bl = [s.sbuf("N0b%d" % g_, [128, 512], BF16, stack=ph) for g_ in range(2)]
            Ptbl = [[s.sbuf("Ptb%d_%d" % (g_, i), [128, 512], BF16, stack=ph) for i in range(2)] for g_ in range(2)]
            Pbl = [s.sbuf("Pb%d" % g_, [128, 512], BF16, stack=ph) for g_ in range(2)]
            Zbl = [s.sbuf("Zb%d" % g_, [128, 512], BF16, stack=ph) for g_ in range(2)]
            notI = s.sbuf("notI", [128, 128], stack=ph)
            nones = s.sbuf("nones", [128, 128], stack=ph)
            s.op("dve", lambda e: e.tensor_scalar(out=notI[:], in0=ident[:], scalar1=-1.0, scalar2=1.0, op0=ALU.mult, op1=ALU.add), reads=[ident], writes=[notI])
            s.op("dve", lambda e: e.memset(nones[:], -1.0), writes=[nones])
            NC8 = NCH * 8
            gbt = s.sbuf("dgbt", [128, NCH, 32], stack=ph)
            gc = s.sbuf("dgc", [128, NC8], stack=ph)
            glb = s.sbuf("dglb", [128, NC8], stack=ph)
            egc = s.sbuf("degc", [128, NC8], stack=ph)
            egl = s.sbuf("degl", [128, NC8], stack=ph)
            eglmg = s.sbuf("deglmg", [128, NC8], stack=ph)
            nbt = s.sbuf("dnbt", [128, NC8], stack=ph)
            bet = s.sbuf("dbet", [128, NC8], stack=ph)
            begc = s.sbuf("dbegc", [128, NC8], stack=ph)
            Sf = s.sbuf("Sf", [64, 8, 64], stack=ph)
            Sb = s.sbuf("Sb", [64, 8, 64], BF16, stack=ph)
            qcb = [s.sbuf("qc%d" % i, [64, 8, 128], BF16, stack=ph) for i in range(2)]
            kcb = [s.sbuf("kc%d" % i, [64, 8, 128], BF16, stack=ph) for i in range(2)]
            ktb = [s.sbuf("kt%d" % i, [128, 512], stack=ph) for i in range(2)]
            vtb = [s.sbuf("vt%d" % i, [128, 512], stack=ph) for i in range(2)]
            vb_ = s.sbuf("vb", [128, 512], BF16, stack=ph)
            kbg = s.sbuf("kbg", [128, 512], BF16, stack=ph)
            kdec = s.sbuf("kdec", [128, 512], BF16, stack=ph)
            dg4l = [s.sbuf("dg4_%d" % i, [128, 512], stack=ph) for i in range(2)]
            ng4l = [s.sbuf("ng4_%d" % i, [128, 512], stack=ph) for i in range(2)]
            Dtl = [s.sbuf("Dt%d" % i, [128, 512], stack=ph) for i in range(2)]
            Dstl = [s.sbuf("Dst%d" % i, [128, 512], stack=ph) for i in range(2)]
            intral = [s.sbuf("intra%d" % i, [128, 512], BF16, stack=ph) for i in range(2)]
            intraT = s.sbuf("intraT", [128, 1024], BF16, stack=ph)
            Nkl = [[s.sbuf("Nk%d_%d" % (g_, i), [128, 512], stack=ph) for i in range(3)] for g_ in range(2)]
            Pml = [s.sbuf("Pm%d" % g_, [128, 512], stack=ph) for g_ in range(2)]
            Rtl = [s.sbuf("Rt%d" % g_, [128, 512], stack=ph) for g_ in range(2)]
            Qml = [s.sbuf("Qm%d" % g_, [128, 512], stack=ph) for g_ in range(2)]
            NkTl = [[s.sbuf("NkT%d_%d" % (g_, i), [128, 512], stack=ph) for i in range(2)] for g_ in range(2)]
            PTl = [[s.sbuf("PT%d_%d" % (g_, i), [128, 512], stack=ph) for i in range(2)] for g_ in range(2)]
            TTbl = [s.sbuf("TTb%d" % g_, [128, 512], BF16, stack=ph) for g_ in range(2)]
            wTs = s.sbuf("wTs", [64, 8, 128], BF16, stack=ph)
            us = s.sbuf("us", [128, 512], stack=ph)
            vnew = s.sbuf("vnew", [128, 512], BF16, stack=ph)
            tt = s.sbuf("dtt", [128, 512], stack=ph)
            otb = [s.sbuf("ot%d" % i, [128, 512], stack=ph) for i in range(2)]
            ncx = L // CH
            it = 0
            for b in range(NB):
                for dr in range(2):
                    tri, sel, bigm = mk[dr], mk[2 + dr], mk[4 + dr]
                    s.dma(gbt[:], gb[b, :, :].rearrange("(n p) c -> p n c", p=128), reads=[("gb", b)], writes=[gbt])
                    gd = gbt[:, :, 16 + dr * 8:24 + dr * 8]
                    bdv = gbt[:, :, dr * 8:dr * 8 + 8]
                    g3 = lambda t_: t_[:].rearrange("p (n h) -> p n h", h=8)
                    s.op("pe", lambda e: e.matmul(ps[0][:, 0:NC8].rearrange("p (n h) -> p n h", h=8), lhsT=tri[:], rhs=gd, start=True, stop=True), reads=[tri, gbt], writes=[ps[0]])
                    s.op("act", lambda e: e.copy(out=gc[:], in_=ps[0][:, 0:NC8]), reads=[ps[0]], writes=[gc])
                    s.op("pe", lambda e: e.matmul(ps[1][:, 0:NC8], lhsT=sel[:], rhs=gc[:], start=True, stop=True), reads=[sel, gc], writes=[ps[1]])
                    s.op("dve", lambda e: e.tensor_copy(out=glb[:], in_=ps[1][:, 0:NC8]), reads=[ps[1]], writes=[glb])
                    s.op("act", lambda e: e.activation(out=egc[:], in_=gc[:], func=AF.Exp), reads=[gc], writes=[egc])
                    s.op("act", lambda e: e.activation(out=egl[:], in_=glb[:], func=AF.Exp), reads=[glb], writes=[egl])
                    s.op("dve", lambda e: e.tensor_tensor(out=eglmg[:], in0=glb[:], in1=gc[:], op=ALU.subtract), reads=[glb, gc], writes=[eglmg])
                    s.op("act", lambda e: e.activation(out=eglmg[:], in_=eglmg[:], func=AF.Exp), reads=[eglmg], writes=[eglmg])
                    s.op("dve", lambda e: e.tensor_copy(out=g3(bet), in_=bdv), reads=[gbt], writes=[bet])
                    s.op("dve", lambda e: e.tensor_scalar(out=nbt[:], in0=bet[:], scalar1=-1.0, scalar2=None, op0=ALU.mult), reads=[bet], writes=[nbt])
                    s.op("dve", lambda e: e.tensor_tensor(out=begc[:], in0=bet[:], in1=egc[:], op=ALU.mult), reads=[bet, egc], writes=[begc])
                    tap("gc", gc, gc[:], [128, NC8]); tap("glb", glb, glb[:], [128, NC8]); tap("bet", bet, bet[:], [128, NC8])
                    s.op("dve", lambda e: e.memset(Sf[:], 0.0), writes=[Sf])
                    s.op("dve", lambda e: e.memset(Sb[:], 0.0), writes=[Sb])
                    order = list(range(NCH)) if dr == 0 else (list(range(ncx - 1, -1, -1)) + list(range(NCH - 1, ncx - 1, -1)))
                    for n_ in order:
                        c0 = n_ * 8
                        tk0 = n_ * CH
                        qc, kc, kt, vt, ot = qcb[it % 2], kcb[it % 2], ktb[it % 2], vtb[it % 2], otb[it % 2]
                        it += 1
                        s.dma(qc[:], qnT[b, :, tk0:tk0 + CH].rearrange("(h d) t -> d h t", d=64), reads=[("qknT", b)], writes=[qc])
                        s.dma(kc[:], knT[b, :, tk0:tk0 + CH].rearrange("(h d) t -> d h t", d=64), reads=[("qknT", b)], writes=[kc])
                        s.dma(kt[:], k_tok[b, tk0:tk0 + CH, :], reads=[("kvtok", b)], writes=[kt])
                        s.dma(vt[:], v_tok[b, tk0:tk0 + CH, :], reads=[("kvtok", b)], writes=[vt])
                        bc8 = lambda t_: t_[:, c0:c0 + 8].unsqueeze(2).broadcast_to([128, 8, 64])
                        v3 = lambda t_: t_[:].rearrange("p (h e) -> p h e", e=64)
                        s.op("dve", lambda e: e.tensor_tensor(out=v3(vb_), in0=v3(vt), in1=bc8(bet), op=ALU.mult), reads=[vt, bet], writes=[vb_])
                        s.op("dve", lambda e: e.tensor_tensor(out=v3(kbg), in0=v3(kt), in1=bc8(begc), op=ALU.mult), reads=[kt, begc], writes=[kbg])
                        s.op("dve", lambda e: e.tensor_tensor(out=v3(kdec), in0=v3(kt), in1=bc8(eglmg), op=ALU.mult), reads=[kt, eglmg], writes=[kdec])
                        d3 = lambda t_: t_[:].rearrange("p (h j) -> p h j", j=128)
                        pD, pE = ps[6], ps[7]
                        pEb = pE[:].bitcast(BF16)
                        bank = [(ps[0], ps[1], ps[2]), (ps[3], ps[4], ps[5])]
                        cur = [0, 0]
                        curT = [0, 0]
                        pcur = [0, 0]

                        def phase1(hg):
                            pA, pB_, pC = bank[hg]
                            Dt, Dst, intra = Dtl[hg], Dstl[hg], intral[hg]
                            Nk, NkT, PT = Nkl[hg], NkTl[hg], PTl[hg]
                            for hh in range(4):
                                h = hg * 4 + hh
                                s.op("pe", lambda e: e.matmul(pA[:, hh * 128:(hh + 1) * 128], lhsT=kc[:, h, :], rhs=kc[:, h, :], start=True, stop=True), reads=[kc], writes=[pA])
                            for hh in range(4):
                                h = hg * 4 + hh
                                s.op("pe", lambda e: e.matmul(pB_[:, hh * 128:(hh + 1) * 128], lhsT=qc[:, h, :], rhs=kc[:, h, :], start=True, stop=True), reads=[qc, kc], writes=[pB_])
                            dg4, ng4 = dg4l[hg], ng4l[hg]
                            gsl = gc[:, c0 + hg * 4:c0 + hg * 4 + 4].unsqueeze(2).broadcast_to([128, 4, 128])
                            s.op("dve", lambda e: e.tensor_tensor(out=d3(dg4), in0=ident[:].unsqueeze(1).broadcast_to([128, 4, 128]), in1=gsl, op=ALU.mult), reads=[ident, gc], writes=[dg4])
                            s.op("dve", lambda e: e.tensor_tensor(out=d3(ng4), in0=bigm[:].unsqueeze(1).broadcast_to([128, 4, 128]), in1=gsl, op=ALU.subtract), reads=[bigm, gc], writes=[ng4])
                            s.op("pe", lambda e: e.matmul(pC[:], lhsT=ones[:], rhs=dg4[:], start=True, stop=False), reads=[ones, dg4], writes=[pC])
                            s.op("pe", lambda e: e.matmul(pC[:], lhsT=ident[:], rhs=ng4[:], start=False, stop=True), reads=[ident, ng4], writes=[pC])
                            s.op("act", lambda e: e.activation(out=Dt[:], in_=pC[:], func=AF.Exp, scale=-1.0), reads=[pC], writes=[Dt])
                            s.op("dve", lambda e: e.tensor_tensor(out=intra[:], in0=pB_[:], in1=Dt[:], op=ALU.mult), reads=[pB_, Dt], writes=[intra])
                            s.op("dve", lambda e: e.tensor_tensor(out=d3(Dst), in0=d3(Dt), in1=notI[:].unsqueeze(1).broadcast_to([128, 4, 128]), op=ALU.mult), reads=[Dt, notI], writes=[Dst])
                            s.op("dve", lambda e: e.tensor_tensor(out=d3(Dst), in0=d3(Dst), in1=nbt[:, c0 + hg * 4:c0 + hg * 4 + 4].unsqueeze(2).broadcast_to([128, 4, 128]), op=ALU.mult),
                                 reads=[Dst, nbt], writes=[Dst])
                            s.op("dve", lambda e: e.tensor_tensor(out=Nk[2][:], in0=pA[:], in1=Dst[:], op=ALU.mult), reads=[pA, Dst], writes=[Nk[2]])
                            N0b, Ptb_, Pb, Zb = N0bl[hg], Ptbl[hg], Pbl[hg], Zbl[hg]
                            m0 = lmk[dr][0]
                            s.op("act", lambda e: e.copy(out=N0b[:], in_=Nk[2][:]), reads=[Nk[2]], writes=[N0b])
                            s.op("dve", lambda e: e.tensor_tensor(out=d3(Pb), in0=d3(Nk[2]), in1=m0[:].unsqueeze(1).broadcast_to([128, 4, 128]), op=ALU.mult), reads=[Nk[2], m0], writes=[Pb])
                            s.op("dve", lambda e: e.tensor_tensor(out=d3(Pb), in0=d3(Pb), in1=identb[:].unsqueeze(1).broadcast_to([128, 4, 128]), op=ALU.add), reads=[Pb, identb], writes=[Pb])
                            pDb = pD[:].bitcast(BF16)
                            for hh in range(4):
                                s.op("pe", lambda e: e.transpose(pDb[:, hh * 128:(hh + 1) * 128], Pb[:, hh * 128:(hh + 1) * 128], identb[:]), reads=[Pb, identb], writes=[pD])
                            for hh in range(4):
                                s.op("pe", lambda e: e.transpose(pEb[:, hh * 128:(hh + 1) * 128], intra[:, hh * 128:(hh + 1) * 128], identb[:]), reads=[intra, identb], writes=[pE])
                            s.op("act", lambda e: e.copy(out=Ptb_[0][:], in_=pDb[:, 0:512]), reads=[pD], writes=[Ptb_[0]])
                            s.op("act", lambda e: e.copy(out=intraT[:, hg * 512:(hg + 1) * 512], in_=pEb[:, 0:512]), reads=[pE], writes=[intraT])
                            cur[hg] = 0
                            pcur[hg] = 0

                        def nstep(hg, lv):
                            pA, pB_, pC = bank[hg]
                            N0b, Ptb_, Pb, Zb = N0bl[hg], Ptbl[hg], Pbl[hg], Zbl[hg]
                            PT = PTl[hg]
                            c_ = cur[hg]
                            X = Ptb_[c_]
                            mz = lmk[1 - dr][lv]
                            for hh in range(4):
                                sl = slice(hh * 128, (hh + 1) * 128)
                                s.op("pe", lambda e: e.matmul(pA[:, sl], lhsT=N0b[:, sl], rhs=X[:, sl], start=True, stop=True), reads=[N0b, X], writes=[pA])
                            s.op("dve", lambda e: e.tensor_tensor(out=d3(Zb), in0=pA[:].rearrange("p (h j) -> p h j", j=128), in1=mz[:].unsqueeze(1).broadcast_to([128, 4, 128]), op=ALU.mult),
                                 reads=[pA, mz], writes=[Zb])
                            for hh in range(4):
                                sl = slice(hh * 128, (hh + 1) * 128)
                                s.op("pe", lambda e: e.matmul(pB_[:, sl], lhsT=Pb[:, sl], rhs=Zb[:, sl], start=True, stop=True), reads=[Pb, Zb], writes=[pB_])
                            if lv < 6:
                                Xn = Ptb_[1 - c_]
                                s.op("dve", lambda e: e.tensor_tensor(out=Xn[:], in0=pB_[:], in1=X[:], op=ALU.add), reads=[pB_, X], writes=[Xn])
                                pCb = pC[:].bitcast(BF16)
                                for hh in range(4):
                                    sl = slice(hh * 128, (hh + 1) * 128)
                                    s.op("pe", lambda e: e.transpose(pCb[:, sl], Xn[:, sl], identb[:]), reads=[Xn, identb], writes=[pC])
                                s.op("act", lambda e: e.copy(out=Pb[:], in_=pCb[:, 0:512]), reads=[pC], writes=[Pb])
                                cur[hg] = 1 - c_
                            else:
                                s.op("dve", lambda e: e.tensor_tensor(out=PT[0][:], in0=pB_[:], in1=X[:], op=ALU.add), reads=[pB_, X], writes=[PT[0]])
                                pcur[hg] = 0

                        def tail(hg):
                            PT = PTl[hg]
                            TT = TTbl[hg]
                            pA, pB_, pC = bank[hg]
                            N0s = Nkl[hg][2]
                            Pm, Rt, Qm = Pml[hg], Rtl[hg], Qml[hg]
                            nsteps = cfg.get("newton", 0)
                            for ns_ in range(nsteps):
                                X = PT[pcur[hg]]
                                Xn = PT[1 - pcur[hg]]
                                for hh in range(4):
                                    sl = slice(hh * 128, (hh + 1) * 128)
                                    s.op("pe", lambda e: e.transpose(pA[:, sl], X[:, sl], ident[:]), reads=[X, ident], writes=[pA])
                                    s.op("pe", lambda e: e.matmul(pB_[:, sl], lhsT=N0s[:, sl], rhs=X[:, sl], start=True, stop=True), reads=[N0s, X], writes=[pB_])
                                s.op("act", lambda e: e.copy(out=Pm[:], in_=pA[:]), reads=[pA], writes=[Pm])
                                s.op("dve", lambda e: e.tensor_tensor(out=d3(Qm), in0=ident[:].unsqueeze(1).broadcast_to([128, 4, 128]), in1=d3(X), op=ALU.subtract), reads=[ident, X], writes=[Qm])
                                s.op("dve", lambda e: e.tensor_tensor(out=Rt[:], in0=pB_[:], in1=Qm[:], op=ALU.add), reads=[pB_, Qm], writes=[Rt])
                                for hh in range(4):
                                    sl = slice(hh * 128, (hh + 1) * 128)
                                    s.op("pe", lambda e: e.matmul(pC[:, sl], lhsT=Pm[:, sl], rhs=Rt[:, sl], start=True, stop=True), reads=[Pm, Rt], writes=[pC])
                                s.op("dve", lambda e: e.tensor_tensor(out=Xn[:], in0=pC[:], in1=X[:], op=ALU.add), reads=[pC, X], writes=[Xn])
                                pcur[hg] = 1 - pcur[hg]
                            s.op("act", lambda e: e.copy(out=TT[:], in_=PT[pcur[hg]][:]), reads=[PT[pcur[hg]]], writes=[TT])
                            pU = ps[6]
                            pW = bank[hg][2]
                            for hh in range(4):
                                h = hg * 4 + hh
                                sl = slice(hh * 128, (hh + 1) * 128)
                                s.op("pe", lambda e: e.matmul(pU[:, h * 64:(h + 1) * 64], lhsT=TT[:, sl], rhs=vb_[:, h * 64:(h + 1) * 64], start=True, stop=True), reads=[TT, vb_], writes=[pU])
                                s.op("pe", lambda e: e.matmul(pW[0:64, sl], lhsT=kbg[:, h * 64:(h + 1) * 64], rhs=TT[:, sl], start=True, stop=True), reads=[TT, kbg], writes=[pW])
                            s.op("act", lambda e: e.copy(out=wTs[:, hg * 4:(hg + 1) * 4, :], in_=pW[0:64, :].rearrange("p (h i) -> p h i", i=128)), reads=[pW], writes=[wTs])

                        for hg in range(2):
                            phase1(hg)
                        for lv in range(1, 7):
                            for hg in range(2):
                                nstep(hg, lv)
                        for hg in range(2):
                            tail(hg)
                        s.op("dve", lambda e: e.tensor_copy(out=us[:], in_=ps[6][:]), reads=[ps[6]], writes=[us])
                        tap("us", us, us[:], [128, 512]); tap("wTs", wTs, wTs[:], [64, 8, 128], BF16)
                        for h in range(8):
                            s.op("pe", lambda e: e.matmul(ps[0][:, h * 64:(h + 1) * 64], lhsT=wTs[:, h, :], rhs=Sb[:, h, :], start=True, stop=True), reads=[wTs, Sb], writes=[ps[0]])
                        s.op("dve", lambda e: e.tensor_tensor(out=vnew[:], in0=us[:], in1=ps[0][:], op=ALU.subtract), reads=[us, ps[0]], writes=[vnew])
                        for h in range(8):
                            s.op("pe", lambda e: e.matmul(ps[1][:, h * 64:(h + 1) * 64], lhsT=qc[:, h, :], rhs=Sb[:, h, :], start=True, stop=True), reads=[qc, Sb], writes=[ps[1]])
                        for h in range(8):
                            s.op("pe", lambda e: e.matmul(ps[2][:, h * 64:(h + 1) * 64], lhsT=intraT[:, h * 128:(h + 1) * 128], rhs=vnew[:, h * 64:(h + 1) * 64], start=True, stop=True),
                                 reads=[intraT, vnew], writes=[ps[2]])
                        for h in range(8):
                            s.op("pe", lambda e: e.matmul(ps[3][0:64, h * 64:(h + 1) * 64], lhsT=kdec[:, h * 64:(h + 1) * 64], rhs=vnew[:, h * 64:(h + 1) * 64], start=True, stop=True),
                                 reads=[kdec, vnew], writes=[ps[3]])
                        s.op("dve", lambda e: e.tensor_tensor(out=v3(tt), in0=ps[1][:].rearrange("p (h e) -> p h e", e=64), in1=bc8(egc), op=ALU.mult), reads=[ps[1], egc], writes=[tt])
                        s.op("dve", lambda e: e.tensor_tensor(out=ot[:], in0=tt[:], in1=ps[2][:], op=ALU.add), reads=[tt, ps[2]], writes=[ot])
                        tap("ot", ot, ot[:], [128, 512]); tap("vnew", vnew, vnew[:], [128, 512], BF16)
                        s.dma(o_dir[dr][b, tk0:tk0 + CH, :], ot[:], reads=[ot], writes=[("o_dir", b)])
                        s.op("dve", lambda e: e.tensor_tensor(out=Sf[:], in0=Sf[:], in1=egl[0:64, c0:c0 + 8].unsqueeze(2).broadcast_to([64, 8, 64]), op=ALU.mult), reads=[Sf, egl], writes=[Sf])
                        s.op("dve", lambda e: e.tensor_tensor(out=Sf[:], in0=Sf[:], in1=ps[3][0:64, :].rearrange("p (h e) -> p h e", e=64), op=ALU.add), reads=[Sf, ps[3]], writes=[Sf])
                        s.op("act", lambda e: e.copy(out=Sb[:], in_=Sf[:]), reads=[Sf], writes=[Sb])
            s.barrier()
        if stop == "DN":
            s.finish(); s.close(); return nc

        with contextlib.ExitStack() as ph:
            wo_a = s.sbuf("wo_a", [64, 8, D], BF16, stack=ph)
            wo_d = s.sbuf("wo_d", [128, 4, D], BF16, stack=ph)
            wst = [s.sbuf("wos%d" % i, [128, D], stack=ph) for i in range(2)]
            for i in range(12):
                st = wst[i % 2]
                if i < 8:
                    s.dma(st[0:64, :], w_out[l, i * 64:(i + 1) * 64, :], writes=[st])
                    s.op("act" if i % 2 else "dve", (lambda e: e.copy(out=wo_a[:, i, :], in_=st[0:64, :])) if i % 2 else (lambda e: e.tensor_copy(out=wo_a[:, i, :], in_=st[0:64, :])),
                         reads=[st], writes=[wo_a])
                else:
                    c = i - 8
                    s.dma(st[:, :], w_out[l, 512 + c * 128:512 + (c + 1) * 128, :], writes=[st])
                    s.op("act" if i % 2 else "dve", (lambda e: e.copy(out=wo_d[:, c, :], in_=st[:, :])) if i % 2 else (lambda e: e.tensor_copy(out=wo_d[:, c, :], in_=st[:, :])),
                         reads=[st], writes=[wo_d])
            wr = s.sbuf("wr", [128, 8, 36], stack=ph)
            rbb = s.sbuf("rbb", [128, 36], stack=ph)
            dnw = s.sbuf("dnw", [128, 64], stack=ph)
            s.dma(wr[:], wr_in[l, :, :].rearrange("(kc p) n -> p kc n", p=128), writes=[wr])
            s.dma(rbb[:], rb_in[l:l + 1, :].partition_broadcast(128), writes=[rbb])
            s.dma(dnw[:], dn_norm_w[l:l + 1, :].partition_broadcast(128), writes=[dnw])
            ofb = [s.sbuf("of%d" % i, [128, 4, 512], stack=ph) for i in range(2)]
            obb = [s.sbuf("ob%d" % i, [128, 4, 512], stack=ph) for i in range(2)]
            gsb = [s.sbuf("gs%d" % i, [128, 4, 512], stack=ph) for i in range(2)]
            atb = [s.sbuf("at%d" % i, [64, 8, 512], BF16, stack=ph) for i in range(2)]
            xtb = [s.sbuf("mxt%d" % i, [128, 8, 512], stack=ph) for i in range(2)]
            ss = s.sbuf("mss", [128, 32], stack=ph)
            dnT = s.sbuf("dnT", [128, 4, 512], BF16, stack=ph)
            sqb = s.sbuf("msq", [128, 8, 512], stack=ph)
            rstd = s.sbuf("mrstd", [128, 512], stack=ph)
            h2b = s.sbuf("h2b", [128, 8, 512], BF16, stack=ph)
            lg = s.sbuf("lg", [128, 4, 36], stack=ph)
            gmx = s.sbuf("gmx", [128, 4], stack=ph)
            oh = s.sbuf("oh", [128, 4, 4], stack=ph)
            eg = s.sbuf("eg", [128, 4, 4], stack=ph)
            sg = s.sbuf("sg", [128, 4], stack=ph)
            ml = s.sbuf("ml", [128, 4, 32], stack=ph)
            top8 = s.sbuf("top8", [128, 4, 8], stack=ph)
            selt = s.sbuf("selt", [128, 4, 32], stack=ph)
            ex = s.sbuf("ex", [128, 4, 32], stack=ph)
            se = s.sbuf("se", [128, 4], stack=ph)
            wts = s.sbuf("wts", [32, 512], stack=ph)
            it = 0
            for b in range(NB):
                for (t0, n) in tiles:
                    j = NB if t0 < L else b
                    nsub = n // 128
                    of_, ob_, gs_, at_, xt = ofb[it % 2], obb[it % 2], gsb[it % 2], atb[it % 2], xtb[it % 2]
                    it += 1
                    tm = lambda ap_: ap_.rearrange("(s p) f -> p s f", p=128)
                    s.dma(of_[:, 0:nsub, :], tm(o_dir[0][b, t0:t0 + n, :]), reads=[("o_dir", b)], writes=[of_])
                    s.dma(ob_[:, 0:nsub, :], tm(o_dir[1][b, t0:t0 + n, :]), reads=[("o_dir", b)], writes=[ob_])
                    s.dma(gs_[:, 0:nsub, :], tm(gate_s[b, t0:t0 + n, :]), reads=[("gate_s", b, t0)], writes=[gs_])
                    s.dma(at_[:, :, 0:n], attnT[b, :, :, t0:t0 + n].rearrange("h d t -> d h t"), reads=[("attnT", b)], writes=[at_])
                    s.dma(xt[:, :, 0:n], xT[b, :, t0:t0 + n].rearrange("(c p) t -> p c t", p=128), reads=[("xT", b, t0)], writes=[xt])
                    o4 = lambda t_: t_[:, 0:nsub, :].rearrange("p s (h e) -> p (s h) e", e=64)
                    s.op("dve", lambda e: e.tensor_tensor(out=of_[:, 0:nsub, :], in0=of_[:, 0:nsub, :], in1=ob_[:, 0:nsub, :], op=ALU.add), reads=[of_, ob_], writes=[of_])
                    s.op("act", lambda e: e.activation(out=ob_[:, 0:nsub, :], in_=of_[:, 0:nsub, :], func=AF.Square), reads=[of_], writes=[ob_])
                    s.op("dve", lambda e: e.tensor_reduce(out=ss[:, 0:nsub * 8], in_=o4(ob_), axis=AX.X, op=ALU.add), reads=[ob_], writes=[ss])
                    rsqrt_("act", ss[:, 0:nsub * 8], ss[:, 0:nsub * 8], [ss], [ss], ss[:, 0:nsub * 8], scale=1.0 / 64)
                    s.op("dve", lambda e: e.tensor_tensor(out=o4(of_), in0=o4(of_), in1=ss[:, 0:nsub * 8].unsqueeze(2).broadcast_to([128, nsub * 8, 64]), op=ALU.mult), reads=[of_, ss], writes=[of_])
                    s.op("dve", lambda e: e.tensor_tensor(out=o4(of_), in0=o4(of_), in1=dnw[:].unsqueeze(1).broadcast_to([128, nsub * 8, 64]), op=ALU.mult), reads=[of_, dnw], writes=[of_])
                    s.op("dve", lambda e: e.tensor_tensor(out=of_[:, 0:nsub, :], in0=of_[:, 0:nsub, :], in1=gs_[:, 0:nsub, :], op=ALU.mult), reads=[of_, gs_], writes=[of_])
                    for c in range(4):
                        pT_ = ps[c % 2]
                        for sub in range(nsub):
                            s.op("pe", lambda e: e.transpose(pT_[:, sub * 128:(sub + 1) * 128], of_[:, sub, c * 128:(c + 1) * 128], ident[:]), reads=[of_, ident], writes=[pT_])
                        if c % 2:
                            s.op("act", lambda e: e.copy(out=dnT[:, c, 0:n], in_=pT_[:, 0:n]), reads=[pT_], writes=[dnT])
                        else:
                            s.op("dve", lambda e: e.tensor_copy(out=dnT[:, c, 0:n], in_=pT_[:, 0:n]), reads=[pT_], writes=[dnT])
                    for oc in range(8):
                        py = ps[2 + oc % 4]
                        osl = slice(oc * 128, (oc + 1) * 128)
                        for h in range(8):
                            s.op("pe", lambda e: e.matmul(py[:, 0:n], lhsT=wo_a[0:64, h, osl], rhs=at_[0:64, h, 0:n], start=(h == 0), stop=False), reads=[wo_a, at_], writes=[py])
                        for c in range(4):
                            s.op("pe", lambda e: e.matmul(py[:, 0:n], lhsT=wo_d[:, c, osl], rhs=dnT[:, c, 0:n], start=False, stop=(c == 3)), reads=[wo_d, dnT], writes=[py])
                        s.op("dve", lambda e: e.scalar_tensor_tensor(out=xt[:, oc, 0:n], in0=py[:, 0:n], scalar=mod[l][:, 16 + oc, j:j + 1], in1=xt[:, oc, 0:n], op0=ALU.mult, op1=ALU.add),
                             reads=[py, mod[l], xt], writes=[xt])
                    s.dma(xT[b, :, t0:t0 + n].rearrange("(c p) t -> p c t", p=128), xt[:, :, 0:n], reads=[xt], writes=[("xT", b, t0)])
                    s.op("act", lambda e: e.activation(out=sqb[:, :, 0:n], in_=xt[:, :, 0:n], func=AF.Square), reads=[xt], writes=[sqb])
                    for c in range(8):
                        s.op("pe", lambda e: e.matmul(ps[6][:, 0:n], lhsT=onesm[:], rhs=sqb[:, c, 0:n], start=(c == 0), stop=(c == 7)), reads=[onesm, sqb], writes=[ps[6]])
                    rsqrt_("act", rstd[:, 0:n], ps[6][:, 0:n], [ps[6]], [rstd], rstd[:, 0:n])
                    s.op("dve", lambda e: e.tensor_tensor(out=sqb[:, :, 0:n], in0=xt[:, :, 0:n], in1=rstd[:, 0:n].unsqueeze(1).broadcast_to([128, 8, n]), op=ALU.mult),
                         reads=[xt, rstd], writes=[sqb])
                    for c in range(8):
                        s.op("dve", lambda e: e.tensor_scalar(out=sqb[:, c, 0:n], in0=sqb[:, c, 0:n], scalar1=affn[l][:, c, j:j + 1], scalar2=mod[l][:, 24 + c, j:j + 1],
                                                              op0=ALU.mult, op1=ALU.add), reads=[sqb, affn[l], mod[l]], writes=[sqb])
                    s.op("act", lambda e: e.copy(out=h2b[:, :, 0:n], in_=sqb[:, :, 0:n]), reads=[sqb], writes=[h2b])
                    s.dma(h2T[b, :, t0:t0 + n].rearrange("(c p) t -> p c t", p=128), h2b[:, :, 0:n], reads=[h2b], writes=[("h2T", b)])
                    pl_ = ps[7]
                    for sub in range(nsub):
                        for kc in range(8):
                            s.op("pe", lambda e: e.matmul(pl_[:, sub * 36:(sub + 1) * 36], lhsT=sqb[:, kc, sub * 128:(sub + 1) * 128], rhs=wr[:, kc, :], start=(kc == 0), stop=(kc == 7)),
                                 reads=[sqb, wr], writes=[pl_])
                    L3 = lg[:, 0:nsub, :]
                    s.op("dve", lambda e: e.tensor_tensor(out=L3, in0=pl_[:, 0:nsub * 36].rearrange("p (s c) -> p s c", c=36), in1=rbb[:].unsqueeze(1).broadcast_to([128, nsub, 36]), op=ALU.add),
                         reads=[pl_, rbb], writes=[lg])
                    s.op("dve", lambda e: e.tensor_reduce(out=gmx[:, 0:nsub], in_=lg[:, 0:nsub, 0:4], axis=AX.X, op=ALU.max), reads=[lg], writes=[gmx])
                    gmb = gmx[:, 0:nsub].unsqueeze(2).broadcast_to([128, nsub, 4])
                    s.op("dve", lambda e: e.tensor_tensor(out=oh[:, 0:nsub, :], in0=lg[:, 0:nsub, 0:4], in1=gmb, op=ALU.is_equal), reads=[lg, gmx], writes=[oh])
                    s.op("dve", lambda e: e.tensor_tensor(out=eg[:, 0:nsub, :], in0=lg[:, 0:nsub, 0:4], in1=gmb, op=ALU.subtract), reads=[lg, gmx], writes=[eg])
                    s.op("act", lambda e: e.activation(out=eg[:, 0:nsub, :], in_=eg[:, 0:nsub, :], func=AF.Exp), reads=[eg], writes=[eg])
                    s.op("dve", lambda e: e.tensor_reduce(out=sg[:, 0:nsub], in_=eg[:, 0:nsub, :], axis=AX.X, op=ALU.add), reads=[eg], writes=[sg])
                    s.op("dve", lambda e: e.tensor_scalar(out=oh[:, 0:nsub, :], in0=oh[:, 0:nsub, :], scalar1=1.0e30, scalar2=-1.0e30, op0=ALU.mult, op1=ALU.add), reads=[oh], writes=[oh])
                    s.op("dve", lambda e: e.tensor_tensor(out=ml[:, 0:nsub, :].rearrange("p s (g x) -> p s g x", x=8), in0=lg[:, 0:nsub, 4:36].rearrange("p s (g x) -> p s g x", x=8),
                                                          in1=oh[:, 0:nsub, :].unsqueeze(3).broadcast_to([128, nsub, 4, 8]), op=ALU.add), reads=[lg, oh], writes=[ml])
                    for sub in range(nsub):
                        s.op("dve", lambda e: e.max(out=top8[:, sub, :], in_=ml[:, sub, :]), reads=[ml], writes=[top8])
                    s.op("dve", lambda e: e.tensor_tensor(out=selt[:, 0:nsub, :], in0=ml[:, 0:nsub, :], in1=top8[:, 0:nsub, 1:2].broadcast_to([128, nsub, 32]), op=ALU.is_ge), reads=[ml, top8], writes=[selt])
                    s.op("dve", lambda e: e.tensor_tensor(out=ex[:, 0:nsub, :], in0=ml[:, 0:nsub, :], in1=top8[:, 0:nsub, 0:1].broadcast_to([128, nsub, 32]), op=ALU.subtract), reads=[ml, top8], writes=[ex])
                    s.op("act", lambda e: e.activation(out=ex[:, 0:nsub, :], in_=ex[:, 0:nsub, :], func=AF.Exp), reads=[ex], writes=[ex])
                    s.op("dve", lambda e: e.tensor_tensor(out=ex[:, 0:nsub, :], in0=ex[:, 0:nsub, :], in1=selt[:, 0:nsub, :], op=ALU.mult), reads=[ex, selt], writes=[ex])
                    s.op("dve", lambda e: e.tensor_reduce(out=se[:, 0:nsub], in_=ex[:, 0:nsub, :], axis=AX.X, op=ALU.add), reads=[ex], writes=[se])
                    s.op("dve", lambda e: e.tensor_tensor(out=se[:, 0:nsub], in0=se[:, 0:nsub], in1=sg[:, 0:nsub], op=ALU.mult), reads=[se, sg], writes=[se])
                    s.op("dve", lambda e: e.reciprocal(out=se[:, 0:nsub], in_=se[:, 0:nsub]), reads=[se], writes=[se])
                    s.op("dve", lambda e: e.tensor_tensor(out=ex[:, 0:nsub, :], in0=ex[:, 0:nsub, :], in1=se[:, 0:nsub].unsqueeze(2).broadcast_to([128, nsub, 32]), op=ALU.mult), reads=[ex, se], writes=[ex])
                    pw_ = ps[0]
                    for sub in range(nsub):
                        s.op("pe", lambda e: e.transpose(pw_[0:32, sub * 128:(sub + 1) * 128], ex[:, sub, :], ident[:]), reads=[ex, ident], writes=[pw_])
                    s.op("act", lambda e: e.copy(out=wts[:, 0:n], in_=pw_[0:32, 0:n]), reads=[pw_], writes=[wts])
                    s.dma(WtT[b, :, t0:t0 + n], wts[:, 0:n], reads=[wts], writes=[("WtT", b)])
            s.barrier()
        if stop == "M":
            s.finish(); s.close(); return nc

        half = (len(tiles) + 1) // 2
        groups = [tiles[:half], tiles[half:]]
        GMAX = max(sum(n for (_, n) in g) for g in groups)
        with contextlib.ExitStack() as ph:
            h2g = s.sbuf("h2g", [128, 8, GMAX], BF16, stack=ph)
            acc = s.sbuf("eacc", [128, 8, GMAX], stack=ph)
            for b in range(NB):
                for grp in groups:
                    g0 = grp[0][0]
                    G = sum(n for (_, n) in grp)
                    s.dma(h2g[:, :, 0:G], h2T[b, :, g0:g0 + G].rearrange("(c p) t -> p c t", p=128), reads=[("h2T", b)], writes=[h2g])
                    with contextlib.ExitStack() as ph2:
                        stg = [s.sbuf("stg%d" % i, [128, 2048], stack=ph2) for i in range(2)]
                        w1b = [s.sbuf("w1b%d" % i, [128, 8, FF], BF16, stack=ph2) for i in range(2)]
                        w3b = [s.sbuf("w3b%d" % i, [128, 8, FF], BF16, stack=ph2) for i in range(2)]
                        w2b = [s.sbuf("w2b%d" % i, [128, 2, D], BF16, stack=ph2) for i in range(2)]
                        wbc = [s.sbuf("wbc%d" % i, [128, GMAX], stack=ph2) for i in range(2)]
                        hhb2 = [[s.sbuf("hhc%d_%d" % (i, k_), [128, 512], BF16, stack=ph2) for k_ in range(2)] for i in range(2)]
                        eel = [s.sbuf("eel%d" % i, [128, 512], stack=ph2) for i in range(2)]
                        hhl = [s.sbuf("hhl%d" % i, [128, 512], stack=ph2) for i in range(2)]
                        sicnt = [0]

                        def load_w(ex_):
                            st_ = ex_ % 2
                            for (dstw, srcw) in ((w1b[st_], w1_in[l, ex_, :, :].rearrange("(kc p) f -> p kc f", p=128)),
                                                 (w3b[st_], w3_in[l, ex_, :, :].rearrange("(kc p) f -> p kc f", p=128)),
                                                 (w2b[st_], w2_in[l, ex_, :, :].rearrange("(fc p) n -> p fc n", p=128))):
                                sg_ = stg[sicnt[0] % 2]
                                sicnt[0] += 1
                                a_ = dstw.t.shape[1]
                                s.dma(sg_[:].rearrange("p (a b) -> p a b", a=a_), srcw, writes=[sg_])
                                s.op("act", lambda e: e.copy(out=dstw[:], in_=sg_[:].rearrange("p (a b) -> p a b", a=a_)), reads=[sg_], writes=[dstw])
                            s.dma(wbc[st_][:, 0:G], WtT[b, ex_:ex_ + 1, g0:g0 + G].partition_broadcast(128), reads=[("WtT", b)], writes=[wbc[st_]])

                        items = [(ex_, ti) for ex_ in range(NEXP) for ti in range(len(grp))]

                        def stage1(idx):
                            ex_, ti = items[idx]
                            st_ = ex_ % 2
                            t0, n = grp[ti]
                            u0 = t0 - g0
                            for fc in range(2):
                                pa, pb = ps[fc * 2], ps[fc * 2 + 1]
                                ee, hh = eel[fc], hhl[fc]
                                for kc in range(8):
                                    s.op("pe", lambda e: e.matmul(pa[:, 0:n], lhsT=w1b[st_][:, kc, fc * 128:(fc + 1) * 128], rhs=h2g[:, kc, u0:u0 + n], start=(kc == 0), stop=(kc == 7)),
                                         reads=[w1b[st_], h2g], writes=[pa])
                                for kc in range(8):
                                    s.op("pe", lambda e: e.matmul(pb[:, 0:n], lhsT=w3b[st_][:, kc, fc * 128:(fc + 1) * 128], rhs=h2g[:, kc, u0:u0 + n], start=(kc == 0), stop=(kc == 7)),
                                         reads=[w3b[st_], h2g], writes=[pb])
                                s.op("act", lambda e: e.activation(out=ee[:, 0:n], in_=pa[:, 0:n], func=AF.Exp, scale=-1.0), reads=[pa], writes=[ee])
                                s.op("act", lambda e: e.activation(out=ee[:, 0:n], in_=ee[:, 0:n], func=AF.Ln, bias=1.0), reads=[ee], writes=[ee])
                                s.op("act", lambda e: e.activation(out=ee[:, 0:n], in_=ee[:, 0:n], func=AF.Exp, scale=-1.0), reads=[ee], writes=[ee])
                                s.op("dve", lambda e: e.tensor_tensor(out=hh[:, 0:n], in0=pa[:, 0:n], in1=ee[:, 0:n], op=ALU.mult), reads=[pa, ee], writes=[hh])
                                s.op("dve", lambda e: e.tensor_tensor(out=hh[:, 0:n], in0=pb[:, 0:n], in1=hh[:, 0:n], op=ALU.mult), reads=[pb, hh], writes=[hh])
                                s.op("dve", lambda e: e.tensor_tensor(out=hhb2[idx % 2][fc][:, 0:n], in0=hh[:, 0:n], in1=wbc[st_][:, u0:u0 + n], op=ALU.mult),
                                     reads=[hh, wbc[st_]], writes=[hhb2[idx % 2][fc]])

                        def stage2(idx):
                            ex_, ti = items[idx]
                            st_ = ex_ % 2
                            t0, n = grp[ti]
                            u0 = t0 - g0
                            for oc in range(8):
                                py = ps[4 + oc % 4]
                                for fc in range(2):
                                    s.op("pe", lambda e: e.matmul(py[:, 0:n], lhsT=w2b[st_][:, fc, oc * 128:(oc + 1) * 128], rhs=hhb2[idx % 2][fc][:, 0:n], start=(fc == 0), stop=(fc == 1)),
                                         reads=[w2b[st_], hhb2[idx % 2][fc]], writes=[py])
                                if ex_ == 0:
                                    s.op("act", lambda e: e.copy(out=acc[:, oc, u0:u0 + n], in_=py[:, 0:n]), reads=[py], writes=[acc])
                                else:
                                    s.op("dve", lambda e: e.tensor_tensor(out=acc[:, oc, u0:u0 + n], in0=py[:, 0:n], in1=acc[:, oc, u0:u0 + n], op=ALU.add), reads=[py, acc], writes=[acc])

                        load_w(0)
                        for idx in range(len(items) + 1):
                            if idx < len(items):
                                stage1(idx)
                            if idx >= 1:
                                stage2(idx - 1)
                            if idx < len(items) and items[idx][1] == 0 and items[idx][0] + 1 < NEXP:
                                load_w(items[idx][0] + 1)
                        s.barrier()
                    with contextlib.ExitStack() as ph2:
                        xtb = [s.sbuf("ext%d" % i, [128, 8, 512], stack=ph2) for i in range(2)]
                        for ti, (t0, n) in enumerate(grp):
                            j = NB if t0 < L else b
                            u0 = t0 - g0
                            xt = xtb[ti % 2]
                            s.dma(xt[:, :, 0:n], xT[b, :, t0:t0 + n].rearrange("(c p) t -> p c t", p=128), reads=[("xT", b, t0)], writes=[xt])
                            for oc in range(8):
                                s.op("dve", lambda e: e.scalar_tensor_tensor(out=xt[:, oc, 0:n], in0=acc[:, oc, u0:u0 + n], scalar=mod[l][:, 40 + oc, j:j + 1], in1=xt[:, oc, 0:n],
                                                                             op0=ALU.mult, op1=ALU.add), reads=[acc, mod[l], xt], writes=[xt])
                            s.dma(xT[b, :, t0:t0 + n].rearrange("(c p) t -> p c t", p=128), xt[:, :, 0:n], reads=[xt], writes=[("xT", b, t0)])
                        s.barrier()
        if stop == "E":
            s.finish(); s.close(); return nc

    with contextlib.ExitStack() as ph:
        fnw = s.sbuf("fnw", [128, 8], stack=ph)
        s.dma(fnw[:], fnwT[:, :], writes=[fnw])
        xtb = [s.sbuf("fxt%d" % i, [128, 8, 512], stack=ph) for i in range(2)]
        sqb = s.sbuf("fsq", [128, 8, 512], stack=ph)
        rstd = s.sbuf("frstd", [128, 512], stack=ph)
        otb = [s.sbuf("fot%d" % i, [128, 4, D], stack=ph) for i in range(2)]
        it = 0
        for b in range(NB):
            for (t0, n) in tiles:
                if t0 < L:
                    continue
                nsub = n // 128
                xt, ot = xtb[it % 2], otb[it % 2]
                it += 1
                s.dma(xt[:, :, 0:n], xT[b, :, t0:t0 + n].rearrange("(c p) t -> p c t", p=128), reads=[("xT", b, t0)], writes=[xt])
                s.op("act", lambda e: e.activation(out=sqb[:, :, 0:n], in_=xt[:, :, 0:n], func=AF.Square), reads=[xt], writes=[sqb])
                for c in range(8):
                    s.op("pe", lambda e: e.matmul(ps[0][:, 0:n], lhsT=onesm[:], rhs=sqb[:, c, 0:n], start=(c == 0), stop=(c == 7)), reads=[onesm, sqb], writes=[ps[0]])
                rsqrt_("act", rstd[:, 0:n], ps[0][:, 0:n], [ps[0]], [rstd], rstd[:, 0:n])
                s.op("dve", lambda e: e.tensor_tensor(out=sqb[:, :, 0:n], in0=xt[:, :, 0:n], in1=rstd[:, 0:n].unsqueeze(1).broadcast_to([128, 8, n]), op=ALU.mult), reads=[xt, rstd], writes=[sqb])
                s.op("dve", lambda e: e.tensor_tensor(out=sqb[:, :, 0:n], in0=sqb[:, :, 0:n], in1=fnw[:].unsqueeze(2).broadcast_to([128, 8, n]), op=ALU.mult), reads=[sqb, fnw], writes=[sqb])
                for sub in range(nsub):
                    for hf in range(2):
                        pT_ = ps[1 + (sub * 2 + hf) % 4]
                        for c4 in range(4):
                            c = hf * 4 + c4
                            s.op("pe", lambda e: e.transpose(pT_[:, c4 * 128:(c4 + 1) * 128], sqb[:, c, sub * 128:(sub + 1) * 128], ident[:]), reads=[sqb, ident], writes=[pT_])
                        if hf:
                            s.op("act", lambda e: e.copy(out=ot[:, sub, hf * 512:(hf + 1) * 512], in_=pT_[:, :]), reads=[pT_], writes=[ot])
                        else:
                            s.op("dve", lambda e: e.tensor_copy(out=ot[:, sub, hf * 512:(hf + 1) * 512], in_=pT_[:, :]), reads=[pT_], writes=[ot])
                s.dma(out_hbm[b, t0 - L:t0 - L + n, :].rearrange("(s p) f -> p s f", p=128), ot[:, 0:nsub, :], reads=[ot])
    s.finish()
    s.close()
    return nc


def _partner():
    d = np.arange(64)
    return np.where((d % 32) < 16, d + 16, d - 16)


def host_consts(S, L):
    T = S + L
    c = {}
    c["c_ident"] = np.eye(128, dtype=np.float32)
    bd = np.zeros((128, 128), np.float32)
    bd[:64, :64] = 1.0
    bd[64:, 64:] = 1.0
    c["c_bd"] = bd
    d = np.arange(128) % 64
    axis = d // 32
    f = d % 16
    inv = (10000.0 ** (-(np.arange(16, dtype=np.float32)) / 16.0)).astype(np.float32)
    tl = np.arange(S)
    pos = np.stack([(tl // 64).astype(np.float32), (tl % 64).astype(np.float32)], 0)
    ang = pos[axis, :] * inv[f][:, None]
    cos = np.ones((128, T), np.float32)
    sin = np.zeros((128, T), np.float32)
    cos[:, L:] = np.cos(ang.astype(np.float32))
    sgn = np.where((d % 32) < 16, -1.0, 1.0).astype(np.float32)
    sin[:, L:] = np.sin(ang.astype(np.float32)) * sgn[:, None]
    c["c_cos"] = cos
    c["c_sin"] = sin
    p = np.arange(128)[:, None]
    i = np.arange(128)[None, :]
    m = np.zeros((6, 128, 128), np.float32)
    m[0] = (p <= i)
    m[1] = (p >= i)
    m[2] = (p == 127)
    m[3] = (p == 0)
    m[4] = np.where(i > p, BIG, 0.0)
    m[5] = np.where(i < p, BIG, 0.0)
    c["c_masks"] = m
    lm = np.zeros((2, 7, 128, 128), np.float32)
    for l_ in range(7):
        f = ((p >> (l_ + 1)) == (i >> (l_ + 1))) & (((p >> l_) & 1) == 1) & (((i >> l_) & 1) == 0)
        lm[0, l_] = f
        lm[1, l_] = f.T
    c["c_lmask"] = lm
    return c


def host_weights(inp):
    DEPTH = inp["w_in"].shape[0]
    o = {}
    o["ada_w"] = np.ascontiguousarray(inp["ada_w"])
    o["ada_bT"] = np.ascontiguousarray(inp["ada_b"].reshape(DEPTH, 48, 128).transpose(0, 2, 1))
    o["nmixT"] = np.ascontiguousarray(inp["norm_mix_w"].reshape(DEPTH, 8, 128).transpose(0, 2, 1))
    o["nffnT"] = np.ascontiguousarray(inp["norm_ffn_w"].reshape(DEPTH, 8, 128).transpose(0, 2, 1))
    o["fnwT"] = np.ascontiguousarray(inp["final_norm_w"].reshape(8, 128).T)
    w = inp["w_in"]
    pt = _partner()
    ext = np.empty((DEPTH, D, WEXT), np.float32)
    ext[:, :, CQ:WEXT - 640] = w[:, :, 0:2848]
    qcols = np.empty(512, np.int64)
    rqcols = np.empty(512, np.int64)
    for c in range(4):
        for two in range(2):
            h = two * 4 + c
            qcols[c * 128 + two * 64:c * 128 + two * 64 + 64] = h * 64 + np.arange(64)
            rqcols[c * 128 + two * 64:c * 128 + two * 64 + 64] = h * 64 + pt
    ext[:, :, CQ:CQ + 512] = w[:, :, qcols]
    ext[:, :, CRQ:CRQ + 512] = w[:, :, rqcols]
    rk = np.concatenate([512 + pt, 512 + 64 + pt])
    ext[:, :, CRK:CRK + 128] = w[:, :, rk]
    o["w_in_ext"] = ext
    qw, kw = inp["q_norm_w"], inp["k_norm_w"]
    dd = np.arange(128) % 64
    o["qkw"] = np.ascontiguousarray(np.stack([qw[:, dd], qw[:, pt[dd]], kw[:, dd], kw[:, pt[dd]]], -1))
    o["qkw_row"] = np.ascontiguousarray(np.stack([qw, kw], 1))
    o["conv_wT"] = np.ascontiguousarray(inp["conv_w"].reshape(DEPTH, 5, 12, 128).transpose(0, 3, 2, 1))
    o["dn_A_log"] = np.ascontiguousarray(inp["dn_A_log"].reshape(DEPTH, 16))
    o["dn_dt_bias"] = np.ascontiguousarray(inp["dn_dt_bias"].reshape(DEPTH, 16))
    o["dn_norm_w"] = np.ascontiguousarray(inp["dn_norm_w"])
    o["w_out"] = np.ascontiguousarray(inp["w_out"])
    o["wr"] = np.ascontiguousarray(np.concatenate([inp["rg_w"], inp["re_w"]], -1))
    o["rb"] = np.ascontiguousarray(np.concatenate([inp["rg_b"], inp["re_b"]], -1))
    o["w1"] = np.ascontiguousarray(inp["w1"])
    o["w3"] = np.ascontiguousarray(inp["w3"])
    o["w2"] = np.ascontiguousarray(inp["w2"])
    return o


def host_core_inputs(inp, core, NB):
    b0 = core * NB
    o = {}
    o["x"] = np.ascontiguousarray(inp["x"][b0:b0 + NB])
    o["ctx"] = np.ascontiguousarray(inp["ctx"][b0:b0 + NB])
    vecs = [inp["c"][b0 + j] for j in range(NB)] + [inp["c_ctx"]]
    o["cT"] = np.ascontiguousarray(np.stack(vecs, -1).reshape(8, 128, NB + 1).transpose(1, 0, 2))
    return o


def kernel(**inputs):
    inputs = {k: np.asarray(v, dtype=np.float32) for k, v in inputs.items()}
    B, S, _ = inputs["x"].shape
    L = inputs["ctx"].shape[1]
    DEPTH = inputs["w_in"].shape[0]
    ncores = 8
    NB = B // ncores
    cfg = dict(NB=NB, S=S, L=L, DEPTH=DEPTH)
    nc = build(cfg)
    shared = host_weights(inputs)
    shared.update(host_consts(S, L))
    in_maps = []
    for core in range(ncores):
        m = dict(shared)
        m.update(host_core_inputs(inputs, core, NB))
        in_maps.append(m)
    res = run_bass_kernel_spmd(nc, in_maps, core_ids=list(range(ncores)))
    return np.concatenate([r["out"] for r in res.results], axis=0)
```

```python
import contextlib
import math
import numpy as np
import concourse.bass as bass
import concourse.mybir as mybir
from concourse.bass_utils import run_bass_kernel_spmd

F32 = mybir.dt.float32
BF16 = mybir.dt.bfloat16
AF = mybir.ActivationFunctionType
ALU = mybir.AluOpType
AX = mybir.AxisListType

D = 1024
NH = 8
HD = 64
EPS = 1e-6
CQ, CK, CV, CDQKV, CGATE, CBA, CRQ, CRK, WEXT = 0, 512, 640, 768, 2304, 2816, 2848, 3360, 3488
NEXP = 32
FF = 256
BIG = 1.0e5
CH = 128


class Buf:
    def __init__(self, t, key):
        self.t = t
        self.key = key

    def __getitem__(self, idx):
        return self.t[idx]


class Sched:
    NSLOT = 16

    def __init__(self, nc):
        self.nc = nc
        self.es = contextlib.ExitStack()
        self.engs = {"pe": nc.tensor, "act": nc.scalar, "dve": nc.vector, "pool": nc.gpsimd, "sp": nc.sync}
        self.sem = {}
        self.cnt = {}
        for n in self.engs:
            self.sem[n] = self.es.enter_context(nc.semaphore("s_" + n))
            self.cnt[n] = 0
        for i in range(self.NSLOT):
            k = ("d", i)
            self.sem[k] = self.es.enter_context(nc.semaphore("s_d%d" % i))
            self.cnt[k] = 0
        self.waited = {n: {} for n in self.engs}
        self.res = {}
        self.slot = 0
        self.ninst = 0
        self.dead = False
        self.excl = set()

    def sbuf(self, name, shape, dtype=F32, stack=None):
        self.nbuf = getattr(self, "nbuf", 0) + 1
        name = "%s_u%d" % (name, self.nbuf)
        t = (stack or self.es).enter_context(self.nc.sbuf_tensor(name, list(shape), dtype))
        return Buf(t, name)

    def psum(self, name, shape, dtype=F32):
        t = self.es.enter_context(self.nc.psum_tensor(name, list(shape), dtype))
        self.excl.add(name)
        return Buf(t, name)

    def _val(self, k, c):
        return c * 16 if isinstance(k, tuple) else c

    def _keys(self, xs):
        return [x.key if isinstance(x, Buf) else x for x in xs]

    def _deps(self, me, reads, writes):
        deps = {}
        for r in reads:
            st = self.res.get(r)
            if st is None:
                continue
            for k, c in st[0].items():
                if k == me and me == "pe":
                    continue
                if c > deps.get(k, 0):
                    deps[k] = c
            if r in self.excl:
                for k, c in st[1].items():
                    if k != me and c > deps.get(k, 0):
                        deps[k] = c
        for w in writes:
            st = self.res.get(w)
            if st is None:
                continue
            for dd in st:
                for k, c in dd.items():
                    if (k != me or me == "pool") and c > deps.get(k, 0):
                        deps[k] = c
        return deps

    def _wait(self, eng, deps):
        wd = self.waited[eng]
        e = self.engs[eng]
        for k, c in deps.items():
            v = self._val(k, c)
            if wd.get(k, 0) >= v:
                continue
            e.wait_ge(self.sem[k], v)
            wd[k] = v

    def _commit(self, me, reads, writes):
        c = self.cnt[me]
        for r in reads:
            st = self.res.setdefault(r, [{}, {}])
            st[1][me] = c
        for w in writes:
            st = self.res.setdefault(w, [{}, {}])
            st[0] = {me: c}
            st[1] = {}

    def op(self, eng, fn, reads=(), writes=()):
        if self.dead:
            return None
        reads = self._keys(reads)
        writes = self._keys(writes)
        self._wait(eng, self._deps(eng, reads, writes))
        ins = fn(self.engs[eng])
        ins.then_inc(self.sem[eng], 1)
        self.cnt[eng] += 1
        self.ninst += 1
        self._commit(eng, reads, writes)
        return ins

    def dma(self, out, in_, reads=(), writes=(), eng="sp", **kw):
        if self.dead:
            return None
        reads = self._keys(reads)
        writes = self._keys(writes)
        k = ("d", self.slot)
        self.slot = (self.slot + 1) % self.NSLOT
        deps = self._deps(k, reads, writes)
        if self.cnt[k] > 0:
            deps[k] = max(deps.get(k, 0), self.cnt[k])
        self._wait(eng, deps)
        ins = self.engs[eng].dma_start(out=out, in_=in_, **kw)
        ins.then_inc(self.sem[k], 16)
        self.cnt[k] += 1
        self.ninst += 1
        self._commit(k, reads, writes)
        return ins

    def barrier(self):
        if self.dead:
            return
        deps = {}
        for k, c in self.cnt.items():
            if c > 0 and k != "sp":
                deps[k] = c
        for n in self.engs:
            d = {k: c for k, c in deps.items() if k != n}
            self._wait(n, d)
        self.res = {}

    def finish(self):
        if self.dead:
            return
        deps = {k: c for k, c in self.cnt.items() if c > 0 and k != "sp"}
        self._wait("sp", deps)

    def close(self):
        self.es.close()


def _tiles(L, S):
    ts = [(0, L)]
    for i in range(S // 512):
        ts.append((L + i * 512, 512))
    return ts


def build(cfg):
    NB, S, L, DEPTH = cfg["NB"], cfg["S"], cfg["L"], cfg["DEPTH"]
    taps = cfg.get("taps", ())
    stop = cfg.get("stop", "end")
    T = S + L
    TP = T
    NT128 = T // 128
    NCH = T // CH
    tiles = _tiles(L, S)
    PL = cfg.get('pooleng', 'dve')
    nc = bass.Bass("TRN2", target_bir_lowering=False)

    def din(name, shape, dt=F32):
        return nc.dram_tensor(name, list(shape), dt, kind="ExternalInput").ap()

    dbg_kind = "ExternalOutput" if taps else "Internal"

    def dscr(name, shape, dt=F32):
        kind = "ExternalOutput" if name in taps else "Internal"
        return nc.dram_tensor(name, list(shape), dt, kind=kind).ap()

    x_in = din("x", [NB, S, D])
    ctx_in = din("ctx", [NB, L, D])
    cT_in = din("cT", [128, 8, NB + 1])
    ada_w = din("ada_w", [DEPTH, D, 6 * D])
    ada_bT = din("ada_bT", [DEPTH, 128, 48])
    nmixT = din("nmixT", [DEPTH, 128, 8])
    nffnT = din("nffnT", [DEPTH, 128, 8])
    w_in_ext = din("w_in_ext", [DEPTH, D, WEXT])
    qkw = din("qkw", [DEPTH, 128, 4])
    qkw_row = din("qkw_row", [DEPTH, 2, 64])
    conv_wT = din("conv_wT", [DEPTH, 128, 12, 5])
    dn_A_log = din("dn_A_log", [DEPTH, 16])
    dn_dt_bias = din("dn_dt_bias", [DEPTH, 16])
    dn_norm_w = din("dn_norm_w", [DEPTH, 64])
    w_out = din("w_out", [DEPTH, D, D])
    wr_in = din("wr", [DEPTH, D, 36])
    rb_in = din("rb", [DEPTH, 36])
    w1_in = din("w1", [DEPTH, NEXP, D, FF])
    w3_in = din("w3", [DEPTH, NEXP, D, FF])
    w2_in = din("w2", [DEPTH, NEXP, FF, D])
    fnwT = din("fnwT", [128, 8])
    c_ident = din("c_ident", [128, 128])
    c_bd = din("c_bd", [128, 128])
    c_cos = din("c_cos", [128, T])
    c_sin = din("c_sin", [128, T])
    c_masks = din("c_masks", [6, 128, 128])
    c_lmask = din("c_lmask", [2, 7, 128, 128])
    out_hbm = nc.dram_tensor("out", [NB, S, D], F32, kind="ExternalOutput").ap()

    xT = dscr("xT", [NB, D, T])
    dq_raw = dscr("dq_raw", [NB, 1536, TP])
    gate_s = dscr("gate_s", [NB, T, 512])
    gb = dscr("gb", [NB, T, 32])
    attnT = dscr("attnT", [NB, NH, HD, T], BF16)
    qnT = dscr("qnT", [NB, 512, T], BF16)
    knT = dscr("knT", [NB, 512, T], BF16)
    k_tok = dscr("k_tok", [NB, T, 512])
    v_tok = dscr("v_tok", [NB, T, 512])
    o_dir = [dscr("o_f", [NB, T, 512]), dscr("o_b", [NB, T, 512])]
    h2T = dscr("h2T", [NB, D, T], BF16)
    WtT = dscr("WtT", [NB, NEXP, T])
    tapd = {}
    for nm, shp, dt in [("t_qT", [NB, 128, 4, T], BF16), ("t_kT", [NB, 128, T], BF16), ("t_V", [NB, 128, NT128, 200], BF16),
                        ("t_mod", [DEPTH, 128, 48, NB + 1], F32)]:
        if nm in taps:
            tapd[nm] = nc.dram_tensor(nm, shp, dt, kind="ExternalOutput").ap()

    s = Sched(nc)
    _tapped = set()

    def tap(name, buf, ap, shape, dt=F32):
        if ("dbg_" + name) not in taps or name in _tapped:
            return
        _tapped.add(name)
        dtens = nc.dram_tensor("dbg_" + name, list(shape), dt, kind="ExternalOutput").ap()
        s.dma(dtens, ap, reads=[buf])

    ps = [s.psum("ps%d" % i, [128, 512]) for i in range(4)]
    psS = [s.psum("psS%d" % i, [128, 1024]) for i in range(2)]
    for i in range(2):
        for hf in range(2):
            kname = "psv%d" % (4 + i * 2 + hf)
            s.excl.add(kname)
            ps.append(Buf(psS[i].t[:, hf * 512:(hf + 1) * 512], kname))

    ident = s.sbuf("ident", [128, 128])
    identb = s.sbuf("identb", [128, 128], BF16)
    ones = s.sbuf("ones", [128, 128])
    onesm = s.sbuf("onesm", [128, 128])
    bd64 = s.sbuf("bd64", [128, 128])
    bd1 = s.sbuf("bd1", [128, 128])
    epsc = s.sbuf("epsc", [128, 1])
    mod = [s.sbuf("mod%d" % l, [128, 48, NB + 1]) for l in range(DEPTH)]
    amix = [s.sbuf("amix%d" % l, [128, 8, NB + 1]) for l in range(DEPTH)]
    affn = [s.sbuf("affn%d" % l, [128, 8, NB + 1]) for l in range(DEPTH)]
    s.dma(ident[:], c_ident[:, :], writes=[ident])
    s.dma(bd1[:], c_bd[:, :], writes=[bd1])
    s.op("dve", lambda e: e.tensor_copy(out=identb[:], in_=ident[:]), reads=[ident], writes=[identb])
    s.op("dve", lambda e: e.memset(ones[:], 1.0), writes=[ones])
    s.op("dve", lambda e: e.memset(onesm[:], 1.0 / D), writes=[onesm])
    s.op("dve", lambda e: e.memset(epsc[:], EPS), writes=[epsc])
    s.op("dve", lambda e: e.tensor_scalar(out=bd64[:], in0=bd1[:], scalar1=1.0 / 64, scalar2=None, op0=ALU.mult), reads=[bd1], writes=[bd64])

    def rsqrt_(eng_ln, out_ap, in_ap, reads, writes, tmp, scale=1.0):
        s.op("act", lambda e: e.activation(out=tmp, in_=in_ap, func=AF.Ln, bias=epsc[:, 0:1], scale=scale), reads=list(reads) + [epsc], writes=writes)
        s.op("act", lambda e: e.activation(out=out_ap, in_=tmp, func=AF.Exp, scale=-0.5), reads=writes, writes=writes)

    with contextlib.ExitStack() as ph:
        scT = s.sbuf("scT", [128, 8, NB + 1], stack=ph)
        sct = s.sbuf("sct", [128, 8, NB + 1], stack=ph)
        adab = s.sbuf("adab", [128, 48], stack=ph)
        nw = s.sbuf("nw", [128, 8], stack=ph)
        nw2 = s.sbuf("nw2", [128, 8], stack=ph)
        awp = [s.sbuf("awp%d" % i, [128, 8, 1024], stack=ph) for i in range(2)]
        s.dma(scT[:], cT_in[:, :, :], writes=[scT])
        s.op("act", lambda e: e.activation(out=sct[:], in_=scT[:], func=AF.Exp, scale=-1.0), reads=[scT], writes=[sct])
        s.op("dve", lambda e: e.tensor_scalar(out=sct[:], in0=sct[:], scalar1=1.0, scalar2=None, op0=ALU.add), reads=[sct], writes=[sct])
        s.op("dve", lambda e: e.reciprocal(out=sct[:], in_=sct[:]), reads=[sct], writes=[sct])
        s.op("dve", lambda e: e.tensor_tensor(out=scT[:], in0=scT[:], in1=sct[:], op=ALU.mult), reads=[scT, sct], writes=[scT])
        NJ = NB + 1
        for l in range(DEPTH):
            s.dma(adab[:], ada_bT[l, :, :], writes=[adab])
            s.dma(nw[:], nmixT[l, :, :], writes=[nw])
            s.dma(nw2[:], nffnT[l, :, :], writes=[nw2])
            for piece in range(6):
                aw = awp[piece % 2]
                s.dma(aw[:], ada_w[l, :, piece * 1024:(piece + 1) * 1024].rearrange("(kc p) n -> p kc n", p=128), writes=[aw])
                pb = ps[piece % 2]
                for oc in range(8):
                    for kc in range(8):
                        s.op("pe", lambda e: e.matmul(pb[:, oc * NJ:(oc + 1) * NJ], lhsT=aw[:, kc, oc * 128:(oc + 1) * 128], rhs=scT[:, kc, :],
                                                     start=(kc == 0), stop=(kc == 7)), reads=[aw, scT], writes=[pb])
                s.op("dve", lambda e: e.tensor_tensor(out=mod[l][:, piece * 8:(piece + 1) * 8, :],
                                                      in0=pb[:, 0:8 * NJ].rearrange("p (a b) -> p a b", b=NJ),
                                                      in1=adab[:, piece * 8:(piece + 1) * 8].unsqueeze(2).broadcast_to([128, 8, NJ]), op=ALU.add),
                     reads=[pb, adab], writes=[mod[l]])
            for (dst, wv, off) in ((amix[l], nw, 8), (affn[l], nw2, 32)):
                s.op("dve", lambda e: e.tensor_scalar(out=dst[:], in0=mod[l][:, off:off + 8, :], scalar1=1.0, scalar2=None, op0=ALU.add), reads=[mod[l]], writes=[dst])
                s.op("dve", lambda e: e.tensor_tensor(out=dst[:], in0=dst[:], in1=wv[:].unsqueeze(2).broadcast_to([128, 8, NJ]), op=ALU.mult), reads=[dst, wv], writes=[dst])
            if "t_mod" in tapd:
                s.dma(tapd["t_mod"][l], mod[l][:], reads=[mod[l]])
        s.barrier()
    if stop == "prep":
        s.finish(); s.close(); return nc

    with contextlib.ExitStack() as ph:
        xin = [s.sbuf("xin%d" % i, [128, 4, D], stack=ph) for i in range(2)]
        xo = [s.sbuf("xo%d" % i, [128, 8, 512], stack=ph) for i in range(2)]
        zt = s.sbuf("zt", [128, 2], stack=ph)
        s.op("dve", lambda e: e.memset(zt[:], 0.0), writes=[zt])
        it = 0
        for b in range(NB):
            for (t0, n) in tiles:
                xi, xb_ = xin[it % 2], xo[it % 2]
                nsub = n // 128
                src = ctx_in[b, t0:t0 + n, :] if t0 < L else x_in[b, t0 - L:t0 - L + n, :]
                s.dma(xi[:, 0:nsub, :], src.rearrange("(s p) f -> p s f", p=128), writes=[xi])
                for fc in range(8):
                    pb = ps[fc % 4]
                    for sub in range(nsub):
                        s.op("pe", lambda e: e.transpose(pb[:, sub * 128:(sub + 1) * 128], xi[:, sub, fc * 128:(fc + 1) * 128], ident[:]), reads=[xi, ident], writes=[pb])
                    if fc % 2 == 0:
                        s.op("act", lambda e: e.copy(out=xb_[:, fc, 0:n], in_=pb[:, 0:n]), reads=[pb], writes=[xb_])
                    else:
                        s.op("dve", lambda e: e.tensor_copy(out=xb_[:, fc, 0:n], in_=pb[:, 0:n]), reads=[pb], writes=[xb_])
                s.dma(xT[b, :, t0:t0 + n].rearrange("(c p) t -> p c t", p=128), xb_[:, :, 0:n], reads=[xb_], writes=[("xT", b, t0)])
                it += 1
        s.barrier()

    if stop == "s0":
        s.finish(); s.close(); return nc

    for l in range(DEPTH):
        last = (l == DEPTH - 1)
        with contextlib.ExitStack() as ph:
            winb = s.sbuf("winb", [128, 8, WEXT], BF16, stack=ph)
            qkwt = s.sbuf("qkwt", [128, 4], stack=ph)
            qkrow = s.sbuf("qkrow", [128, 128], stack=ph)
            nshift = s.sbuf("nshift", [128, 1], stack=ph)
            mx = s.sbuf("mx", [128, 2], stack=ph)
            alog = s.sbuf("alog", [128, 16], stack=ph)
            dtb = s.sbuf("dtb", [128, 16], stack=ph)
            qTb = s.sbuf("qTb", [128, 4, T], BF16, stack=ph)
            kTb = s.sbuf("kTb", [128, T], BF16, stack=ph)
            Vb = s.sbuf("Vb", [128, NT128, 200], BF16, stack=ph)
            phw = contextlib.ExitStack()
            wst = [s.sbuf("wst%d" % i, [128, 872], stack=phw) for i in range(2)]
            ci = 0
            for kc in range(8):
                for q4 in range(4):
                    st = wst[ci % 2]
                    s.dma(st[:], w_in_ext[l, kc * 128:(kc + 1) * 128, q4 * 872:(q4 + 1) * 872], writes=[st])
                    eng = "act" if ci % 2 == 0 else "dve"
                    if eng == "act":
                        s.op("act", lambda e: e.copy(out=winb[:, kc, q4 * 872:(q4 + 1) * 872], in_=st[:]), reads=[st], writes=[winb])
                    else:
                        s.op("dve", lambda e: e.tensor_copy(out=winb[:, kc, q4 * 872:(q4 + 1) * 872], in_=st[:]), reads=[st], writes=[winb])
                    ci += 1
            s.barrier()
            phw.close()
            s.dma(qkwt[:], qkw[l, :, :], writes=[qkwt])
            s.dma(qkrow[:], qkw_row[l:l + 1, :, :].rearrange("o a b -> o (a b)").partition_broadcast(128), writes=[qkrow])
            s.dma(alog[:], dn_A_log[l:l + 1, :].partition_broadcast(128), writes=[alog])
            s.dma(dtb[:], dn_dt_bias[l:l + 1, :].partition_broadcast(128), writes=[dtb])
            s.op("act", lambda e: e.activation(out=alog[:], in_=alog[:], func=AF.Exp), reads=[alog], writes=[alog])
            s.op("dve", lambda e: e.tensor_scalar(out=alog[:], in0=alog[:], scalar1=-1.0, scalar2=None, op0=ALU.mult), reads=[alog], writes=[alog])
            s.op("dve", lambda e: e.tensor_reduce(out=mx[:, 0:1], in_=qkrow[:, 0:64], axis=AX.X, op=ALU.max, apply_absolute_value=True), reads=[qkrow], writes=[mx])
            s.op("dve", lambda e: e.tensor_reduce(out=mx[:, 1:2], in_=qkrow[:, 64:128], axis=AX.X, op=ALU.max, apply_absolute_value=True), reads=[qkrow, mx], writes=[mx])
            s.op("dve", lambda e: e.tensor_tensor(out=nshift[:], in0=mx[:, 0:1], in1=mx[:, 1:2], op=ALU.mult), reads=[mx], writes=[nshift])
            s.op("dve", lambda e: e.tensor_scalar(out=nshift[:], in0=nshift[:], scalar1=-8.0, scalar2=None, op0=ALU.mult), reads=[nshift], writes=[nshift])

            if cfg.get('cut') == 1:
                s.finish(); s.dead = True

            s.op("dve", lambda e: e.memset(Vb[:], 1.0), writes=[Vb])

            if cfg.get('cut') == 2:
                s.finish(); s.dead = True
            xt2 = [s.sbuf("xt%d" % i, [128, 8, 512], stack=ph) for i in range(1)]
            sqb = s.sbuf("sqb", [128, 8, 512], stack=ph)
            hTb = s.sbuf("hTb", [128, 8, 512], BF16, stack=ph)
            rstd = s.sbuf("rstd", [128, 512], stack=ph)
            cst = [s.sbuf("cst%d" % i, [128, 2, 512], stack=ph) for i in range(1)]
            rq = s.sbuf("rq", [128, 512], stack=ph)
            t1 = s.sbuf("t1", [128, 512], stack=ph)
            t2 = s.sbuf("t2", [128, 512], stack=ph)
            sqq = t2
            dstl = [s.sbuf("dqst%d" % i, [128, 4, 512], stack=ph) for i in range(1)]
            qzl = [[s.sbuf("qz%d_%d" % (k_, r_), [128, 1024], BF16, stack=ph) for r_ in range(2)] for k_ in range(2)]
            for k_ in range(2):
                for r_ in range(2):
                    s.op("dve", lambda e: e.memset(qzl[k_][r_][:], 0.0), writes=[qzl[k_][r_]])
            gstl = [s.sbuf("gst%d" % i, [128, 512], stack=ph) for i in range(1)]
            ge = s.sbuf("ge", [128, 512], stack=ph)
            gbt = s.sbuf("gbt", [128, 4, 32], stack=ph)
            gb1 = s.sbuf("gb1", [128, 4, 16], stack=ph)
            gb2 = s.sbuf("gb2", [128, 4, 16], stack=ph)
            ptb = [s.sbuf("ptb%d" % i, [128, 1024], BF16, stack=ph) for i in range(3)]
            rrowl = [s.sbuf("rrow%d" % i, [128, 512], stack=ph) for i in range(2)]
            aul = [s.sbuf("au%d" % i, [64, 512], stack=ph) for i in range(2)]
            ao = [s.sbuf("ao%d" % i, [64, 512], BF16, stack=ph) for i in range(2)]

            def proj(pb, col0, ncol, n):
                for kc in range(8):
                    s.op("pe", lambda e: e.matmul(pb[0:ncol, 0:n], lhsT=winb[:, kc, col0:col0 + ncol], rhs=hTb[:, kc, 0:n], start=(kc == 0), stop=(kc == 7)),
                         reads=[winb, hTb], writes=[pb])

            it = 0
            for b in range(NB):
                for (t0, n) in tiles:
                    j = NB if t0 < L else b
                    nsub = n // 128
                    xt = xt2[0]
                    cs = cst[0]
                    it += 1
                    s.dma(xt[:, :, 0:n], xT[b, :, t0:t0 + n].rearrange("(c p) t -> p c t", p=128), reads=[("xT", b, t0)], writes=[xt])
                    s.dma(cs[:, 0, 0:n], c_cos[:, t0:t0 + n], writes=[cs])
                    s.dma(cs[:, 1, 0:n], c_sin[:, t0:t0 + n], writes=[cs])
                    s.op("act", lambda e: e.activation(out=sqb[:, :, 0:n], in_=xt[:, :, 0:n], func=AF.Square), reads=[xt], writes=[sqb])
                    for c in range(8):
                        s.op("pe", lambda e: e.matmul(ps[0][:, 0:n], lhsT=onesm[:], rhs=sqb[:, c, 0:n], start=(c == 0), stop=(c == 7)), reads=[onesm, sqb], writes=[ps[0]])
                    rsqrt_("act", rstd[:, 0:n], ps[0][:, 0:n], [ps[0]], [rstd], rstd[:, 0:n])
                    s.op("dve", lambda e: e.tensor_tensor(out=sqb[:, :, 0:n], in0=xt[:, :, 0:n], in1=rstd[:, 0:n].unsqueeze(1).broadcast_to([128, 8, n]), op=ALU.mult),
                         reads=[xt, rstd], writes=[sqb])
                    for c in range(8):
                        s.op("dve", lambda e: e.tensor_scalar(out=hTb[:, c, 0:n], in0=sqb[:, c, 0:n], scalar1=amix[l][:, c, j:j + 1], scalar2=mod[l][:, c, j:j + 1],
                                                              op0=ALU.mult, op1=ALU.add), reads=[sqb, amix[l], mod[l]], writes=[hTb])

                    if cfg.get('cut') == 3 and it == cfg.get('cutit', 1):
                        s.finish(); s.dead = True
                    for c in range(5):
                        isq = c < 4
                        col = CQ + c * 128 if isq else CK
                        rcol = CRQ + c * 128 if isq else CRK
                        wi = 0 if isq else 2
                        pq, prq, pm = ps[1 + (c % 2) * 3], ps[2 + (c % 2) * 3], ps[3 + (c % 2) * 3]
                        proj(pq, col, 128, n)
                        if cfg.get('cut') == 11 and it == cfg.get('cutit', 1):
                            s.finish(); s.dead = True
                        proj(prq, rcol, 128, n)
                        if cfg.get('cut') == 10 and it == cfg.get('cutit', 1):
                            s.finish(); s.dead = True
                        s.op("act", lambda e: e.activation(out=sqq[:, 0:n], in_=pq[:, 0:n], func=AF.Square), reads=[pq], writes=[sqq])
                        if cfg.get('cut') == 12 and it == cfg.get('cutit', 1):
                            s.finish(); s.dead = True
                        if cfg.get('exp') == 1:
                            s.op("dve", lambda e: e.memset(rq[:, 0:n], 1.0), writes=[rq])
                        else:
                            s.op("pe", lambda e: e.matmul(pm[:, 0:n], lhsT=bd64[:], rhs=sqq[:, 0:n], start=True, stop=True), reads=[bd64, sqq], writes=[pm])
                            rsqrt_("act", rq[:, 0:n], pm[:, 0:n], [pm], [rq], rq[:, 0:n])
                        s.op("dve", lambda e: e.scalar_tensor_tensor(out=t1[:, 0:n], in0=pq[:, 0:n], scalar=qkwt[:, wi:wi + 1], in1=cs[:, 0, 0:n], op0=ALU.mult, op1=ALU.mult),
                             reads=[pq, qkwt, cs], writes=[t1])
                        if cfg.get('cut') == 13 and it == cfg.get('cutit', 1):
                            s.finish(); s.dead = True
                        s.op("dve", lambda e: e.scalar_tensor_tensor(out=t2[:, 0:n], in0=prq[:, 0:n], scalar=qkwt[:, wi + 1:wi + 2], in1=cs[:, 1, 0:n], op0=ALU.mult, op1=ALU.mult),
                             reads=[prq, qkwt, cs], writes=[t2])
                        if cfg.get('cut') == 14 and it == cfg.get('cutit', 1):
                            s.finish(); s.dead = True
                        s.op(PL, lambda e: e.tensor_tensor(out=t1[:, 0:n], in0=t1[:, 0:n], in1=t2[:, 0:n], op=ALU.add), reads=[t1, t2], writes=[t1])
                        dstb = qTb[:, c, t0:t0 + n] if isq else kTb[:, t0:t0 + n]
                        s.op(PL, lambda e: e.tensor_tensor(out=dstb, in0=t1[:, 0:n], in1=rq[:, 0:n], op=ALU.mult), reads=[t1, rq], writes=[qTb if isq else kTb])

                    if cfg.get('cut') == 4 and it == cfg.get('cutit', 1):
                        s.finish(); s.dead = True
                    for cc in range(12):
                        pb = ps[1 + cc % 6]
                        dst_ = dstl[0]
                        proj(pb, CDQKV + cc * 128, 128, n)
                        if cc % 2 == 0:
                            s.op("act", lambda e: e.copy(out=dst_[:, cc % 4, 0:n], in_=pb[:, 0:n]), reads=[pb], writes=[dst_])
                        else:
                            s.op("dve", lambda e: e.tensor_copy(out=dst_[:, cc % 4, 0:n], in_=pb[:, 0:n]), reads=[pb], writes=[dst_])
                        if cc % 4 == 3:
                            c4 = cc // 4
                            s.dma(dq_raw[b, c4 * 512:(c4 + 1) * 512, t0:t0 + n].rearrange("(c p) t -> p c t", p=128), dst_[:, :, 0:n], reads=[dst_], writes=[("dq_raw", b)])

                    if cfg.get('cut') == 5 and it == cfg.get('cutit', 1):
                        s.finish(); s.dead = True
                    pv = ps[7]
                    for sub in range(nsub):
                        for kc in range(8):
                            s.op("pe", lambda e: e.matmul(pv[:, sub * 128:(sub + 1) * 128], lhsT=hTb[:, kc, sub * 128:(sub + 1) * 128], rhs=winb[:, kc, CV:CV + 128],
                                                         start=(kc == 0), stop=(kc == 7)), reads=[hTb, winb], writes=[pv])
                    s.op("act", lambda e: e.copy(out=Vb[:, t0 // 128:t0 // 128 + nsub, 0:130].rearrange("p s (k d) -> p s k d", k=2)[:, :, :, 0:64],
                                                 in_=pv[:, 0:n].rearrange("p (s k d) -> p s k d", k=2, d=64)), reads=[pv], writes=[Vb])

                    if cfg.get('cut') == 6 and it == cfg.get('cutit', 1):
                        s.finish(); s.dead = True
                    for sub in range(nsub):
                        pg = ps[1 + sub % 4]
                        for kc in range(8):
                            s.op("pe", lambda e: e.matmul(pg[:, :], lhsT=hTb[:, kc, sub * 128:(sub + 1) * 128], rhs=winb[:, kc, CGATE:CGATE + 512],
                                                         start=(kc == 0), stop=(kc == 7)), reads=[hTb, winb], writes=[pg])
                        s.op("act", lambda e: e.activation(out=ge[:], in_=pg[:], func=AF.Exp, scale=-1.0), reads=[pg], writes=[ge])
                        s.op("act", lambda e: e.activation(out=ge[:], in_=ge[:], func=AF.Ln, bias=1.0), reads=[ge], writes=[ge])
                        s.op("act", lambda e: e.activation(out=ge[:], in_=ge[:], func=AF.Exp, scale=-1.0), reads=[ge], writes=[ge])
                        gst = gstl[0]
                        s.op("dve", lambda e: e.tensor_tensor(out=gst[:], in0=pg[:], in1=ge[:], op=ALU.mult), reads=[pg, ge], writes=[gst])
                        s.dma(gate_s[b, t0 + sub * 128:t0 + (sub + 1) * 128, :], gst[:], reads=[gst], writes=[("gate_s", b, t0)])

                    if cfg.get('cut') == 7 and it == cfg.get('cutit', 1):
                        s.finish(); s.dead = True
                    pba = ps[5]
                    for sub in range(nsub):
                        for kc in range(8):
                            s.op("pe", lambda e: e.matmul(pba[:, sub * 32:(sub + 1) * 32], lhsT=hTb[:, kc, sub * 128:(sub + 1) * 128], rhs=winb[:, kc, CBA:CBA + 32],
                                                         start=(kc == 0), stop=(kc == 7)), reads=[hTb, winb], writes=[pba])
                    pba3 = pba[:, 0:nsub * 32].rearrange("p (s c) -> p s c", c=32)
                    s.op("act", lambda e: e.activation(out=gb1[:, 0:nsub, :], in_=pba3[:, :, 0:16], func=AF.Exp, scale=-1.0), reads=[pba], writes=[gb1])
                    s.op("dve", lambda e: e.tensor_scalar(out=gb1[:, 0:nsub, :], in0=gb1[:, 0:nsub, :], scalar1=1.0, scalar2=None, op0=ALU.add), reads=[gb1], writes=[gb1])
                    s.op("dve", lambda e: e.reciprocal(out=gbt[:, 0:nsub, 0:16], in_=gb1[:, 0:nsub, :]), reads=[gb1], writes=[gbt])
                    s.op("dve", lambda e: e.tensor_tensor(out=gb2[:, 0:nsub, :], in0=pba3[:, :, 16:32], in1=dtb[:].unsqueeze(1).broadcast_to([128, nsub, 16]), op=ALU.add),
                         reads=[pba, dtb], writes=[gb2])
                    s.op("act", lambda e: e.activation(out=gb2[:, 0:nsub, :], in_=gb2[:, 0:nsub, :], func=AF.Exp), reads=[gb2], writes=[gb2])
                    s.op("act", lambda e: e.activation(out=gb2[:, 0:nsub, :], in_=gb2[:, 0:nsub, :], func=AF.Ln, bias=1.0), reads=[gb2], writes=[gb2])
                    s.op("dve", lambda e: e.tensor_tensor(out=gbt[:, 0:nsub, 16:32], in0=gb2[:, 0:nsub, :], in1=alog[:].unsqueeze(1).broadcast_to([128, nsub, 16]), op=ALU.mult),
                         reads=[gb2, alog, gbt], writes=[gbt])
                    s.dma(gb[b, t0:t0 + n, :].rearrange("(s p) c -> p s c", p=128), gbt[:, 0:nsub, :], reads=[gbt], writes=[("gb", b)])

                    if cfg.get('cut') == 8 and it == cfg.get('cutit', 1):
                        s.finish(); s.dead = True

                if "t_qT" in tapd:
                    if cfg.get('cut') == 9:
                        s.finish(); s.dead = True
                    s.dma(tapd["t_qT"][b], qTb[:], reads=[qTb])
                    s.dma(tapd["t_kT"][b], kTb[:], reads=[kTb])
                    s.dma(tapd["t_V"][b], Vb[:], reads=[Vb])
                if stop == "A":
                    continue
                aoi = 0
                ui = 0
                for kv in range(2):
                    pl = slice(kv * 64, (kv + 1) * 64)
                    for (t0, n) in tiles:
                        kts = list(range(L // 128)) if t0 < L else list(range(NT128))
                        for gp in range(2):
                            accb = [ps[(ui % 2) * 2], ps[(ui % 2) * 2 + 1]]
                            qz = qzl[kv][ui % 2]
                            ui += 1
                            LA = 2
                            s.op("dve", lambda e: e.tensor_copy(out=qz[pl, :].rearrange("p (two c) -> p two c", two=2)[:, :, 0:n], in_=qTb[pl, gp * 2:gp * 2 + 2, t0:t0 + n]),
                                 reads=[qTb], writes=[qz])

                            def emit_s(si):
                                kt = kts[si]
                                big = psS[si % 2]
                                halves = [ps[4 + (si % 2) * 2], ps[5 + (si % 2) * 2]]
                                for hh in range(2):
                                    g = gp * 2 + hh
                                    s.op("pe", lambda e: e.matmul(halves[hh][:, 0:n], lhsT=kTb[:, kt * 128:(kt + 1) * 128], rhs=qz[:, hh * 512:hh * 512 + n], start=True, stop=True),
                                         reads=[kTb, qz], writes=[halves[hh]])
                                pt_ = ptb[si % 3]
                                s.op("act", lambda e: e.activation(out=pt_[:].rearrange("p (two c) -> p two c", two=2)[:, :, 0:n],
                                                                   in_=big.t[:].rearrange("p (two c) -> p two c", two=2)[:, :, 0:n], func=AF.Exp, bias=nshift[:, 0:1], scale=0.125),
                                     reads=[halves[0], halves[1], nshift], writes=[pt_])

                            def emit_pv(si):
                                kt = kts[si]
                                pt_ = ptb[si % 3]
                                for hh in range(2):
                                    s.op("pe", lambda e: e.matmul(accb[hh][:, 0:n], lhsT=Vb[:, kt, kv * 65:kv * 65 + 128], rhs=pt_[:, hh * 512:hh * 512 + n],
                                                                 start=(si == 0), stop=(si == len(kts) - 1)), reads=[Vb, pt_], writes=[accb[hh]])

                            for si in range(len(kts) + LA):
                                if si < len(kts):
                                    emit_s(si)
                                if si - LA >= 0:
                                    emit_pv(si - LA)
                            for hh in range(2):
                                head = kv * 4 + gp * 2 + hh
                                pa_ = accb[hh]
                                a_o = ao[aoi % 2]
                                au = aul[aoi % 2]
                                rrow = rrowl[aoi % 2]
                                aoi += 1
                                s.op("dve", lambda e: e.reciprocal(out=rrow[64:65, 0:n], in_=pa_[64:65, 0:n]), reads=[pa_], writes=[rrow])
                                s.op("dve", lambda e: e.tensor_copy(out=au[:, 0:n], in_=pa_[0:64, 0:n]), reads=[pa_], writes=[au])
                                s.op("pe", lambda e: e.matmul(pa_[0:64, 0:n], lhsT=ones[64:65, 0:64], rhs=rrow[64:65, 0:n], start=True, stop=True), reads=[ones, rrow], writes=[pa_])
                                s.op("dve", lambda e: e.tensor_tensor(out=a_o[:, 0:n], in0=pa_[0:64, 0:n], in1=au[:, 0:n], op=ALU.mult), reads=[pa_, au], writes=[a_o])
                                s.dma(attnT[b, head, :, t0:t0 + n], a_o[:, 0:n], reads=[a_o], writes=[("attnT", b)])
            s.barrier()
        if stop in ("A", "attn"):
            s.finish(); s.close(); return nc

        with contextlib.ExitStack() as ph:
            cw = s.sbuf("cw", [128, 12, 5], stack=ph)
            s.dma(cw[:], conv_wT[l, :, :, :], writes=[cw])
            NR = 6
            rwb = [s.sbuf("rw%d" % i, [128, 516], stack=ph) for i in range(NR)]
            accl = [s.sbuf("cacc%d" % i, [128, 512], stack=ph) for i in range(NR)]
            cel = [s.sbuf("ce%d" % i, [128, 512], stack=ph) for i in range(NR)]
            cyl = [s.sbuf("cy%d" % i, [128, 512], stack=ph) for i in range(NR)]
            csql = [s.sbuf("csq%d" % i, [128, 512], stack=ph) for i in range(NR)]
            crnl = [s.sbuf("crn%d" % i, [128, 512], stack=ph) for i in range(NR)]
            cynl = [s.sbuf("cyn%d" % i, [128, 512], stack=ph) for i in range(NR)]
            cynb = [s.sbuf("cynb%d" % i, [128, 512], BF16, stack=ph) for i in range(NR)]
            ctk = [s.sbuf("ctk%d" % i, [128, 4, 128], stack=ph) for i in range(NR)]
            its = [(b, cc, t0, n) for b in range(NB) for cc in range(12) for (t0, n) in tiles]

            def bufs(i):
                r_ = i % NR
                return rwb[r_], cynb[r_], ctk[r_], accl[r_], cel[r_], cyl[r_], csql[r_], crnl[r_], cynl[r_]

            def st_load(i):
                b, cc, t0, n = its[i]
                rw = bufs(i)[0]
                lo = t0 if t0 in (0, L) else t0 - 2
                hi = t0 + n if (t0 + n) in (L, T) else t0 + n + 2
                s.op("dve", lambda e: e.memset(rw[:, 0:2], 0.0), writes=[rw])
                s.op("dve", lambda e: e.memset(rw[:, 2 + n:4 + n], 0.0), writes=[rw])
                s.dma(rw[:, 2 - (t0 - lo):2 + n + (hi - t0 - n)], dq_raw[b, cc * 128:(cc + 1) * 128, lo:hi], reads=[("dq_raw", b)], writes=[rw])

            def st_conv(i):
                b, cc, t0, n = its[i]
                rw, ynb, tk, acc, ce, cy, csq, crn, cyn = bufs(i)
                s.op("act", lambda e: e.activation(out=acc[:, 0:n], in_=rw[:, 0:n], func=AF.Copy, scale=cw[:, cc, 0:1]), reads=[rw, cw], writes=[acc])
                for jj in range(1, 5):
                    s.op("dve", lambda e: e.scalar_tensor_tensor(out=acc[:, 0:n], in0=rw[:, jj:jj + n], scalar=cw[:, cc, jj:jj + 1], in1=acc[:, 0:n],
                                                                 op0=ALU.mult, op1=ALU.add), reads=[rw, cw, acc], writes=[acc])

            def st_sig(i):
                b, cc, t0, n = its[i]
                rw, ynb, tk, acc, ce, cy, csq, crn, cyn = bufs(i)
                s.op("act", lambda e: e.activation(out=ce[:, 0:n], in_=acc[:, 0:n], func=AF.Exp, scale=-1.0), reads=[acc], writes=[ce])
                s.op("act", lambda e: e.activation(out=ce[:, 0:n], in_=ce[:, 0:n], func=AF.Ln, bias=1.0), reads=[ce], writes=[ce])
                s.op("act", lambda e: e.activation(out=ce[:, 0:n], in_=ce[:, 0:n], func=AF.Exp, scale=-1.0), reads=[ce], writes=[ce])
                s.op("dve", lambda e: e.tensor_tensor(out=cy[:, 0:n], in0=acc[:, 0:n], in1=ce[:, 0:n], op=ALU.mult), reads=[acc, ce], writes=[cy])
                if cc < 8:
                    s.op("act", lambda e: e.activation(out=csq[:, 0:n], in_=cy[:, 0:n], func=AF.Square), reads=[cy], writes=[csq])
                    pm = ps[i % 4]
                    s.op("pe", lambda e: e.matmul(pm[:, 0:n], lhsT=bd1[:], rhs=csq[:, 0:n], start=True, stop=True), reads=[bd1, csq], writes=[pm])

            def st_fin(i):
                b, cc, t0, n = its[i]
                nsub = n // 128
                rw, ynb, tk, acc, ce, cy, csq, crn, cyn = bufs(i)
                src_tm = cy
                if cc < 8:
                    pm = ps[i % 4]
                    rsqrt_("act", crn[:, 0:n], pm[:, 0:n], [pm], [crn], crn[:, 0:n])
                    sc_ = 0.125 if cc < 4 else 1.0
                    s.op("dve", lambda e: e.scalar_tensor_tensor(out=cyn[:, 0:n], in0=cy[:, 0:n], scalar=sc_, in1=crn[:, 0:n], op0=ALU.mult, op1=ALU.mult),
                         reads=[cy, crn], writes=[cyn])
                    s.op("dve", lambda e: e.tensor_copy(out=ynb[:, 0:n], in_=cyn[:, 0:n]), reads=[cyn], writes=[ynb])
                    dstT = qnT if cc < 4 else knT
                    s.dma(dstT[b, (cc % 4) * 128:(cc % 4 + 1) * 128, t0:t0 + n], ynb[:, 0:n], reads=[ynb], writes=[("qknT", b)])
                    src_tm = cyn
                if cc >= 4:
                    pt_ = ps[4 + i % 4]
                    for sub in range(nsub):
                        s.op("pe", lambda e: e.transpose(pt_[:, sub * 128:(sub + 1) * 128], src_tm[:, sub * 128:(sub + 1) * 128], ident[:]), reads=[src_tm, ident], writes=[pt_])
                    s.op("act", lambda e: e.copy(out=tk[:, 0:nsub, :], in_=pt_[:, 0:n].rearrange("p (s c) -> p s c", c=128)), reads=[pt_], writes=[tk])
                    dtk = k_tok if cc < 8 else v_tok
                    s.dma(dtk[b, t0:t0 + n, (cc % 4) * 128:(cc % 4 + 1) * 128].rearrange("(s p) c -> p s c", p=128), tk[:, 0:nsub, :], reads=[tk], writes=[("kvtok", b)])

            NI = len(its)
            stages = [st_load, st_conv, st_sig, st_fin]
            for step in range(NI + len(stages) - 1):
                for k_, fn_ in enumerate(stages):
                    i_ = step - k_
                    if 0 <= i_ < NI:
                        fn_(i_)
            s.barrier()
        if stop == "B":
            s.finish(); s.close(); return nc

        with contextlib.ExitStack() as ph:
            mk = [s.sbuf("mk%d" % i, [128, 128], stack=ph) for i in range(6)]
            for i in range(6):
                s.dma(mk[i][:], c_masks[i, :, :], writes=[mk[i]])
            lmk = [[s.sbuf("lmk%d_%d" % (a_, l_), [128, 128], BF16, stack=ph) for l_ in range(7)] for a_ in range(2)]
            lmst = s.sbuf("lmst", [128, 128], stack=ph)
            for a_ in range(2):
                for l_ in range(7):
                    s.dma(lmst[:], c_lmask[a_, l_, :, :], writes=[lmst])
                    s.op("act", lambda e: e.copy(out=lmk[a_][l_][:], in_=lmst[:]), reads=[lmst], writes=[lmk[a_][l_]])
            N0bl = [s.sbuf("N0b%d" % g_, [128, 512], BF16, stack=ph) for g_ in range(2)]
            Ptbl = [[s.sbuf("Ptb%d_%d" % (g_, i), [128, 512], BF16, stack=ph) for i in range(2)] for g_ in range(2)]
            Pbl = [s.sbuf("Pb%d" % g_, [128, 512], BF16, stack=ph) for g_ in range(2)]
            Zbl = [s.sbuf("Zb%d" % g_, [128, 512], BF16, stack=ph) for g_ in range(2)]
            notI = s.sbuf("notI", [128, 128], stack=ph)
            nones = s.sbuf("nones", [128, 128], stack=ph)
            s.op("dve", lambda e: e.tensor_scalar(out=notI[:], in0=ident[:], scalar1=-1.0, scalar2=1.0, op0=ALU.mult, op1=ALU.add), reads=[ident], writes=[notI])
            s.op("dve", lambda e: e.memset(nones[:], -1.0), writes=[nones])
            NC8 = NCH * 8
            gbt = s.sbuf("dgbt", [128, NCH, 32], stack=ph)
            gc = s.sbuf("dgc", [128, NC8], stack=ph)
            glb = s.sbuf("dglb", [128, NC8], stack=ph)
            egc = s.sbuf("degc", [128, NC8], stack=ph)
            egl = s.sbuf("degl", [128, NC8], stack=ph)
            eglmg = s.sbuf("deglmg", [128, NC8], stack=ph)
            nbt = s.sbuf("dnbt", [128, NC8], stack=ph)
            bet = s.sbuf("dbet", [128, NC8], stack=ph)
            begc = s.sbuf("dbegc", [128, NC8], stack=ph)
            Sf = s.sbuf("Sf", [64, 8, 64], stack=ph)
            Sb = s.sbuf("Sb", [64, 8, 64], BF16, stack=ph)
            qcb = [s.sbuf("qc%d" % i, [64, 8, 128], BF16, stack=ph) for i in range(2)]
            kcb = [s.sbuf("kc%d" % i, [64, 8, 128], BF16, stack=ph) for i in range(2)]
            ktb = [s.sbuf("kt%d" % i, [128, 512], stack=ph) for i in range(2)]
            vtb = [s.sbuf("vt%d" % i, [128, 512], stack=ph) for i in range(2)]
            vb_ = s.sbuf("vb", [128, 512], BF16, stack=ph)
            kbg = s.sbuf("kbg", [128, 512], BF16, stack=ph)
            kdec = s.sbuf("kdec", [128, 512], BF16, stack=ph)
            dg4l = [s.sbuf("dg4_%d" % i, [128, 512], stack=ph) for i in range(2)]
            ng4l = [s.sbuf("ng4_%d" % i, [128, 512], stack=ph) for i in range(2)]
            Dtl = [s.sbuf("Dt%d" % i, [128, 512], stack=ph) for i in range(2)]
            Dstl = [s.sbuf("Dst%d" % i, [128, 512], stack=ph) for i in range(2)]
            intral = [s.sbuf("intra%d" % i, [128, 512], BF16, stack=ph) for i in range(2)]
            intraT = s.sbuf("intraT", [128, 1024], BF16, stack=ph)
            Nkl = [[s.sbuf("Nk%d_%d" % (g_, i), [128, 512], stack=ph) for i in range(3)] for g_ in range(2)]
            Pml = [s.sbuf("Pm%d" % g_, [128, 512], stack=ph) for g_ in range(2)]
            Rtl = [s.sbuf("Rt%d" % g_, [128, 512], stack=ph) for g_ in range(2)]
            Qml = [s.sbuf("Qm%d" % g_, [128, 512], stack=ph) for g_ in range(2)]
            NkTl = [[s.sbuf("NkT%d_%d" % (g_, i), [128, 512], stack=ph) for i in range(2)] for g_ in range(2)]
            PTl = [[s.sbuf("PT%d_%d" % (g_, i), [128, 512], stack=ph) for i in range(2)] for g_ in range(2)]
            TTbl = [s.sbuf("TTb%d" % g_, [128, 512], BF16, stack=ph) for g_ in range(2)]
            wTs = s.sbuf("wTs", [64, 8, 128], BF16, stack=ph)
            us = s.sbuf("us", [128, 512], stack=ph)
            vnew = s.sbuf("vnew", [128, 512], BF16, stack=ph)
            tt = s.sbuf("dtt", [128, 512], stack=ph)
            otb = [s.sbuf("ot%d" % i, [128, 512], stack=ph) for i in range(2)]
            ncx = L // CH
            it = 0
            for b in range(NB):
                for dr in range(2):
                    tri, sel, bigm = mk[dr], mk[2 + dr], mk[4 + dr]
                    s.dma(gbt[:], gb[b, :, :].rearrange("(n p) c -> p n c", p=128), reads=[("gb", b)], writes=[gbt])
                    gd = gbt[:, :, 16 + dr * 8:24 + dr * 8]
                    bdv = gbt[:, :, dr * 8:dr * 8 + 8]
                    g3 = lambda t_: t_[:].rearrange("p (n h) -> p n h", h=8)
                    s.op("pe", lambda e: e.matmul(ps[0][:, 0:NC8].rearrange("p (n h) -> p n h", h=8), lhsT=tri[:], rhs=gd, start=True, stop=True), reads=[tri, gbt], writes=[ps[0]])
                    s.op("act", lambda e: e.copy(out=gc[:], in_=ps[0][:, 0:NC8]), reads=[ps[0]], writes=[gc])
                    s.op("pe", lambda e: e.matmul(ps[1][:, 0:NC8], lhsT=sel[:], rhs=gc[:], start=True, stop=True), reads=[sel, gc], writes=[ps[1]])
                    s.op("dve", lambda e: e.tensor_copy(out=glb[:], in_=ps[1][:, 0:NC8]), reads=[ps[1]], writes=[glb])
                    s.op("act", lambda e: e.activation(out=egc[:], in_=gc[:], func=AF.Exp), reads=[gc], writes=[egc])
                    s.op("act", lambda e: e.activation(out=egl[:], in_=glb[:], func=AF.Exp), reads=[glb], writes=[egl])
                    s.op("dve", lambda e: e.tensor_tensor(out=eglmg[:], in0=glb[:], in1=gc[:], op=ALU.subtract), reads=[glb, gc], writes=[eglmg])
                    s.op("act", lambda e: e.activation(out=eglmg[:], in_=eglmg[:], func=AF.Exp), reads=[eglmg], writes=[eglmg])
                    s.op("dve", lambda e: e.tensor_copy(out=g3(bet), in_=bdv), reads=[gbt], writes=[bet])
                    s.op("dve", lambda e: e.tensor_scalar(out=nbt[:], in0=bet[:], scalar1=-1.0, scalar2=None, op0=ALU.mult), reads=[bet], writes=[nbt])
                    s.op("dve", lambda e: e.tensor_tensor(out=begc[:], in0=bet[:], in1=egc[:], op=ALU.mult), reads=[bet, egc], writes=[begc])
                    tap("gc", gc, gc[:], [128, NC8]); tap("glb", glb, glb[:], [128, NC8]); tap("bet", bet, bet[:], [128, NC8])
                    s.op("dve", lambda e: e.memset(Sf[:], 0.0), writes=[Sf])
                    s.op("dve", lambda e: e.memset(Sb[:], 0.0), writes=[Sb])
                    order = list(range(NCH)) if dr == 0 else (list(range(ncx - 1, -1, -1)) + list(range(NCH - 1, ncx - 1, -1)))
                    for n_ in order:
                        c0 = n_ * 8
                        tk0 = n_ * CH
                        qc, kc, kt, vt, ot = qcb[it % 2], kcb[it % 2], ktb[it % 2], vtb[it % 2], otb[it % 2]
                        it += 1
                        s.dma(qc[:], qnT[b, :, tk0:tk0 + CH].rearrange("(h d) t -> d h t", d=64), reads=[("qknT", b)], writes=[qc])
                        s.dma(kc[:], knT[b, :, tk0:tk0 + CH].rearrange("(h d) t -> d h t", d=64), reads=[("qknT", b)], writes=[kc])
                        s.dma(kt[:], k_tok[b, tk0:tk0 + CH, :], reads=[("kvtok", b)], writes=[kt])
                        s.dma(vt[:], v_tok[b, tk0:tk0 + CH, :], reads=[("kvtok", b)], writes=[vt])
                        bc8 = lambda t_: t_[:, c0:c0 + 8].unsqueeze(2).broadcast_to([128, 8, 64])
                        v3 = lambda t_: t_[:].rearrange("p (h e) -> p h e", e=64)
                        s.op("dve", lambda e: e.tensor_tensor(out=v3(vb_), in0=v3(vt), in1=bc8(bet), op=ALU.mult), reads=[vt, bet], writes=[vb_])
                        s.op("dve", lambda e: e.tensor_tensor(out=v3(kbg), in0=v3(kt), in1=bc8(begc), op=ALU.mult), reads=[kt, begc], writes=[kbg])
                        s.op("dve", lambda e: e.tensor_tensor(out=v3(kdec), in0=v3(kt), in1=bc8(eglmg), op=ALU.mult), reads=[kt, eglmg], writes=[kdec])
                        d3 = lambda t_: t_[:].rearrange("p (h j) -> p h j", j=128)
                        pD, pE = ps[6], ps[7]
                        pEb = pE[:].bitcast(BF16)
                        bank = [(ps[0], ps[1], ps[2]), (ps[3], ps[4], ps[5])]
                        cur = [0, 0]
                        curT = [0, 0]
                        pcur = [0, 0]

                        def phase1(hg):
                            pA, pB_, pC = bank[hg]
                            Dt, Dst, intra = Dtl[hg], Dstl[hg], intral[hg]
                            Nk, NkT, PT = Nkl[hg], NkTl[hg], PTl[hg]
                            for hh in range(4):
                                h = hg * 4 + hh
                                s.op("pe", lambda e: e.matmul(pA[:, hh * 128:(hh + 1) * 128], lhsT=kc[:, h, :], rhs=kc[:, h, :], start=True, stop=True), reads=[kc], writes=[pA])
                            for hh in range(4):
                                h = hg * 4 + hh
                                s.op("pe", lambda e: e.matmul(pB_[:, hh * 128:(hh + 1) * 128], lhsT=qc[:, h, :], rhs=kc[:, h, :], start=True, stop=True), reads=[qc, kc], writes=[pB_])
                            dg4, ng4 = dg4l[hg], ng4l[hg]
                            gsl = gc[:, c0 + hg * 4:c0 + hg * 4 + 4].unsqueeze(2).broadcast_to([128, 4, 128])
                            s.op("dve", lambda e: e.tensor_tensor(out=d3(dg4), in0=ident[:].unsqueeze(1).broadcast_to([128, 4, 128]), in1=gsl, op=ALU.mult), reads=[ident, gc], writes=[dg4])
                            s.op("dve", lambda e: e.tensor_tensor(out=d3(ng4), in0=bigm[:].unsqueeze(1).broadcast_to([128, 4, 128]), in1=gsl, op=ALU.subtract), reads=[bigm, gc], writes=[ng4])
                            s.op("pe", lambda e: e.matmul(pC[:], lhsT=ones[:], rhs=dg4[:], start=True, stop=False), reads=[ones, dg4], writes=[pC])
                            s.op("pe", lambda e: e.matmul(pC[:], lhsT=ident[:], rhs=ng4[:], start=False, stop=True), reads=[ident, ng4], writes=[pC])
                            s.op("act", lambda e: e.activation(out=Dt[:], in_=pC[:], func=AF.Exp, scale=-1.0), reads=[pC], writes=[Dt])
                            s.op("dve", lambda e: e.tensor_tensor(out=intra[:], in0=pB_[:], in1=Dt[:], op=ALU.mult), reads=[pB_, Dt], writes=[intra])
                            s.op("dve", lambda e: e.tensor_tensor(out=d3(Dst), in0=d3(Dt), in1=notI[:].unsqueeze(1).broadcast_to([128, 4, 128]), op=ALU.mult), reads=[Dt, notI], writes=[Dst])
                            s.op("dve", lambda e: e.tensor_tensor(out=d3(Dst), in0=d3(Dst), in1=nbt[:, c0 + hg * 4:c0 + hg * 4 + 4].unsqueeze(2).broadcast_to([128, 4, 128]), op=ALU.mult),
                                 reads=[Dst, nbt], writes=[Dst])
                            s.op("dve", lambda e: e.tensor_tensor(out=Nk[2][:], in0=pA[:], in1=Dst[:], op=ALU.mult), reads=[pA, Dst], writes=[Nk[2]])
                            N0b, Ptb_, Pb, Zb = N0bl[hg], Ptbl[hg], Pbl[hg], Zbl[hg]
                            m0 = lmk[dr][0]
                            s.op("act", lambda e: e.copy(out=N0b[:], in_=Nk[2][:]), reads=[Nk[2]], writes=[N0b])
                            s.op("dve", lambda e: e.tensor_tensor(out=d3(Pb), in0=d3(Nk[2]), in1=m0[:].unsqueeze(1).broadcast_to([128, 4, 128]), op=ALU.mult), reads=[Nk[2], m0], writes=[Pb])
                            s.op("dve", lambda e: e.tensor_tensor(out=d3(Pb), in0=d3(Pb), in1=identb[:].unsqueeze(1).broadcast_to([128, 4, 128]), op=ALU.add), reads=[Pb, identb], writes=[Pb])
                            pDb = pD[:].bitcast(BF16)
                            for hh in range(4):
                                s.op("pe", lambda e: e.transpose(pDb[:, hh * 128:(hh + 1) * 128], Pb[:, hh * 128:(hh + 1) * 128], identb[:]), reads=[Pb, identb], writes=[pD])
                            for hh in range(4):
                                s.op("pe", lambda e: e.transpose(pEb[:, hh * 128:(hh + 1) * 128], intra[:, hh * 128:(hh + 1) * 128], identb[:]), reads=[intra, identb], writes=[pE])
                            s.op("act", lambda e: e.copy(out=Ptb_[0][:], in_=pDb[:, 0:512]), reads=[pD], writes=[Ptb_[0]])
                            s.op("act", lambda e: e.copy(out=intraT[:, hg * 512:(hg + 1) * 512], in_=pEb[:, 0:512]), reads=[pE], writes=[intraT])
                            cur[hg] = 0
                            pcur[hg] = 0

                        def nstep(hg, lv):
                            pA, pB_, pC = bank[hg]
                            N0b, Ptb_, Pb, Zb = N0bl[hg], Ptbl[hg], Pbl[hg], Zbl[hg]
                            PT = PTl[hg]
                            c_ = cur[hg]
                            X = Ptb_[c_]
                            mz = lmk[1 - dr][lv]
                            for hh in range(4):
                                sl = slice(hh * 128, (hh + 1) * 128)
                                s.op("pe", lambda e: e.matmul(pA[:, sl], lhsT=N0b[:, sl], rhs=X[:, sl], start=True, stop=True), reads=[N0b, X], writes=[pA])
                            s.op("dve", lambda e: e.tensor_tensor(out=d3(Zb), in0=pA[:].rearrange("p (h j) -> p h j", j=128), in1=mz[:].unsqueeze(1).broadcast_to([128, 4, 128]), op=ALU.mult),
                                 reads=[pA, mz], writes=[Zb])
                            for hh in range(4):
                                sl = slice(hh * 128, (hh + 1) * 128)
                                s.op("pe", lambda e: e.matmul(pB_[:, sl], lhsT=Pb[:, sl], rhs=Zb[:, sl], start=True, stop=True), reads=[Pb, Zb], writes=[pB_])
                            if lv < 6:
                                Xn = Ptb_[1 - c_]
                                s.op("dve", lambda e: e.tensor_tensor(out=Xn[:], in0=pB_[:], in1=X[:], op=ALU.add), reads=[pB_, X], writes=[Xn])
                                pCb = pC[:].bitcast(BF16)
                                for hh in range(4):
                                    sl = slice(hh * 128, (hh + 1) * 128)
                                    s.op("pe", lambda e: e.transpose(pCb[:, sl], Xn[:, sl], identb[:]), reads=[Xn, identb], writes=[pC])
                                s.op("act", lambda e: e.copy(out=Pb[:], in_=pCb[:, 0:512]), reads=[pC], writes=[Pb])
                                cur[hg] = 1 - c_
                            else:
                                s.op("dve", lambda e: e.tensor_tensor(out=PT[0][:], in0=pB_[:], in1=X[:], op=ALU.add), reads=[pB_, X], writes=[PT[0]])
                                pcur[hg] = 0

                        def tail(hg):
                            PT = PTl[hg]
                            TT = TTbl[hg]
                            pA, pB_, pC = bank[hg]
                            N0s = Nkl[hg][2]
                            Pm, Rt, Qm = Pml[hg], Rtl[hg], Qml[hg]
                            nsteps = cfg.get("newton", 0)
                            for ns_ in range(nsteps):
                                X = PT[pcur[hg]]
                                Xn = PT[1 - pcur[hg]]
                                for hh in range(4):
                                    sl = slice(hh * 128, (hh + 1) * 128)
                                    s.op("pe", lambda e: e.transpose(pA[:, sl], X[:, sl], ident[:]), reads=[X, ident], writes=[pA])
                                    s.op("pe", lambda e: e.matmul(pB_[:, sl], lhsT=N0s[:, sl], rhs=X[:, sl], start=True, stop=True), reads=[N0s, X], writes=[pB_])
                                s.op("act", lambda e: e.copy(out=Pm[:], in_=pA[:]), reads=[pA], writes=[Pm])
                                s.op("dve", lambda e: e.tensor_tensor(out=d3(Qm), in0=ident[:].unsqueeze(1).broadcast_to([128, 4, 128]), in1=d3(X), op=ALU.subtract), reads=[ident, X], writes=[Qm])
                                s.op("dve", lambda e: e.tensor_tensor(out=Rt[:], in0=pB_[:], in1=Qm[:], op=ALU.add), reads=[pB_, Qm], writes=[Rt])
                                for hh in range(4):
                                    sl = slice(hh * 128, (hh + 1) * 128)
                                    s.op("pe", lambda e: e.matmul(pC[:, sl], lhsT=Pm[:, sl], rhs=Rt[:, sl], start=True, stop=True), reads=[Pm, Rt], writes=[pC])
                                s.op("dve", lambda e: e.tensor_tensor(out=Xn[:], in0=pC[:], in1=X[:], op=ALU.add), reads=[pC, X], writes=[Xn])
                                pcur[hg] = 1 - pcur[hg]
                            s.op("act", lambda e: e.copy(out=TT[:], in_=PT[pcur[hg]][:]), reads=[PT[pcur[hg]]], writes=[TT])
                            pU = ps[6]
                            pW = bank[hg][2]
                            for hh in range(4):
                                h = hg * 4 + hh
                                sl = slice(hh * 128, (hh + 1) * 128)
                                s.op("pe", lambda e: e.matmul(pU[:, h * 64:(h + 1) * 64], lhsT=TT[:, sl], rhs=vb_[:, h * 64:(h + 1) * 64], start=True, stop=True), reads=[TT, vb_], writes=[pU])
                                s.op("pe", lambda e: e.matmul(pW[0:64, sl], lhsT=kbg[:, h * 64:(h + 1) * 64], rhs=TT[:, sl], start=True, stop=True), reads=[TT, kbg], writes=[pW])
                            s.op("act", lambda e: e.copy(out=wTs[:, hg * 4:(hg + 1) * 4, :], in_=pW[0:64, :].rearrange("p (h i) -> p h i", i=128)), reads=[pW], writes=[wTs])

                        for hg in range(2):
                            phase1(hg)
                        for lv in range(1, 7):
                            for hg in range(2):
                                nstep(hg, lv)
                        for hg in range(2):
                            tail(hg)
                        s.op("dve", lambda e: e.tensor_copy(out=us[:], in_=ps[6][:]), reads=[ps[6]], writes=[us])
                        tap("us", us, us[:], [128, 512]); tap("wTs", wTs, wTs[:], [64, 8, 128], BF16)
                        for h in range(8):
                            s.op("pe", lambda e: e.matmul(ps[0][:, h * 64:(h + 1) * 64], lhsT=wTs[:, h, :], rhs=Sb[:, h, :], start=True, stop=True), reads=[wTs, Sb], writes=[ps[0]])
                        s.op("dve", lambda e: e.tensor_tensor(out=vnew[:], in0=us[:], in1=ps[0][:], op=ALU.subtract), reads=[us, ps[0]], writes=[vnew])
                        for h in range(8):
                            s.op("pe", lambda e: e.matmul(ps[1][:, h * 64:(h + 1) * 64], lhsT=qc[:, h, :], rhs=Sb[:, h, :], start=True, stop=True), reads=[qc, Sb], writes=[ps[1]])
                        for h in range(8):
                            s.op("pe", lambda e: e.matmul(ps[2][:, h * 64:(h + 1) * 64], lhsT=intraT[:, h * 128:(h + 1) * 128], rhs=vnew[:, h * 64:(h + 1) * 64], start=True, stop=True),
                                 reads=[intraT, vnew], writes=[ps[2]])
                        for h in range(8):
                            s.op("pe", lambda e: e.matmul(ps[3][0:64, h * 64:(h + 1) * 64], lhsT=kdec[:, h * 64:(h + 1) * 64], rhs=vnew[:, h * 64:(h + 1) * 64], start=True, stop=True),
                                 reads=[kdec, vnew], writes=[ps[3]])
                        s.op("dve", lambda e: e.tensor_tensor(out=v3(tt), in0=ps[1][:].rearrange("p (h e) -> p h e", e=64), in1=bc8(egc), op=ALU.mult), reads=[ps[1], egc], writes=[tt])
                        s.op("dve", lambda e: e.tensor_tensor(out=ot[:], in0=tt[:], in1=ps[2][:], op=ALU.add), reads=[tt, ps[2]], writes=[ot])
                        tap("ot", ot, ot[:], [128, 512]); tap("vnew", vnew, vnew[:], [128, 512], BF16)
                        s.dma(o_dir[dr][b, tk0:tk0 + CH, :], ot[:], reads=[ot], writes=[("o_dir", b)])
                        s.op("dve", lambda e: e.tensor_tensor(out=Sf[:], in0=Sf[:], in1=egl[0:64, c0:c0 + 8].unsqueeze(2).broadcast_to([64, 8, 64]), op=ALU.mult), reads=[Sf, egl], writes=[Sf])
                        s.op("dve", lambda e: e.tensor_tensor(out=Sf[:], in0=Sf[:], in1=ps[3][0:64, :].rearrange("p (h e) -> p h e", e=64), op=ALU.add), reads=[Sf, ps[3]], writes=[Sf])
                        s.op("act", lambda e: e.copy(out=Sb[:], in_=Sf[:]), reads=[Sf], writes=[Sb])
            s.barrier()
        if stop == "DN":
            s.finish(); s.close(); return nc

        with contextlib.ExitStack() as ph:
            wo_a = s.sbuf("wo_a", [64, 8, D], BF16, stack=ph)
            wo_d = s.sbuf("wo_d", [128, 4, D], BF16, stack=ph)
            wst = [s.sbuf("wos%d" % i, [128, D], stack=ph) for i in range(2)]
            for i in range(12):
                st = wst[i % 2]
                if i < 8:
                    s.dma(st[0:64, :], w_out[l, i * 64:(i + 1) * 64, :], writes=[st])
                    s.op("act" if i % 2 else "dve", (lambda e: e.copy(out=wo_a[:, i, :], in_=st[0:64, :])) if i % 2 else (lambda e: e.tensor_copy(out=wo_a[:, i, :], in_=st[0:64, :])),
                         reads=[st], writes=[wo_a])
                else:
                    c = i - 8
                    s.dma(st[:, :], w_out[l, 512 + c * 128:512 + (c + 1) * 128, :], writes=[st])
                    s.op("act" if i % 2 else "dve", (lambda e: e.copy(out=wo_d[:, c, :], in_=st[:, :])) if i % 2 else (lambda e: e.tensor_copy(out=wo_d[:, c, :], in_=st[:, :])),
                         reads=[st], writes=[wo_d])
            wr = s.sbuf("wr", [128, 8, 36], stack=ph)
            rbb = s.sbuf("rbb", [128, 36], stack=ph)
            dnw = s.sbuf("dnw", [128, 64], stack=ph)
            s.dma(wr[:], wr_in[l, :, :].rearrange("(kc p) n -> p kc n", p=128), writes=[wr])
            s.dma(rbb[:], rb_in[l:l + 1, :].partition_broadcast(128), writes=[rbb])
            s.dma(dnw[:], dn_norm_w[l:l + 1, :].partition_broadcast(128), writes=[dnw])
            ofb = [s.sbuf("of%d" % i, [128, 4, 512], stack=ph) for i in range(2)]
            obb = [s.sbuf("ob%d" % i, [128, 4, 512], stack=ph) for i in range(2)]
            gsb = [s.sbuf("gs%d" % i, [128, 4, 512], stack=ph) for i in range(2)]
            atb = [s.sbuf("at%d" % i, [64, 8, 512], BF16, stack=ph) for i in range(2)]
            xtb = [s.sbuf("mxt%d" % i, [128, 8, 512], stack=ph) for i in range(2)]
            ss = s.sbuf("mss", [128, 32], stack=ph)
            dnT = s.sbuf("dnT", [128, 4, 512], BF16, stack=ph)
            sqb = s.sbuf("msq", [128, 8, 512], stack=ph)
            rstd = s.sbuf("mrstd", [128, 512], stack=ph)
            h2b = s.sbuf("h2b", [128, 8, 512], BF16, stack=ph)
            lg = s.sbuf("lg", [128, 4, 36], stack=ph)
            gmx = s.sbuf("gmx", [128, 4], stack=ph)
            oh = s.sbuf("oh", [128, 4, 4], stack=ph)
            eg = s.sbuf("eg", [128, 4, 4], stack=ph)
            sg = s.sbuf("sg", [128, 4], stack=ph)
            ml = s.sbuf("ml", [128, 4, 32], stack=ph)
            top8 = s.sbuf("top8", [128, 4, 8], stack=ph)
            selt = s.sbuf("selt", [128, 4, 32], stack=ph)
            ex = s.sbuf("ex", [128, 4, 32], stack=ph)
            se = s.sbuf("se", [128, 4], stack=ph)
            wts = s.sbuf("wts", [32, 512], stack=ph)
            it = 0
            for b in range(NB):
                for (t0, n) in tiles:
                    j = NB if t0 < L else b
                    nsub = n // 128
                    of_, ob_, gs_, at_, xt = ofb[it % 2], obb[it % 2], gsb[it % 2], atb[it % 2], xtb[it % 2]
                    it += 1
                    tm = lambda ap_: ap_.rearrange("(s p) f -> p s f", p=128)
                    s.dma(of_[:, 0:nsub, :], tm(o_dir[0][b, t0:t0 + n, :]), reads=[("o_dir", b)], writes=[of_])
                    s.dma(ob_[:, 0:nsub, :], tm(o_dir[1][b, t0:t0 + n, :]), reads=[("o_dir", b)], writes=[ob_])
                    s.dma(gs_[:, 0:nsub, :], tm(gate_s[b, t0:t0 + n, :]), reads=[("gate_s", b, t0)], writes=[gs_])
                    s.dma(at_[:, :, 0:n], attnT[b, :, :, t0:t0 + n].rearrange("h d t -> d h t"), reads=[("attnT", b)], writes=[at_])
                    s.dma(xt[:, :, 0:n], xT[b, :, t0:t0 + n].rearrange("(c p) t -> p c t", p=128), reads=[("xT", b, t0)], writes=[xt])
                    o4 = lambda t_: t_[:, 0:nsub, :].rearrange("p s (h e) -> p (s h) e", e=64)
                    s.op("dve", lambda e: e.tensor_tensor(out=of_[:, 0:nsub, :], in0=of_[:, 0:nsub, :], in1=ob_[:, 0:nsub, :], op=ALU.add), reads=[of_, ob_], writes=[of_])
                    s.op("act", lambda e: e.activation(out=ob_[:, 0:nsub, :], in_=of_[:, 0:nsub, :], func=AF.Square), reads=[of_], writes=[ob_])
                    s.op("dve", lambda e: e.tensor_reduce(out=ss[:, 0:nsub * 8], in_=o4(ob_), axis=AX.X, op=ALU.add), reads=[ob_], writes=[ss])
                    rsqrt_("act", ss[:, 0:nsub * 8], ss[:, 0:nsub * 8], [ss], [ss], ss[:, 0:nsub * 8], scale=1.0 / 64)
                    s.op("dve", lambda e: e.tensor_tensor(out=o4(of_), in0=o4(of_), in1=ss[:, 0:nsub * 8].unsqueeze(2).broadcast_to([128, nsub * 8, 64]), op=ALU.mult), reads=[of_, ss], writes=[of_])
                    s.op("dve", lambda e: e.tensor_tensor(out=o4(of_), in0=o4(of_), in1=dnw[:].unsqueeze(1).broadcast_to([128, nsub * 8, 64]), op=ALU.mult), reads=[of_, dnw], writes=[of_])
                    s.op("dve", lambda e: e.tensor_tensor(out=of_[:, 0:nsub, :], in0=of_[:, 0:nsub, :], in1=gs_[:, 0:nsub, :], op=ALU.mult), reads=[of_, gs_], writes=[of_])
                    for c in range(4):
                        pT_ = ps[c % 2]
                        for sub in range(nsub):
                            s.op("pe", lambda e: e.transpose(pT_[:, sub * 128:(sub + 1) * 128], of_[:, sub, c * 128:(c + 1) * 128], ident[:]), reads=[of_, ident], writes=[pT_])
                        if c % 2:
                            s.op("act", lambda e: e.copy(out=dnT[:, c, 0:n], in_=pT_[:, 0:n]), reads=[pT_], writes=[dnT])
                        else:
                            s.op("dve", lambda e: e.tensor_copy(out=dnT[:, c, 0:n], in_=pT_[:, 0:n]), reads=[pT_], writes=[dnT])
                    for oc in range(8):
                        py = ps[2 + oc % 4]
                        osl = slice(oc * 128, (oc + 1) * 128)
                        for h in range(8):
                            s.op("pe", lambda e: e.matmul(py[:, 0:n], lhsT=wo_a[0:64, h, osl], rhs=at_[0:64, h, 0:n], start=(h == 0), stop=False), reads=[wo_a, at_], writes=[py])
                        for c in range(4):
                            s.op("pe", lambda e: e.matmul(py[:, 0:n], lhsT=wo_d[:, c, osl], rhs=dnT[:, c, 0:n], start=False, stop=(c == 3)), reads=[wo_d, dnT], writes=[py])
                        s.op("dve", lambda e: e.scalar_tensor_tensor(out=xt[:, oc, 0:n], in0=py[:, 0:n], scalar=mod[l][:, 16 + oc, j:j + 1], in1=xt[:, oc, 0:n], op0=ALU.mult, op1=ALU.add),
                             reads=[py, mod[l], xt], writes=[xt])
                    s.dma(xT[b, :, t0:t0 + n].rearrange("(c p) t -> p c t", p=128), xt[:, :, 0:n], reads=[xt], writes=[("xT", b, t0)])
                    s.op("act", lambda e: e.activation(out=sqb[:, :, 0:n], in_=xt[:, :, 0:n], func=AF.Square), reads=[xt], writes=[sqb])
                    for c in range(8):
                        s.op("pe", lambda e: e.matmul(ps[6][:, 0:n], lhsT=onesm[:], rhs=sqb[:, c, 0:n], start=(c == 0), stop=(c == 7)), reads=[onesm, sqb], writes=[ps[6]])
                    rsqrt_("act", rstd[:, 0:n], ps[6][:, 0:n], [ps[6]], [rstd], rstd[:, 0:n])
                    s.op("dve", lambda e: e.tensor_tensor(out=sqb[:, :, 0:n], in0=xt[:, :, 0:n], in1=rstd[:, 0:n].unsqueeze(1).broadcast_to([128, 8, n]), op=ALU.mult),
                         reads=[xt, rstd], writes=[sqb])
                    for c in range(8):
                        s.op("dve", lambda e: e.tensor_scalar(out=sqb[:, c, 0:n], in0=sqb[:, c, 0:n], scalar1=affn[l][:, c, j:j + 1], scalar2=mod[l][:, 24 + c, j:j + 1],
                                                              op0=ALU.mult, op1=ALU.add), reads=[sqb, affn[l], mod[l]], writes=[sqb])
                    s.op("act", lambda e: e.copy(out=h2b[:, :, 0:n], in_=sqb[:, :, 0:n]), reads=[sqb], writes=[h2b])
                    s.dma(h2T[b, :, t0:t0 + n].rearrange("(c p) t -> p c t", p=128), h2b[:, :, 0:n], reads=[h2b], writes=[("h2T", b)])
                    pl_ = ps[7]
                    for sub in range(nsub):
                        for kc in range(8):
                            s.op("pe", lambda e: e.matmul(pl_[:, sub * 36:(sub + 1) * 36], lhsT=sqb[:, kc, sub * 128:(sub + 1) * 128], rhs=wr[:, kc, :], start=(kc == 0), stop=(kc == 7)),
                                 reads=[sqb, wr], writes=[pl_])
                    L3 = lg[:, 0:nsub, :]
                    s.op("dve", lambda e: e.tensor_tensor(out=L3, in0=pl_[:, 0:nsub * 36].rearrange("p (s c) -> p s c", c=36), in1=rbb[:].unsqueeze(1).broadcast_to([128, nsub, 36]), op=ALU.add),
                         reads=[pl_, rbb], writes=[lg])
                    s.op("dve", lambda e: e.tensor_reduce(out=gmx[:, 0:nsub], in_=lg[:, 0:nsub, 0:4], axis=AX.X, op=ALU.max), reads=[lg], writes=[gmx])
                    gmb = gmx[:, 0:nsub].unsqueeze(2).broadcast_to([128, nsub, 4])
                    s.op("dve", lambda e: e.tensor_tensor(out=oh[:, 0:nsub, :], in0=lg[:, 0:nsub, 0:4], in1=gmb, op=ALU.is_equal), reads=[lg, gmx], writes=[oh])
                    s.op("dve", lambda e: e.tensor_tensor(out=eg[:, 0:nsub, :], in0=lg[:, 0:nsub, 0:4], in1=gmb, op=ALU.subtract), reads=[lg, gmx], writes=[eg])
                    s.op("act", lambda e: e.activation(out=eg[:, 0:nsub, :], in_=eg[:, 0:nsub, :], func=AF.Exp), reads=[eg], writes=[eg])
                    s.op("dve", lambda e: e.tensor_reduce(out=sg[:, 0:nsub], in_=eg[:, 0:nsub, :], axis=AX.X, op=ALU.add), reads=[eg], writes=[sg])
                    s.op("dve", lambda e: e.tensor_scalar(out=oh[:, 0:nsub, :], in0=oh[:, 0:nsub, :], scalar1=1.0e30, scalar2=-1.0e30, op0=ALU.mult, op1=ALU.add), reads=[oh], writes=[oh])
                    s.op("dve", lambda e: e.tensor_tensor(out=ml[:, 0:nsub, :].rearrange("p s (g x) -> p s g x", x=8), in0=lg[:, 0:nsub, 4:36].rearrange("p s (g x) -> p s g x", x=8),
                                                          in1=oh[:, 0:nsub, :].unsqueeze(3).broadcast_to([128, nsub, 4, 8]), op=ALU.add), reads=[lg, oh], writes=[ml])
                    for sub in range(nsub):
                        s.op("dve", lambda e: e.max(out=top8[:, sub, :], in_=ml[:, sub, :]), reads=[ml], writes=[top8])
                    s.op("dve", lambda e: e.tensor_tensor(out=selt[:, 0:nsub, :], in0=ml[:, 0:nsub, :], in1=top8[:, 0:nsub, 1:2].broadcast_to([128, nsub, 32]), op=ALU.is_ge), reads=[ml, top8], writes=[selt])
                    s.op("dve", lambda e: e.tensor_tensor(out=ex[:, 0:nsub, :], in0=ml[:, 0:nsub, :], in1=top8[:, 0:nsub, 0:1].broadcast_to([128, nsub, 32]), op=ALU.subtract), reads=[ml, top8], writes=[ex])
                    s.op("act", lambda e: e.activation(out=ex[:, 0:nsub, :], in_=ex[:, 0:nsub, :], func=AF.Exp), reads=[ex], writes=[ex])
                    s.op("dve", lambda e: e.tensor_tensor(out=ex[:, 0:nsub, :], in0=ex[:, 0:nsub, :], in1=selt[:, 0:nsub, :], op=ALU.mult), reads=[ex, selt], writes=[ex])
                    s.op("dve", lambda e: e.tensor_reduce(out=se[:, 0:nsub], in_=ex[:, 0:nsub, :], axis=AX.X, op=ALU.add), reads=[ex], writes=[se])
                    s.op("dve", lambda e: e.tensor_tensor(out=se[:, 0:nsub], in0=se[:, 0:nsub], in1=sg[:, 0:nsub], op=ALU.mult), reads=[se, sg], writes=[se])
                    s.op("dve", lambda e: e.reciprocal(out=se[:, 0:nsub], in_=se[:, 0:nsub]), reads=[se], writes=[se])
                    s.op("dve", lambda e: e.tensor_tensor(out=ex[:, 0:nsub, :], in0=ex[:, 0:nsub, :], in1=se[:, 0:nsub].unsqueeze(2).broadcast_to([128, nsub, 32]), op=ALU.mult), reads=[ex, se], writes=[ex])
                    pw_ = ps[0]
                    for sub in range(nsub):
                        s.op("pe", lambda e: e.transpose(pw_[0:32, sub * 128:(sub + 1) * 128], ex[:, sub, :], ident[:]), reads=[ex, ident], writes=[pw_])
                    s.op("act", lambda e: e.copy(out=wts[:, 0:n], in_=pw_[0:32, 0:n]), reads=[pw_], writes=[wts])
                    s.dma(WtT[b, :, t0:t0 + n], wts[:, 0:n], reads=[wts], writes=[("WtT", b)])
            s.barrier()
        if stop == "M":
            s.finish(); s.close(); return nc

        half = (len(tiles) + 1) // 2
        groups = [tiles[:half], tiles[half:]]
        GMAX = max(sum(n for (_, n) in g) for g in groups)
        with contextlib.ExitStack() as ph:
            h2g = s.sbuf("h2g", [128, 8, GMAX], BF16, stack=ph)
            acc = s.sbuf("eacc", [128, 8, GMAX], stack=ph)
            for b in range(NB):
                for grp in groups:
                    g0 = grp[0][0]
                    G = sum(n for (_, n) in grp)
                    s.dma(h2g[:, :, 0:G], h2T[b, :, g0:g0 + G].rearrange("(c p) t -> p c t", p=128), reads=[("h2T", b)], writes=[h2g])
                    with contextlib.ExitStack() as ph2:
                        stg = [s.sbuf("stg%d" % i, [128, 2048], stack=ph2) for i in range(2)]
                        w1b = [s.sbuf("w1b%d" % i, [128, 8, FF], BF16, stack=ph2) for i in range(2)]
                        w3b = [s.sbuf("w3b%d" % i, [128, 8, FF], BF16, stack=ph2) for i in range(2)]
                        w2b = [s.sbuf("w2b%d" % i, [128, 2, D], BF16, stack=ph2) for i in range(2)]
                        wbc = [s.sbuf("wbc%d" % i, [128, GMAX], stack=ph2) for i in range(2)]
                        hhb2 = [[s.sbuf("hhc%d_%d" % (i, k_), [128, 512], BF16, stack=ph2) for k_ in range(2)] for i in range(2)]
                        eel = [s.sbuf("eel%d" % i, [128, 512], stack=ph2) for i in range(2)]
                        hhl = [s.sbuf("hhl%d" % i, [128, 512], stack=ph2) for i in range(2)]
                        sicnt = [0]

                        def load_w(ex_):
                            st_ = ex_ % 2
                            for (dstw, srcw) in ((w1b[st_], w1_in[l, ex_, :, :].rearrange("(kc p) f -> p kc f", p=128)),
                                                 (w3b[st_], w3_in[l, ex_, :, :].rearrange("(kc p) f -> p kc f", p=128)),
                                                 (w2b[st_], w2_in[l, ex_, :, :].rearrange("(fc p) n -> p fc n", p=128))):
                                sg_ = stg[sicnt[0] % 2]
                                sicnt[0] += 1
                                a_ = dstw.t.shape[1]
                                s.dma(sg_[:].rearrange("p (a b) -> p a b", a=a_), srcw, writes=[sg_])
                                s.op("act", lambda e: e.copy(out=dstw[:], in_=sg_[:].rearrange("p (a b) -> p a b", a=a_)), reads=[sg_], writes=[dstw])
                            s.dma(wbc[st_][:, 0:G], WtT[b, ex_:ex_ + 1, g0:g0 + G].partition_broadcast(128), reads=[("WtT", b)], writes=[wbc[st_]])

                        items = [(ex_, ti) for ex_ in range(NEXP) for ti in range(len(grp))]

                        def stage1(idx):
                            ex_, ti = items[idx]
                            st_ = ex_ % 2
                            t0, n = grp[ti]
                            u0 = t0 - g0
                            for fc in range(2):
                                pa, pb = ps[fc * 2], ps[fc * 2 + 1]
                                ee, hh = eel[fc], hhl[fc]
                                for kc in range(8):
                                    s.op("pe", lambda e: e.matmul(pa[:, 0:n], lhsT=w1b[st_][:, kc, fc * 128:(fc + 1) * 128], rhs=h2g[:, kc, u0:u0 + n], start=(kc == 0), stop=(kc == 7)),
                                         reads=[w1b[st_], h2g], writes=[pa])
                                for kc in range(8):
                                    s.op("pe", lambda e: e.matmul(pb[:, 0:n], lhsT=w3b[st_][:, kc, fc * 128:(fc + 1) * 128], rhs=h2g[:, kc, u0:u0 + n], start=(kc == 0), stop=(kc == 7)),
                                         reads=[w3b[st_], h2g], writes=[pb])
                                s.op("act", lambda e: e.activation(out=ee[:, 0:n], in_=pa[:, 0:n], func=AF.Exp, scale=-1.0), reads=[pa], writes=[ee])
                                s.op("act", lambda e: e.activation(out=ee[:, 0:n], in_=ee[:, 0:n], func=AF.Ln, bias=1.0), reads=[ee], writes=[ee])
                                s.op("act", lambda e: e.activation(out=ee[:, 0:n], in_=ee[:, 0:n], func=AF.Exp, scale=-1.0), reads=[ee], writes=[ee])
                                s.op("dve", lambda e: e.tensor_tensor(out=hh[:, 0:n], in0=pa[:, 0:n], in1=ee[:, 0:n], op=ALU.mult), reads=[pa, ee], writes=[hh])
                                s.op("dve", lambda e: e.tensor_tensor(out=hh[:, 0:n], in0=pb[:, 0:n], in1=hh[:, 0:n], op=ALU.mult), reads=[pb, hh], writes=[hh])
                                s.op("dve", lambda e: e.tensor_tensor(out=hhb2[idx % 2][fc][:, 0:n], in0=hh[:, 0:n], in1=wbc[st_][:, u0:u0 + n], op=ALU.mult),
                                     reads=[hh, wbc[st_]], writes=[hhb2[idx % 2][fc]])

                        def stage2(idx):
                            ex_, ti = items[idx]
                            st_ = ex_ % 2
                            t0, n = grp[ti]
                            u0 = t0 - g0
                            for oc in range(8):
                                py = ps[4 + oc % 4]
                                for fc in range(2):
                                    s.op("pe", lambda e: e.matmul(py[:, 0:n], lhsT=w2b[st_][:, fc, oc * 128:(oc + 1) * 128], rhs=hhb2[idx % 2][fc][:, 0:n], start=(fc == 0), stop=(fc == 1)),
                                         reads=[w2b[st_], hhb2[idx % 2][fc]], writes=[py])
                                if ex_ == 0:
                                    s.op("act", lambda e: e.copy(out=acc[:, oc, u0:u0 + n], in_=py[:, 0:n]), reads=[py], writes=[acc])
                                else:
                                    s.op("dve", lambda e: e.tensor_tensor(out=acc[:, oc, u0:u0 + n], in0=py[:, 0:n], in1=acc[:, oc, u0:u0 + n], op=ALU.add), reads=[py, acc], writes=[acc])

                        load_w(0)
                        for idx in range(len(items) + 1):
                            if idx < len(items):
                                stage1(idx)
                            if idx >= 1:
                                stage2(idx - 1)
                            if idx < len(items) and items[idx][1] == 0 and items[idx][0] + 1 < NEXP:
                                load_w(items[idx][0] + 1)
                        s.barrier()
                    with contextlib.ExitStack() as ph2:
                        xtb = [s.sbuf("ext%d" % i, [128, 8, 512], stack=ph2) for i in range(2)]
                        for ti, (t0, n) in enumerate(grp):
                            j = NB if t0 < L else b
                            u0 = t0 - g0
                            xt = xtb[ti % 2]
                            s.dma(xt[:, :, 0:n], xT[b, :, t0:t0 + n].rearrange("(c p) t -> p c t", p=128), reads=[("xT", b, t0)], writes=[xt])
                            for oc in range(8):
                                s.op("dve", lambda e: e.scalar_tensor_tensor(out=xt[:, oc, 0:n], in0=acc[:, oc, u0:u0 + n], scalar=mod[l][:, 40 + oc, j:j + 1], in1=xt[:, oc, 0:n],
                                                                             op0=ALU.mult, op1=ALU.add), reads=[acc, mod[l], xt], writes=[xt])
                            s.dma(xT[b, :, t0:t0 + n].rearrange("(c p) t -> p c t", p=128), xt[:, :, 0:n], reads=[xt], writes=[("xT", b, t0)])
                        s.barrier()
        if stop == "E":
            s.finish(); s.close(); return nc

    with contextlib.ExitStack() as ph:
        fnw = s.sbuf("fnw", [128, 8], stack=ph)
        s.dma(fnw[:], fnwT[:, :], writes=[fnw])
        xtb = [s.sbuf("fxt%d" % i, [128, 8, 512], stack=ph) for i in range(2)]
        sqb = s.sbuf("fsq", [128, 8, 512], stack=ph)
        rstd = s.sbuf("frstd", [128, 512], stack=ph)
        otb = [s.sbuf("fot%d" % i, [128, 4, D], stack=ph) for i in range(2)]
        it = 0
        for b in range(NB):
            for (t0, n) in tiles:
                if t0 < L:
                    continue
                nsub = n // 128
                xt, ot = xtb[it % 2], otb[it % 2]
                it += 1
                s.dma(xt[:, :, 0:n], xT[b, :, t0:t0 + n].rearrange("(c p) t -> p c t", p=128), reads=[("xT", b, t0)], writes=[xt])
                s.op("act", lambda e: e.activation(out=sqb[:, :, 0:n], in_=xt[:, :, 0:n], func=AF.Square), reads=[xt], writes=[sqb])
                for c in range(8):
                    s.op("pe", lambda e: e.matmul(ps[0][:, 0:n], lhsT=onesm[:], rhs=sqb[:, c, 0:n], start=(c == 0), stop=(c == 7)), reads=[onesm, sqb], writes=[ps[0]])
                rsqrt_("act", rstd[:, 0:n], ps[0][:, 0:n], [ps[0]], [rstd], rstd[:, 0:n])
                s.op("dve", lambda e: e.tensor_tensor(out=sqb[:, :, 0:n], in0=xt[:, :, 0:n], in1=rstd[:, 0:n].unsqueeze(1).broadcast_to([128, 8, n]), op=ALU.mult), reads=[xt, rstd], writes=[sqb])
                s.op("dve", lambda e: e.tensor_tensor(out=sqb[:, :, 0:n], in0=sqb[:, :, 0:n], in1=fnw[:].unsqueeze(2).broadcast_to([128, 8, n]), op=ALU.mult), reads=[sqb, fnw], writes=[sqb])
                for sub in range(nsub):
                    for hf in range(2):
                        pT_ = ps[1 + (sub * 2 + hf) % 4]
                        for c4 in range(4):
                            c = hf * 4 + c4
                            s.op("pe", lambda e: e.transpose(pT_[:, c4 * 128:(c4 + 1) * 128], sqb[:, c, sub * 128:(sub + 1) * 128], ident[:]), reads=[sqb, ident], writes=[pT_])
                        if hf:
                            s.op("act", lambda e: e.copy(out=ot[:, sub, hf * 512:(hf + 1) * 512], in_=pT_[:, :]), reads=[pT_], writes=[ot])
                        else:
                            s.op("dve", lambda e: e.tensor_copy(out=ot[:, sub, hf * 512:(hf + 1) * 512], in_=pT_[:, :]), reads=[pT_], writes=[ot])
                s.dma(out_hbm[b, t0 - L:t0 - L + n, :].rearrange("(s p) f -> p s f", p=128), ot[:, 0:nsub, :], reads=[ot])
    s.finish()
    s.close()
    return nc


def _partner():
    d = np.arange(64)
    return np.where((d % 32) < 16, d + 16, d - 16)


def host_consts(S, L):
    T = S + L
    c = {}
    c["c_ident"] = np.eye(128, dtype=np.float32)
    bd = np.zeros((128, 128), np.float32)
    bd[:64, :64] = 1.0
    bd[64:, 64:] = 1.0
    c["c_bd"] = bd
    d = np.arange(128) % 64
    axis = d // 32
    f = d % 16
    inv = (10000.0 ** (-(np.arange(16, dtype=np.float32)) / 16.0)).astype(np.float32)
    tl = np.arange(S)
    pos = np.stack([(tl // 64).astype(np.float32), (tl % 64).astype(np.float32)], 0)
    ang = pos[axis, :] * inv[f][:, None]
    cos = np.ones((128, T), np.float32)
    sin = np.zeros((128, T), np.float32)
    cos[:, L:] = np.cos(ang.astype(np.float32))
    sgn = np.where((d % 32) < 16, -1.0, 1.0).astype(np.float32)
    sin[:, L:] = np.sin(ang.astype(np.float32)) * sgn[:, None]
    c["c_cos"] = cos
    c["c_sin"] = sin
    p = np.arange(128)[:, None]
    i = np.arange(128)[None, :]
    m = np.zeros((6, 128, 128), np.float32)
    m[0] = (p <= i)
    m[1] = (p >= i)
    m[2] = (p == 127)
    m[3] = (p == 0)
    m[4] = np.where(i > p, BIG, 0.0)
    m[5] = np.where(i < p, BIG, 0.0)
    c["c_masks"] = m
    lm = np.zeros((2, 7, 128, 128), np.float32)
    for l_ in range(7):
        f = ((p >> (l_ + 1)) == (i >> (l_ + 1))) & (((p >> l_) & 1) == 1) & (((i >> l_) & 1) == 0)
        lm[0, l_] = f
        lm[1, l_] = f.T
    c["c_lmask"] = lm
    return c


def host_weights(inp):
    DEPTH = inp["w_in"].shape[0]
    o = {}
    o["ada_w"] = np.ascontiguousarray(inp["ada_w"])
    o["ada_bT"] = np.ascontiguousarray(inp["ada_b"].reshape(DEPTH, 48, 128).transpose(0, 2, 1))
    o["nmixT"] = np.ascontiguousarray(inp["norm_mix_w"].reshape(DEPTH, 8, 128).transpose(0, 2, 1))
    o["nffnT"] = np.ascontiguousarray(inp["norm_ffn_w"].reshape(DEPTH, 8, 128).transpose(0, 2, 1))
    o["fnwT"] = np.ascontiguousarray(inp["final_norm_w"].reshape(8, 128).T)
    w = inp["w_in"]
    pt = _partner()
    ext = np.empty((DEPTH, D, WEXT), np.float32)
    ext[:, :, CQ:WEXT - 640] = w[:, :, 0:2848]
    qcols = np.empty(512, np.int64)
    rqcols = np.empty(512, np.int64)
    for c in range(4):
        for two in range(2):
            h = two * 4 + c
            qcols[c * 128 + two * 64:c * 128 + two * 64 + 64] = h * 64 + np.arange(64)
            rqcols[c * 128 + two * 64:c * 128 + two * 64 + 64] = h * 64 + pt
    ext[:, :, CQ:CQ + 512] = w[:, :, qcols]
    ext[:, :, CRQ:CRQ + 512] = w[:, :, rqcols]
    rk = np.concatenate([512 + pt, 512 + 64 + pt])
    ext[:, :, CRK:CRK + 128] = w[:, :, rk]
    o["w_in_ext"] = ext
    qw, kw = inp["q_norm_w"], inp["k_norm_w"]
    dd = np.arange(128) % 64
    o["qkw"] = np.ascontiguousarray(np.stack([qw[:, dd], qw[:, pt[dd]], kw[:, dd], kw[:, pt[dd]]], -1))
    o["qkw_row"] = np.ascontiguousarray(np.stack([qw, kw], 1))
    o["conv_wT"] = np.ascontiguousarray(inp["conv_w"].reshape(DEPTH, 5, 12, 128).transpose(0, 3, 2, 1))
    o["dn_A_log"] = np.ascontiguousarray(inp["dn_A_log"].reshape(DEPTH, 16))
    o["dn_dt_bias"] = np.ascontiguousarray(inp["dn_dt_bias"].reshape(DEPTH, 16))
    o["dn_norm_w"] = np.ascontiguousarray(inp["dn_norm_w"])
    o["w_out"] = np.ascontiguousarray(inp["w_out"])
    o["wr"] = np.ascontiguousarray(np.concatenate([inp["rg_w"], inp["re_w"]], -1))
    o["rb"] = np.ascontiguousarray(np.concatenate([inp["rg_b"], inp["re_b"]], -1))
    o["w1"] = np.ascontiguousarray(inp["w1"])
    o["w3"] = np.ascontiguousarray(inp["w3"])
    o["w2"] = np.ascontiguousarray(inp["w2"])
    return o


def host_core_inputs(inp, core, NB):
    b0 = core * NB
    o = {}
    o["x"] = np.ascontiguousarray(inp["x"][b0:b0 + NB])
    o["ctx"] = np.ascontiguousarray(inp["ctx"][b0:b0 + NB])
    vecs = [inp["c"][b0 + j] for j in range(NB)] + [inp["c_ctx"]]
    o["cT"] = np.ascontiguousarray(np.stack(vecs, -1).reshape(8, 128, NB + 1).transpose(1, 0, 2))
    return o


def kernel(**inputs):
    inputs = {k: np.asarray(v, dtype=np.float32) for k, v in inputs.items()}
    B, S, _ = inputs["x"].shape
    L = inputs["ctx"].shape[1]
    DEPTH = inputs["w_in"].shape[0]
    ncores = 8
    NB = B // ncores
    cfg = dict(NB=NB, S=S, L=L, DEPTH=DEPTH)
    nc = build(cfg)
    shared = host_weights(inputs)
    shared.update(host_consts(S, L))
    in_maps = []
    for core in range(ncores):
        m = dict(shared)
        m.update(host_core_inputs(inputs, core, NB))
        in_maps.append(m)
    res = run_bass_kernel_spmd(nc, in_maps, core_ids=list(range(ncores)))
    return np.concatenate([r["out"] for r in res.results], axis=0)
```
